# Optimizing a Trainium2 kernel written in Bass

```python
import jax, jax.numpy as jnp
from jax import lax
import numpy as np

D_MODEL = 1024
BATCH = 2
SEQ = 16384
DEPTH = 2

MLA_HEADS = 16
MLA_Q_RANK = 384
MLA_KV_RANK = 256
MLA_NOPE = 64
MLA_ROPE = 32
MLA_QK = MLA_NOPE + MLA_ROPE
MLA_V = 64
ROPE_THETA = 10000.0
Q_BLOCK = 128
GLA_HEADS = 4
GLA_DK = D_MODEL // 2 // GLA_HEADS
GLA_DV = D_MODEL // GLA_HEADS
GLA_GATE_RANK = 16
GLA_TAU = 16.0
GLA_CHUNK = 64
PEER_HEADS = 8
PEER_NKEYS = 128
PEER_EXPERTS = PEER_NKEYS * PEER_NKEYS
PEER_QDIM = 128
PEER_HALF = PEER_QDIM // 2
PEER_TOPK = 16
PEER_BLOCK = 128
NORM_EPS = 1e-6
N_MLA = (DEPTH + 1) // 2
N_GLA = DEPTH // 2

kernel_name = "hybrid_mla_gla_peer_trunk"


def rms_norm(x, gain):
    xf = x.astype(jnp.float32)
    y = xf * lax.rsqrt(jnp.mean(xf * xf, axis=-1, keepdims=True) + NORM_EPS)
    return (y * gain.astype(jnp.float32)).astype(x.dtype)


def apply_rope(x, positions):
    half = x.shape[-1] // 2
    inv_freq = ROPE_THETA ** (-jnp.arange(half, dtype=jnp.float32) / half)
    ang = positions.astype(jnp.float32)[..., None] * inv_freq
    cos = jnp.cos(ang)[:, :, None, :]
    sin = jnp.sin(ang)[:, :, None, :]
    x1 = x[..., :half].astype(jnp.float32)
    x2 = x[..., half:].astype(jnp.float32)
    out = jnp.concatenate([x1 * cos - x2 * sin, x2 * cos + x1 * sin], axis=-1)
    return out.astype(x.dtype)


def causal_attention(q, k, v):
    B, S, H, Dqk = q.shape
    Dv = v.shape[-1]
    nb = S // Q_BLOCK
    scale = Dqk ** -0.5
    kh = k.transpose(0, 2, 1, 3)
    vh = v.transpose(0, 2, 1, 3)
    qb = q.reshape(B, nb, Q_BLOCK, H, Dqk).transpose(1, 0, 3, 2, 4)
    key_idx = jnp.arange(S)

    def one_block(args):
        qi, blk = args
        s = jnp.einsum('bhqd,bhkd->bhqk', qi, kh).astype(jnp.float32) * scale
        q_idx = blk * Q_BLOCK + jnp.arange(Q_BLOCK)
        mask = key_idx[None, :] <= q_idx[:, None]
        s = jnp.where(mask, s, -jnp.inf)
        p = jax.nn.softmax(s, axis=-1).astype(vh.dtype)
        return jnp.einsum('bhqk,bhkd->bhqd', p, vh)

    o = lax.map(one_block, (qb, jnp.arange(nb)))
    return o.transpose(1, 0, 3, 2, 4).reshape(B, S, H, Dv)


def mla_mixer(x, positions, w_down, g_q_lat, w_uq, g_kv_lat, w_ukv, g_qn, g_kn, w_o):
    B, S, _ = x.shape
    down = x @ w_down
    c_q = rms_norm(down[..., :MLA_Q_RANK], g_q_lat)
    c_kv = rms_norm(down[..., MLA_Q_RANK:MLA_Q_RANK + MLA_KV_RANK], g_kv_lat)
    k_rope = down[..., MLA_Q_RANK + MLA_KV_RANK:]
    q = (c_q @ w_uq).reshape(B, S, MLA_HEADS, MLA_QK)
    kv = (c_kv @ w_ukv).reshape(B, S, MLA_HEADS, MLA_NOPE + MLA_V)
    k = jnp.concatenate([kv[..., :MLA_NOPE],
                         jnp.broadcast_to(k_rope[:, :, None, :], (B, S, MLA_HEADS, MLA_ROPE))], axis=-1)
    v = kv[..., MLA_NOPE:]
    q = rms_norm(q, g_qn)
    k = rms_norm(k, g_kn)
    q = jnp.concatenate([q[..., :MLA_NOPE], apply_rope(q[..., MLA_NOPE:], positions)], axis=-1)
    k = jnp.concatenate([k[..., :MLA_NOPE], apply_rope(k[..., MLA_NOPE:], positions)], axis=-1)
    o = causal_attention(q, k, v)
    return o.reshape(B, S, MLA_HEADS * MLA_V) @ w_o


def chunked_gla(q, k, v, log_a):
    B, S, H, DK = q.shape
    DV = v.shape[-1]
    C = GLA_CHUNK
    n = S // C

    def to_chunks(t):
        return t.astype(jnp.float32).reshape(B, n, C, H, t.shape[-1]).transpose(1, 0, 3, 2, 4)

    qc, kc, vc, gc = to_chunks(q), to_chunks(k), to_chunks(v), to_chunks(log_a)
    b = jnp.cumsum(gc, axis=3)
    b_last = b[..., -1:, :]
    q_dec = qc * jnp.exp(b)
    k_inv = kc * jnp.exp(-b)
    causal = jnp.tril(jnp.ones((C, C), dtype=bool))
    attn = jnp.where(causal, jnp.einsum('nbhtk,nbhsk->nbhts', q_dec, k_inv), 0.0)
    o_intra = jnp.einsum('nbhts,nbhsv->nbhtv', attn, vc)
    kv_chunk = jnp.einsum('nbhsk,nbhsv->nbhkv', kc * jnp.exp(b_last - b), vc)
    chunk_decay = jnp.exp(b_last[..., 0, :])

    def step(state, inp):
        qd, kv, dec = inp
        o = jnp.einsum('bhtk,bhkv->bhtv', qd, state)
        return state * dec[..., None] + kv, o

    state0 = jnp.zeros((B, H, DK, DV), jnp.float32)
    _, o_inter = lax.scan(step, state0, (q_dec, kv_chunk, chunk_decay))
    o = o_intra + o_inter
    return o.transpose(1, 0, 3, 2, 4).reshape(B, S, H, DV)


def gla_mixer(x, w_in, w_g2, b_g, g_on, w_o):
    B, S, _ = x.shape
    HK = GLA_HEADS * GLA_DK
    HV = GLA_HEADS * GLA_DV
    proj = x @ w_in
    q = proj[..., :HK].reshape(B, S, GLA_HEADS, GLA_DK) * (GLA_DK ** -0.5)
    k = proj[..., HK:2 * HK].reshape(B, S, GLA_HEADS, GLA_DK)
    v = proj[..., 2 * HK:2 * HK + HV].reshape(B, S, GLA_HEADS, GLA_DV)
    r = proj[..., 2 * HK + HV:2 * HK + 2 * HV]
    g_low = proj[..., 2 * HK + 2 * HV:]
    log_a = jax.nn.log_sigmoid((g_low @ w_g2 + b_g).astype(jnp.float32)) / GLA_TAU
    log_a = log_a.reshape(B, S, GLA_HEADS, GLA_DK)
    o = chunked_gla(q, k, v, log_a)
    o = rms_norm(o, g_on).reshape(B, S, HV).astype(x.dtype)
    return (o * jax.nn.silu(r)) @ w_o


def peer_ffn(x, w_query, sub_keys, u_tab, v_tab):
    B, S, D = x.shape
    T = PEER_BLOCK
    nb = S // T
    xb = x.reshape(B, nb, T, D).transpose(1, 0, 2, 3)

    def one_block(xi):
        q = (xi @ w_query).reshape(B, T, PEER_HEADS, 2, PEER_HALF)
        s = jnp.einsum('bthcd,cnd->bthcn', q, sub_keys).astype(jnp.float32)
        s_top, i_top = lax.top_k(s, PEER_TOPK)
        cand = s_top[..., 0, :, None] + s_top[..., 1, None, :]
        cand_idx = i_top[..., 0, :, None] * PEER_NKEYS + i_top[..., 1, None, :]
        best, pos = lax.top_k(cand.reshape(B, T, PEER_HEADS, PEER_TOPK * PEER_TOPK), PEER_TOPK)
        idx = jnp.take_along_axis(cand_idx.reshape(B, T, PEER_HEADS, PEER_TOPK * PEER_TOPK), pos, axis=-1)
        gate = jax.nn.softmax(best, axis=-1)
        u = u_tab[idx]
        h = jax.nn.gelu(jnp.einsum('bthkd,btd->bthk', u, xi).astype(jnp.float32), approximate=False)
        w = (gate * h).astype(xi.dtype)
        return jnp.einsum('bthk,bthkd->btd', w, v_tab[idx])

    out = lax.map(one_block, xb)
    return out.transpose(1, 0, 2, 3).reshape(B, S, D)


def setup_inputs(seed: int = 0) -> dict:
    key = jax.random.key(seed)
    ks = jax.random.split(key, 24)

    def normal(k, shape, scale):
        return jax.random.normal(k, shape, jnp.float32) * scale

    def gain(k, shape):
        return 1.0 + 0.02 * jax.random.normal(k, shape, jnp.float32)

    D = D_MODEL
    HK = GLA_HEADS * GLA_DK
    HV = GLA_HEADS * GLA_DV
    x = jax.random.normal(ks[0], (BATCH, SEQ, D), jnp.float32)
    offsets = jax.random.randint(ks[1], (BATCH, 1), 0, 4096, dtype=jnp.int32)
    positions = (offsets + jnp.arange(SEQ, dtype=jnp.int32)[None, :]).astype(jnp.int32)
    return {
        "x": x,
        "positions": positions,
        "attn_norm_g": gain(ks[2], (DEPTH, D)),
        "ffn_norm_g": gain(ks[3], (DEPTH, D)),
        "mla_w_down": normal(ks[4], (N_MLA, D, MLA_Q_RANK + MLA_KV_RANK + MLA_ROPE), D ** -0.5),
        "mla_g_q_lat": gain(ks[5], (N_MLA, MLA_Q_RANK)),
        "mla_w_uq": normal(ks[6], (N_MLA, MLA_Q_RANK, MLA_HEADS * MLA_QK), MLA_Q_RANK ** -0.5),
        "mla_g_kv_lat": gain(ks[7], (N_MLA, MLA_KV_RANK)),
        "mla_w_ukv": normal(ks[8], (N_MLA, MLA_KV_RANK, MLA_HEADS * (MLA_NOPE + MLA_V)), MLA_KV_RANK ** -0.5),
        "mla_g_qn": gain(ks[9], (N_MLA, MLA_QK)),
        "mla_g_kn": gain(ks[10], (N_MLA, MLA_QK)),
        "mla_w_o": normal(ks[11], (N_MLA, MLA_HEADS * MLA_V, D), (MLA_HEADS * MLA_V) ** -0.5),
        "gla_w_in": normal(ks[12], (N_GLA, D, 2 * HK + 2 * HV + GLA_GATE_RANK), D ** -0.5),
        "gla_w_g2": normal(ks[13], (N_GLA, GLA_GATE_RANK, HK), GLA_GATE_RANK ** -0.5),
        "gla_b_g": normal(ks[14], (N_GLA, HK), 0.02),
        "gla_g_on": gain(ks[15], (N_GLA, GLA_DV)),
        "gla_w_o": normal(ks[16], (N_GLA, HV, D), HV ** -0.5),
        "peer_w_query": normal(ks[17], (DEPTH, D, PEER_HEADS * PEER_QDIM), D ** -0.5),
        "peer_sub_keys": normal(ks[18], (DEPTH, 2, PEER_NKEYS, PEER_HALF), PEER_HALF ** -0.5),
        "peer_u": normal(ks[19], (DEPTH, PEER_EXPERTS, D), D ** -0.5),
        "peer_v": normal(ks[20], (DEPTH, PEER_EXPERTS, D), 0.5 * PEER_HEADS ** -0.5),
    }


def reference(x, positions, attn_norm_g, ffn_norm_g, mla_w_down, mla_g_q_lat, mla_w_uq, mla_g_kv_lat,
              mla_w_ukv, mla_g_qn, mla_g_kn, mla_w_o, gla_w_in, gla_w_g2, gla_b_g, gla_g_on, gla_w_o,
              peer_w_query, peer_sub_keys, peer_u, peer_v):
    h = x
    for i in range(DEPTH):
        hn = rms_norm(h, attn_norm_g[i])
        j = i // 2
        if i % 2 == 0:
            mix = mla_mixer(hn, positions, mla_w_down[j], mla_g_q_lat[j], mla_w_uq[j], mla_g_kv_lat[j],
                            mla_w_ukv[j], mla_g_qn[j], mla_g_kn[j], mla_w_o[j])
        else:
            mix = gla_mixer(hn, gla_w_in[j], gla_w_g2[j], gla_b_g[j], gla_g_on[j], gla_w_o[j])
        h = h + mix
        h = h + peer_ffn(rms_norm(h, ffn_norm_g[i]), peer_w_query[i], peer_sub_keys[i], peer_u[i], peer_v[i])
    return h
```

```python
from contextlib import ExitStack
import numpy as np
import ml_dtypes
import concourse.bass as bass
import concourse.mybir as mybir
from concourse.bass_utils import run_bass_kernel_spmd

AF = mybir.ActivationFunctionType
ALU = mybir.AluOpType
AX = mybir.AxisListType
F32, BF16, I32, U32 = mybir.dt.float32, mybir.dt.bfloat16, mybir.dt.int32, mybir.dt.uint32

ENGS = ("pe", "act", "dve", "pool", "sp")
EPS = 1e-6


class T:
    __slots__ = ("h", "w", "r", "name", "psum")

    def __init__(self, h, name="", psum=False):
        self.h = h
        self.w = None
        self.r = {}
        self.name = name
        self.psum = psum

    def __getitem__(self, idx):
        return self.h[idx]


class KB:
    def __init__(self, nc, n_dma_sems=32):
        self.nc = nc
        self.es = ExitStack()
        self.prog = {e: [] for e in ENGS}
        self.sems = {}
        self.cnt = {}
        for e in ENGS:
            self.sems[e] = self.es.enter_context(nc.semaphore("s_" + e))
            self.cnt[e] = 0
        self.dsems = []
        self.dq = {"sp": [], "pool": [], "act": []}
        for q, n in (("sp", n_dma_sems), ("pool", 16)):
            for i in range(n):
                k = "d%s%d" % (q, i)
                self.sems[k] = self.es.enter_context(nc.semaphore("s_" + k))
                self.cnt[k] = 0
                self.dsems.append(k)
                self.dq[q].append(k)
        self.dnext = {"sp": 0, "pool": 0}
        self.seen = {e: {} for e in ENGS}
        self.final = []
        self.pending = {e: [] for e in ENGS}

    def sb(self, name, shape, dt):
        h = self.es.enter_context(self.nc.sbuf_tensor(name, list(shape), dt))
        return T(h, name)

    def ps(self, name, shape, dt):
        h = self.es.enter_context(self.nc.psum_tensor(name, list(shape), dt))
        return T(h, name, psum=True)

    def _waits(self, eng, reads, writes, relaxed=()):
        deps = {}

        def add(k, v):
            if v > deps.get(k, 0):
                deps[k] = v
        for t in reads:
            if t.w is not None:
                k, v = t.w
                if not (k == eng and eng == "pe"):
                    add(k, v)
            if t.psum:
                for k, v in t.r.items():
                    if k != eng:
                        add(k, v)
        for t in writes:
            same_ok = (eng == "pe") or any(t is r for r in relaxed)
            if t.w is not None:
                k, v = t.w
                if k != eng or not same_ok:
                    add(k, v)
            for k, v in t.r.items():
                if k != eng or not same_ok:
                    add(k, v)
        out = []
        seen = self.seen[eng]
        for k, v in deps.items():
            if seen.get(k, 0) >= v:
                continue
            seen[k] = v
            out.append((k, v))
        return out

    def _commit(self, done, reads, writes):
        k, v = done
        for t in writes:
            t.w = done
            t.r = {}
        for t in reads:
            if t.r.get(k, 0) < v:
                t.r[k] = v

    def dma_barrier(self, eng):
        for k in self.dsems:
            v = self.cnt[k]
            if v > 0 and self.seen[eng].get(k, 0) < v:
                self.seen[eng][k] = v
                self.pending[eng].append((k, v))

    def op(self, eng, fn, reads=(), writes=(), relaxed=()):
        waits = self.pending[eng] + self._waits(eng, reads, writes, relaxed)
        self.pending[eng] = []
        self.cnt[eng] += 1
        done = (eng, self.cnt[eng])
        self.prog[eng].append((waits, fn, (eng, 1)))
        self._commit(done, reads, writes)
        return done

    def dma(self, eng, fn, reads=(), writes=(), final=False):
        k = self.dq[eng][self.dnext[eng]]
        self.dnext[eng] = (self.dnext[eng] + 1) % len(self.dq[eng])
        waits = self.pending[eng] + self._waits(eng, reads, writes)
        self.pending[eng] = []
        prev = self.cnt[k]
        if prev > 0 and self.seen[eng].get(k, 0) < prev:
            self.seen[eng][k] = prev
            waits.append((k, prev))
        self.cnt[k] += 16
        done = (k, self.cnt[k])
        self.prog[eng].append((waits, fn, (k, 16)))
        self._commit(done, reads, writes)
        if final:
            self.final.append(done)
        return done

    def finish(self):
        nc = self.nc
        fw = [(k, self.cnt[k]) for k in self.dsems if self.cnt[k] > 0]
        sems = self.sems
        prog = self.prog

        def replay(name):
            def f(eng):
                for waits, fn, inc in prog[name]:
                    for k, v in waits:
                        eng.wait_ge(sems[k], v)
                    ins = fn(eng)
                    ins.then_inc(sems[inc[0]], inc[1])
                if name == "sp":
                    for k, v in fw:
                        eng.wait_ge(sems[k], v)
            return f
        with nc.Block() as block:
            block.tensor(replay("pe"))
            block.scalar(replay("act"))
            block.vector(replay("dve"))
            block.gpsimd(replay("pool"))
            block.sync(replay("sp"))
        self.es.close()

    def mm(self, out, lhsT, rhs, start, stop, reads, writes):
        return self.op("pe", lambda e: e.matmul(out, lhsT=lhsT, rhs=rhs, start=start, stop=stop), reads, writes)

    def tr(self, out, in_, ident, reads, writes):
        return self.op("pe", lambda e: e.transpose(out, in_, ident), reads, writes)

    def act(self, out, in_, func, reads, writes, bias=None, scale=None, accum=None):
        kw = {}
        if bias is not None:
            kw["bias"] = bias
        if scale is not None:
            kw["scale"] = scale
        if accum is not None:
            kw["accum_out"] = accum
        return self.op("act", lambda e: e.activation(out=out, in_=in_, func=func, **kw), reads, writes)

    def cp(self, eng, out, in_, reads, writes):
        if eng == "act":
            return self.op("act", lambda e: e.copy(out=out, in_=in_), reads, writes)
        return self.op(eng, lambda e: e.tensor_copy(out=out, in_=in_), reads, writes)

    def tt(self, eng, out, in0, in1, op, reads, writes):
        return self.op(eng, lambda e: e.tensor_tensor(out=out, in0=in0, in1=in1, op=op), reads, writes)

    def ts(self, eng, out, in0, s1, s2, op0, op1, reads, writes, accum=None):
        if op1 is None:
            return self.op(eng, lambda e: e.tensor_scalar(out=out, in0=in0, scalar1=s1, scalar2=None, op0=op0), reads, writes)
        if accum is not None:
            return self.op(eng, lambda e: e.tensor_scalar(out=out, in0=in0, scalar1=s1, scalar2=s2, op0=op0, op1=op1, accum_out=accum), reads, writes)
        return self.op(eng, lambda e: e.tensor_scalar(out=out, in0=in0, scalar1=s1, scalar2=s2, op0=op0, op1=op1), reads, writes)

    def stt(self, out, in0, scalar, in1, op0, op1, reads, writes, accum=None, relaxed=()):
        if accum is not None:
            return self.op("dve", lambda e: e.scalar_tensor_tensor(out=out, in0=in0, scalar=scalar, in1=in1, op0=op0, op1=op1, accum_out=accum), reads, writes, relaxed)
        return self.op("dve", lambda e: e.scalar_tensor_tensor(out=out, in0=in0, scalar=scalar, in1=in1, op0=op0, op1=op1), reads, writes)

    def red(self, eng, out, in_, op, reads, writes):
        return self.op(eng, lambda e: e.tensor_reduce(out=out, in_=in_, axis=AX.X, op=op), reads, writes)

    def memset(self, eng, ap, val, writes):
        return self.op(eng, lambda e: e.memset(ap, val), (), writes)

    def load(self, eng, out, in_, writes, reads=()):
        return self.dma(eng, lambda e: e.dma_start(out=out, in_=in_), reads, writes)

    def store(self, eng, out, in_, reads, final=False, writes=()):
        return self.dma(eng, lambda e: e.dma_start(out=out, in_=in_), reads, writes, final=final)


def dram_bcast(row, nparts):
    n = row.shape[-1]
    return bass.AP(row.tensor, row.offset, [[0, nparts], [1, n]])


def fap(ap, dims):
    return bass.AP(ap.tensor, ap.offset, [list(ap.ap[0])] + [list(d) for d in dims])


def rms_rstd(kb, x_ap, n, junk, ssq, rstd, reads, eng="act"):
    kb.act(junk[:, 0:n], x_ap, AF.Square, reads, [junk, ssq], accum=ssq[:, 0:1])
    kb.ts("dve", rstd[:, 0:1], ssq[:, 0:1], 1.0 / n, EPS, ALU.mult, ALU.add, [ssq], [rstd])
    kb.act(rstd[:, 0:1], rstd[:, 0:1], AF.Sqrt, [rstd], [rstd])
    kb.op("dve", lambda e: e.reciprocal(out=rstd[:, 0:1], in_=rstd[:, 0:1]), [rstd], [rstd])


class Peer:
    def __init__(self, kb, cst, NG=6):
        self.kb = kb
        self.cst = cst
        sb, ps = kb.sb, kb.ps
        self.G = sb("pe_G", [128, 1024], F32)
        self.Wq = sb("pe_Wq", [128, 8, 1024], BF16)
        self.KBD = sb("pe_KBD", [128, 256], F32)
        self.SK = sb("pe_SK", [128, 128], F32)
        self.junk = sb("pe_junk", [128, 1024], F32)
        self.junk2 = sb("pe_junk2", [128, 1024], F32)
        self.ssq = sb("pe_ssq", [128, 1], F32)
        self.rstd = sb("pe_rstd", [128, 1], F32)
        self.xn = sb("pe_xn", [128, 1024], F32)
        self.xnb = sb("pe_xnb", [128, 1024], BF16)
        self.xnT = sb("pe_xnT", [128, 8, 128], BF16)
        self.qT = sb("pe_qT", [128, 8, 128], F32)
        self.S = sb("pe_S", [128, 16, 128], F32)
        self.S2 = sb("pe_S2", [128, 16, 128], F32)
        self.V1 = sb("pe_V1", [128, 16, 16], F32)
        self.I1 = sb("pe_I1", [128, 16, 16], U32)
        self.I1f = sb("pe_I1f", [128, 16, 16], F32)
        self.CA = sb("pe_CA", [128, 8, 256], F32)
        self.CA2 = sb("pe_CA2", [128, 8, 256], F32)
        self.BV = sb("pe_BV", [128, 8, 16], F32)
        self.BP = sb("pe_BP", [128, 8, 16], U32)
        self.PA = sb("pe_PA", [128, 8, 16], U32)
        self.PB = sb("pe_PB", [128, 8, 16], U32)
        self.PAf = sb("pe_PAf", [128, 8, 16], F32)
        self.PBf = sb("pe_PBf", [128, 8, 16], F32)
        self.OH = sb("pe_OH", [128, 8, 16, 16], F32)
        self.SEL1 = sb("pe_SEL1", [128, 8, 16], F32)
        self.SEL2 = sb("pe_SEL2", [128, 8, 16], F32)
        self.IDXf = sb("pe_IDXf", [128, 128], F32)
        self.IDX = sb("pe_IDX", [128, 128], I32)
        self.GT = sb("pe_GT", [128, 8, 16], F32)
        self.Z = sb("pe_Z", [128, 8], F32)
        self.HD = sb("pe_HD", [128, 128], F32)
        self.W = sb("pe_W", [128, 128], F32)
        self.NG = NG
        self.gb = [sb("pe_gb%d" % i, [128, 1024], F32) for i in range(NG)]
        self.gi = 0

    def load_weights(self, g_ffn, w_query, sub_keys, u_tab, v_tab, ps_tr):
        kb = self.kb
        self.u_tab, self.v_tab = u_tab, v_tab
        kb.load("sp", self.G[:], dram_bcast(g_ffn, 128), [self.G])
        wq = w_query.rearrange("(c p) n -> p c n", p=128)
        for c in range(8):
            kb.load("pool", self.Wq[:, c, :], wq[:, c, :], [self.Wq])
        kb.load("sp", self.SK[:].rearrange("p (c d) -> p c d", c=2), sub_keys.rearrange("c n d -> n c d"), [self.SK])
        kb.tr(ps_tr[:, 0:128], self.SK[:], self.cst.identf[:], [self.SK, self.cst.identf], [ps_tr])
        kb.memset("dve", self.KBD[:], 0.0, [self.KBD])
        kb.cp("dve", self.KBD[0:64, 0:128], ps_tr[0:64, 0:128], [ps_tr], [self.KBD])
        kb.cp("dve", self.KBD[64:128, 128:256], ps_tr[64:128, 0:128], [ps_tr], [self.KBD])

    def tile(self, h, psb):
        kb, c = self.kb, self.cst
        rms_rstd(kb, h[:], 1024, self.junk, self.ssq, self.rstd, [h])
        kb.stt(self.xn[:], h[:], self.rstd[:, 0:1], self.G[:], ALU.mult, ALU.mult, [h, self.rstd, self.G], [self.xn])
        kb.cp("pool", self.xnb[:], self.xn[:], [self.xn], [self.xnb])
        pt = psb[0]
        ptb = pt[:].bitcast(BF16)
        for ch in range(8):
            kb.tr(ptb[:, ch * 128:(ch + 1) * 128], self.xnb[:, ch * 128:(ch + 1) * 128], c.identb[:], [self.xnb, c.identb], [pt])
        kb.cp("act", self.xnT[:].rearrange("p c t -> p (c t)"), ptb, [pt], [self.xnT])
        for hh in range(8):
            bank = psb[1 + hh // 4]
            o = bank[:, (hh % 4) * 128:(hh % 4 + 1) * 128]
            for ch in range(8):
                kb.mm(o, self.Wq[:, ch, hh * 128:(hh + 1) * 128], self.xnT[:, ch, :], ch == 0, ch == 7, [self.Wq, self.xnT], [bank])
        kb.cp("act", self.qT[:, 0:4, :].rearrange("p h t -> p (h t)"), psb[1][:], [psb[1]], [self.qT])
        kb.cp("act", self.qT[:, 4:8, :].rearrange("p h t -> p (h t)"), psb[2][:], [psb[2]], [self.qT])
        for hh in range(8):
            bank = psb[3 + hh // 2]
            o = bank[:, (hh % 2) * 256:(hh % 2 + 1) * 256]
            kb.mm(o, self.qT[:, hh, :], self.KBD[:], True, True, [self.qT, self.KBD], [bank])
        for b4 in range(4):
            kb.cp("act" if b4 % 2 == 0 else "dve", self.S[:, b4 * 4:(b4 + 1) * 4, :].rearrange("p g n -> p (g n)"), psb[3 + b4][:], [psb[3 + b4]], [self.S])
        S, S2, V1, I1 = self.S, self.S2, self.V1, self.I1
        for g in range(16):
            kb.op("dve", (lambda g: lambda e: e.max(out=V1[:, g, 0:8], in_=S[:, g, :]))(g), [S], [V1])
        for g in range(16):
            kb.op("dve", (lambda g: lambda e: e.match_replace(out=S2[:, g, :], in_to_replace=V1[:, g, 0:8], in_values=S[:, g, :], imm_value=-1e30))(g), [S, V1], [S2])
        for g in range(16):
            kb.op("dve", (lambda g: lambda e: e.max(out=V1[:, g, 8:16], in_=S2[:, g, :]))(g), [S2], [V1])
        for g in range(16):
            kb.op("dve", (lambda g: lambda e: e.max_index(out=I1[:, g, 0:8], in_max=V1[:, g, 0:8], in_values=S[:, g, :]))(g), [S, V1], [I1])
            kb.op("dve", (lambda g: lambda e: e.max_index(out=I1[:, g, 8:16], in_max=V1[:, g, 8:16], in_values=S[:, g, :]))(g), [S, V1], [I1])
        kb.cp("dve", self.I1f[:], I1[:], [I1], [self.I1f])
        v1 = V1[:]
        in0 = fap(v1, [[32, 8], [1, 16], [0, 16]])
        in1 = fap(V1[:, 1:2, :], [[32, 8], [0, 16], [1, 16]])
        CA = self.CA
        kb.tt("dve", CA[:].rearrange("p h (a b) -> p h a b", a=16), in0, in1, ALU.add, [V1], [CA])
        CA2, BV, BP = self.CA2, self.BV, self.BP
        for hh in range(8):
            kb.op("dve", (lambda g: lambda e: e.max(out=BV[:, g, 0:8], in_=CA[:, g, :]))(hh), [CA], [BV])
        for hh in range(8):
            kb.op("dve", (lambda g: lambda e: e.match_replace(out=CA2[:, g, :], in_to_replace=BV[:, g, 0:8], in_values=CA[:, g, :], imm_value=-1e30))(hh), [CA, BV], [CA2])
        for hh in range(8):
            kb.op("dve", (lambda g: lambda e: e.max(out=BV[:, g, 8:16], in_=CA2[:, g, :]))(hh), [CA2], [BV])
        for hh in range(8):
            kb.op("dve", (lambda g: lambda e: e.max_index(out=BP[:, g, 0:8], in_max=BV[:, g, 0:8], in_values=CA[:, g, :]))(hh), [CA, BV], [BP])
            kb.op("dve", (lambda g: lambda e: e.max_index(out=BP[:, g, 8:16], in_max=BV[:, g, 8:16], in_values=CA[:, g, :]))(hh), [CA, BV], [BP])
        BPf = self.SEL1
        kb.cp("dve", BPf[:], BP[:], [BP], [BPf])
        OH = self.OH
        kb.tt("dve", OH[:], fap(BPf[:], [[16, 8], [1, 16], [0, 16]]), fap(c.thr16[:], [[0, 8], [0, 16], [1, 16]]), ALU.is_ge, [BPf, c.thr16], [OH])
        kb.red("dve", self.PAf[:], OH[:], ALU.add, [OH], [self.PAf])
        kb.stt(self.PBf[:].rearrange("p h k -> p (h k)"), self.PAf[:].rearrange("p h k -> p (h k)"), -16.0, BPf[:].rearrange("p h k -> p (h k)"),
               ALU.mult, ALU.add, [self.PAf, BPf], [self.PBf])
        OH = self.OH
        io = fap(c.iota16[:], [[0, 8], [0, 16], [1, 16]])
        for (Pf, SEL, off) in ((self.PAf, self.SEL1, 0), (self.PBf, self.SEL2, 16)):
            pfb = fap(Pf[:], [[16, 8], [1, 16], [0, 16]])
            kb.tt("dve", OH[:], pfb, io, ALU.is_equal, [Pf, c.iota16], [OH])
            i1b = fap(self.I1f[:, off // 16:off // 16 + 1, :], [[32, 8], [0, 16], [1, 16]])
            kb.tt("dve", OH[:], OH[:], i1b, ALU.mult, [OH, self.I1f], [OH])
            kb.red("dve", SEL[:], OH[:], ALU.add, [OH], [SEL])
        kb.stt(self.IDXf[:], self.SEL1[:].rearrange("p h k -> p (h k)"), 128.0, self.SEL2[:].rearrange("p h k -> p (h k)"),
               ALU.mult, ALU.add, [self.SEL1, self.SEL2], [self.IDXf])
        kb.cp("dve", self.IDX[:], self.IDXf[:], [self.IDXf], [self.IDX])
        GT, Z = self.GT, self.Z
        kb.tt("dve", GT[:], BV[:], fap(BV[:], [[16, 8], [0, 16]]), ALU.subtract, [BV], [GT])
        kb.act(GT[:], GT[:], AF.Exp, [GT], [GT])
        kb.red("dve", Z[:], GT[:], ALU.add, [GT], [Z])
        kb.op("dve", lambda e: e.reciprocal(out=Z[:], in_=Z[:]), [Z], [Z])
        kb.tt("dve", GT[:], GT[:], fap(Z[:], [[1, 8], [0, 16]]), ALU.mult, [GT, Z], [GT])
        HD, W = self.HD, self.W
        for s in range(128):
            gb = self.gb[self.gi % self.NG]
            self.gi += 1
            kb.dma("pool", (lambda gb, s: lambda e: e.indirect_dma_start(out=gb[:], out_offset=None, in_=self.u_tab,
                   in_offset=bass.IndirectOffsetOnAxis(ap=self.IDX[:, s:s + 1], axis=0)))(gb, s), [self.IDX], [gb])
            jk = self.junk if s % 2 == 0 else self.junk2
            kb.stt(jk[:], gb[:], 1.0, self.xn[:], ALU.mult, ALU.mult, [gb, self.xn], [jk, HD], accum=HD[:, s:s + 1], relaxed=(HD,))
        kb.act(W[:], HD[:], AF.Gelu, [HD], [W])
        kb.tt("dve", W[:], W[:], GT[:].rearrange("p h k -> p (h k)"), ALU.mult, [W, GT], [W])
        for s in range(128):
            gb = self.gb[self.gi % self.NG]
            self.gi += 1
            kb.dma("pool", (lambda gb, s: lambda e: e.indirect_dma_start(out=gb[:], out_offset=None, in_=self.v_tab,
                   in_offset=bass.IndirectOffsetOnAxis(ap=self.IDX[:, s:s + 1], axis=0)))(gb, s), [self.IDX], [gb])
            kb.stt(h[:], gb[:], W[:, s:s + 1], h[:], ALU.mult, ALU.add, [gb, W, h], [h])
        if getattr(self, "dbg", None):
            d = self.dbg
            for nm, t in (("IDX", self.IDX), ("GT", self.GT), ("HD", self.HD), ("V1", self.V1), ("I1", self.I1), ("BV", self.BV),
                          ("BP", self.BP), ("xn", self.xn), ("S", self.S), ("PAf", self.PAf), ("PBf", self.PBf), ("qT", self.qT), ("W", self.W)):
                ap = t[:]
                if len(ap.shape) == 3:
                    ap = ap.rearrange("p a b -> p (a b)")
                kb.store("sp", d[nm], ap, [t], final=True)


class Consts:
    def __init__(self, kb, cst_dram):
        self.identf = kb.sb("c_identf", [128, 128], F32)
        self.identb = kb.sb("c_identb", [128, 128], BF16)
        self.iota16 = kb.sb("c_iota16", [128, 16], F32)
        self.triuf = kb.sb("c_triuf", [128, 128], F32)
        self.triub = kb.sb("c_triub", [128, 128], BF16)
        self.ropec = kb.sb("c_ropec", [128, 64], F32)
        self.onesf = kb.sb("c_onesf", [128, 128], F32)
        self.onesb = kb.sb("c_onesb", [128, 128], BF16)
        kb.load("sp", self.identf[:], cst_dram[:, 0:128], [self.identf])
        kb.load("sp", self.iota16[:], cst_dram[:, 128:144], [self.iota16])
        kb.load("sp", self.triuf[:], cst_dram[:, 144:272], [self.triuf])
        kb.load("sp", self.ropec[:], cst_dram[:, 272:336], [self.ropec])
        kb.cp("dve", self.identb[:], self.identf[:], [self.identf], [self.identb])
        kb.cp("dve", self.triub[:], self.triuf[:], [self.triuf], [self.triub])
        kb.memset("dve", self.onesf[:], 1.0, [self.onesf])
        kb.memset("dve", self.onesb[:], 1.0, [self.onesb])
        self.thr16 = kb.sb("c_thr16", [128, 16], F32)
        kb.ts("dve", self.thr16[:], self.iota16[:], 1.0, 16.0, ALU.add, ALU.mult, [self.iota16], [self.thr16])


def host_consts():
    c = np.zeros((128, 336), np.float32)
    c[:, 0:128] = np.eye(128, dtype=np.float32)
    c[:, 128:144] = np.arange(16, dtype=np.float32)[None, :]
    c[:, 144:272] = np.triu(np.ones((128, 128), np.float32))
    inv = (10000.0 ** (-np.arange(16, dtype=np.float32) / 16)).astype(np.float32)
    c[:, 272:288] = inv[None, :]
    c[:, 288:304] = inv[None, :]
    c[:, 304:320] = np.float32(np.pi / 2)
    c[:, 320:336] = 0.0
    return c


def build_peer_only(ntiles, dbg=False):
    nc = bass.Bass("TRN2", target_bir_lowering=False)
    h_d = nc.dram_tensor("h", [ntiles * 128, 1024], F32, kind="ExternalInput").ap()
    g_d = nc.dram_tensor("g_ffn", [1, 1024], F32, kind="ExternalInput").ap()
    wq_d = nc.dram_tensor("w_query", [1024, 1024], F32, kind="ExternalInput").ap()
    sk_d = nc.dram_tensor("sub_keys", [2, 128, 64], F32, kind="ExternalInput").ap()
    u_d = nc.dram_tensor("u_tab", [16384, 1024], F32, kind="ExternalInput").ap()
    v_d = nc.dram_tensor("v_tab", [16384, 1024], F32, kind="ExternalInput").ap()
    c_d = nc.dram_tensor("cst", [128, 336], F32, kind="ExternalInput").ap()
    o_d = nc.dram_tensor("out", [ntiles * 128, 1024], F32, kind="ExternalOutput").ap()
    kb = KB(nc)
    cst = Consts(kb, c_d)
    psb = [kb.ps("psb%d" % i, [128, 512], F32) for i in range(8)]
    peer = Peer(kb, cst)
    if dbg:
        peer.dbg = {}
        for nm, w, dt in (("IDX", 128, I32), ("GT", 128, F32), ("HD", 128, F32), ("V1", 256, F32), ("I1", 256, U32), ("BV", 128, F32),
                          ("BP", 128, U32), ("xn", 1024, F32), ("S", 2048, F32), ("PAf", 128, F32), ("PBf", 128, F32), ("qT", 1024, F32), ("W", 128, F32)):
            peer.dbg[nm] = nc.dram_tensor("dbg_" + nm, [128, w], dt, kind="ExternalOutput").ap()
    peer.load_weights(g_d[0:1, :], wq_d, sk_d, u_d, v_d, psb[7])
    hb = [kb.sb("hb%d" % i, [128, 1024], F32) for i in range(2)]
    for t in range(ntiles):
        h = hb[t % 2]
        kb.load("sp", h[:], h_d[t * 128:(t + 1) * 128, :], [h])
        peer.tile(h, psb)
        kb.store("sp", o_d[t * 128:(t + 1) * 128, :], h[:], [h], final=True)
    kb.finish()
    return nc


def phase_A(kb, cst, psb, S, D):
    sb = kb.sb
    NT, NS = S // 128, S // 512
    TWO_PI = float(2 * np.pi)
    Wd = sb("a_Wd", [128, 8, 672], BF16); Wuq = sb("a_Wuq", [128, 3, 384], BF16); Wukv = sb("a_Wukv", [128, 2, 512], BF16)
    Gat = sb("a_Gat", [128, 1024], F32); Gq = sb("a_Gq", [128, 384], F32); Gkv = sb("a_Gkv", [128, 256], F32)
    Gqn = sb("a_Gqn", [128, 96], F32); Gkn = sb("a_Gkn", [128, 96], F32)
    wd = D["w_down"].rearrange("(c p) n -> p c n", p=128)
    for c in range(8):
        kb.load("pool", Wd[:, c, :], wd[:, c, :], [Wd])
    wq = D["w_uq"].rearrange("(c p) n -> p c n", p=128)
    for c in range(3):
        kb.load("pool", Wuq[:, c, :], wq[:, c, :], [Wuq])
    wk = D["w_ukv"].rearrange("(c p) n -> p c n", p=128)
    for c in range(2):
        kb.load("pool", Wukv[:, c, :], wk[:, c, :], [Wukv])
    for (g, src) in ((Gat, "g_attn"), (Gq, "g_q"), (Gkv, "g_kv"), (Gqn, "g_qn"), (Gkn, "g_kn")):
        kb.load("sp", g[:], dram_bcast(D[src], 128), [g])
    POS = sb("a_POS", [128, NT], I32); POSF = sb("a_POSF", [128, NT], F32)
    ANG = sb("a_ANG", [128, NT, 32], F32); KI = sb("a_KI", [128, NT, 32], I32); CS = sb("a_CS", [128, NT, 32], F32)
    kb.load("sp", POS[:], D["posT"], [POS])
    kb.cp("dve", POSF[:], POS[:], [POS], [POSF])
    rc = cst.ropec
    kb.tt("dve", ANG[:], fap(POSF[:], [[1, NT], [0, 32]]), fap(rc[:, 0:32], [[0, NT], [1, 32]]), ALU.mult, [POSF, rc], [ANG])
    kb.tt("dve", ANG[:], ANG[:], fap(rc[:, 32:64], [[0, NT], [1, 32]]), ALU.add, [ANG, rc], [ANG])
    A2 = ANG[:].rearrange("p t f -> p (t f)"); C2 = CS[:].rearrange("p t f -> p (t f)"); K2 = KI[:].rearrange("p t f -> p (t f)")
    kb.ts("dve", C2, A2, 1.0 / TWO_PI, None, ALU.mult, None, [ANG], [CS])
    kb.cp("dve", K2, C2, [CS], [KI])
    kb.cp("dve", C2, K2, [KI], [CS])
    kb.stt(A2, C2, -TWO_PI, A2, ALU.mult, ALU.add, [CS, ANG], [ANG])
    kb.ts("dve", C2, A2, float(np.pi), -TWO_PI, ALU.is_gt, ALU.mult, [ANG], [CS])
    kb.tt("dve", A2, A2, C2, ALU.add, [ANG, CS], [ANG])
    kb.ts("dve", C2, A2, -float(np.pi), TWO_PI, ALU.is_lt, ALU.mult, [ANG], [CS])
    kb.tt("dve", A2, A2, C2, ALU.add, [ANG, CS], [ANG])
    kb.act(C2, A2, AF.Sin, [ANG], [CS])

    xbuf = [sb("a_x%d" % i, [128, 1024], F32) for i in range(2)]
    junk = sb("a_junk", [128, 1024], F32)
    ssq = sb("a_ssq", [128, 1], F32); rstd = sb("a_rstd", [128, 1], F32)
    ssq2 = sb("a_ssq2", [128, 1], F32); rstd2 = sb("a_rstd2", [128, 1], F32)
    ssqr = sb("a_ssqr", [128, 1], F32)
    hnb = sb("a_hnb", [128, 1024], BF16); hnT = sb("a_hnT", [128, 8, 128], BF16)
    cqb = sb("a_cqb", [128, 384], BF16); ckb = sb("a_ckb", [128, 256], BF16); cT = sb("a_cT", [128, 5, 128], BF16)
    krr = sb("a_krr", [128, 32], F32); krg = sb("a_krg", [128, 32], F32); krot = sb("a_krot", [128, 32], F32)
    sq = sb("a_sq", [128, 512], F32)
    rq = sb("a_rq", [128, 4], F32); rk = sb("a_rk", [128, 4], F32)
    qn = sb("a_qn", [128, 4, 96], F32); kn = sb("a_kn", [128, 4, 64], F32)
    t1 = sb("a_t1", [128, 4, 16], F32); t2 = sb("a_t2", [128, 4, 16], F32); t3 = sb("a_t3", [128, 4, 16], F32); t4 = sb("a_t4", [128, 4, 16], F32)
    u1 = sb("a_u1", [128, 16], F32); u2 = sb("a_u2", [128, 16], F32); u3 = sb("a_u3", [128, 16], F32); u4 = sb("a_u4", [128, 16], F32)
    qb = sb("a_qb", [128, 4, 128], BF16); kbf = sb("a_kbf", [128, 4, 128], BF16)
    vb = [sb("a_vb%d" % i, [128, 4, 64], BF16) for i in range(2)]
    QTs = [sb("a_QTs%d" % i, [128, 512], BF16) for i in range(2)]
    KTs = [sb("a_KTs%d" % i, [128, 512], BF16) for i in range(2)]
    kb.memset("pool", qb[:], 0.0, [qb]); kb.memset("pool", kbf[:], 0.0, [kbf])
    identb = cst.identb
    QTd = D["QT_d"].rearrange("h d s -> d h s"); KTd = D["KT_d"].rearrange("h d s -> d h s")
    Vd = D["V_d"].rearrange("h p t d -> p h t d")

    def sqrt_recip(t_ap, tt_):
        kb.act(t_ap, t_ap, AF.Sqrt, [tt_], [tt_])
        kb.op("dve", lambda e: e.reciprocal(out=t_ap, in_=t_ap), [tt_], [tt_])

    for t in range(NT):
        xt = xbuf[t % 2]
        kb.load("sp", xt[:], D["x"][t * 128:(t + 1) * 128, :], [xt])
        rms_rstd(kb, xt[:], 1024, junk, ssq, rstd, [xt])
        kb.stt(hnb[:], xt[:], rstd[:, 0:1], Gat[:], ALU.mult, ALU.mult, [xt, rstd, Gat], [hnb])
        pt = psb[0]
        ptb = pt[:].bitcast(BF16)
        for c in range(8):
            kb.tr(ptb[:, c * 128:(c + 1) * 128], hnb[:, c * 128:(c + 1) * 128], identb[:], [hnb, identb], [pt])
        kb.cp("act", hnT[:].rearrange("p c t -> p (c t)"), ptb, [pt], [hnT])
        for c in range(8):
            kb.mm(psb[1][:, 0:384], hnT[:, c, :], Wd[:, c, 0:384], c == 0, c == 7, [hnT, Wd], [psb[1]])
        for c in range(8):
            kb.mm(psb[2][:, 0:288], hnT[:, c, :], Wd[:, c, 384:672], c == 0, c == 7, [hnT, Wd], [psb[2]])
        rms_rstd(kb, psb[1][:, 0:384], 384, junk, ssq, rstd, [psb[1]])
        kb.stt(cqb[:], psb[1][:, 0:384], rstd[:, 0:1], Gq[:], ALU.mult, ALU.mult, [psb[1], rstd, Gq], [cqb])
        rms_rstd(kb, psb[2][:, 0:256], 256, junk, ssq2, rstd2, [psb[2]])
        kb.stt(ckb[:], psb[2][:, 0:256], rstd2[:, 0:1], Gkv[:], ALU.mult, ALU.mult, [psb[2], rstd2, Gkv], [ckb])
        kb.cp("act", krr[:], psb[2][:, 256:288], [psb[2]], [krr])
        for c in range(3):
            kb.tr(ptb[:, c * 128:(c + 1) * 128], cqb[:, c * 128:(c + 1) * 128], identb[:], [cqb, identb], [pt])
        for c in range(2):
            kb.tr(ptb[:, (3 + c) * 128:(4 + c) * 128], ckb[:, c * 128:(c + 1) * 128], identb[:], [ckb, identb], [pt])
        kb.cp("act", cT[:].rearrange("p c t -> p (c t)"), ptb[:, 0:640], [pt], [cT])
        for c in range(3):
            kb.mm(psb[3][:, 0:384], cT[:, c, :], Wuq[:, c, :], c == 0, c == 2, [cT, Wuq], [psb[3]])
        for c in range(2):
            kb.mm(psb[4][:, 0:512], cT[:, 3 + c, :], Wukv[:, c, :], c == 0, c == 1, [cT, Wukv], [psb[4]])
        cosq = fap(CS[:, t, 0:16], [[0, 4], [1, 16]]); sinq = fap(CS[:, t, 16:32], [[0, 4], [1, 16]])
        q3 = psb[3][:, 0:384].rearrange("p (h d) -> p h d", h=4)
        kb.act(sq[:, 0:384], psb[3][:, 0:384], AF.Square, [psb[3]], [sq])
        kb.red("dve", rq[:, 0:4], sq[:, 0:384].rearrange("p (h d) -> p h d", h=4), ALU.add, [sq], [rq])
        kb.ts("dve", rq[:, 0:4], rq[:, 0:4], 1.0 / 96, EPS, ALU.mult, ALU.add, [rq], [rq])
        sqrt_recip(rq[:, 0:4], rq)
        kb.tt("dve", qn[:], q3, fap(rq[:, 0:4], [[1, 4], [0, 96]]), ALU.mult, [psb[3], rq], [qn])
        kb.tt("dve", qn[:], qn[:], fap(Gqn[:], [[0, 4], [1, 96]]), ALU.mult, [qn, Gqn], [qn])
        kb.tt("dve", t1[:], qn[:, :, 64:80], cosq, ALU.mult, [qn, CS], [t1])
        kb.tt("dve", t2[:], qn[:, :, 80:96], sinq, ALU.mult, [qn, CS], [t2])
        kb.tt("dve", t3[:], qn[:, :, 80:96], cosq, ALU.mult, [qn, CS], [t3])
        kb.tt("dve", t4[:], qn[:, :, 64:80], sinq, ALU.mult, [qn, CS], [t4])
        kb.cp("act", qb[:, :, 0:64], qn[:, :, 0:64], [qn], [qb])
        kb.tt("dve", qb[:, :, 64:80], t1[:], t2[:], ALU.subtract, [t1, t2], [qb])
        kb.tt("dve", qb[:, :, 80:96], t3[:], t4[:], ALU.add, [t3, t4], [qb])
        kv3 = psb[4][:, 0:512].rearrange("p (h d) -> p h d", h=4)
        kb.act(sq[:, 0:512], psb[4][:, 0:512], AF.Square, [psb[4]], [sq])
        kb.red("dve", rk[:, 0:4], sq[:, 0:512].rearrange("p (h d) -> p h d", h=4)[:, :, 0:64], ALU.add, [sq], [rk])
        kb.act(junk[:, 0:32], krr[:], AF.Square, [krr], [junk, ssqr], accum=ssqr[:, 0:1])
        kb.ts("dve", rk[:, 0:4], rk[:, 0:4], ssqr[:, 0:1], 1.0 / 96, ALU.add, ALU.mult, [rk, ssqr], [rk])
        kb.ts("dve", rk[:, 0:4], rk[:, 0:4], EPS, None, ALU.add, None, [rk], [rk])
        sqrt_recip(rk[:, 0:4], rk)
        kb.tt("dve", krg[:], krr[:], Gkn[:, 64:96], ALU.mult, [krr, Gkn], [krg])
        cs_, sn_ = CS[:, t, 0:16], CS[:, t, 16:32]
        kb.tt("dve", u1[:], krg[:, 0:16], cs_, ALU.mult, [krg, CS], [u1])
        kb.tt("dve", u2[:], krg[:, 16:32], sn_, ALU.mult, [krg, CS], [u2])
        kb.tt("dve", u3[:], krg[:, 16:32], cs_, ALU.mult, [krg, CS], [u3])
        kb.tt("dve", u4[:], krg[:, 0:16], sn_, ALU.mult, [krg, CS], [u4])
        kb.tt("dve", krot[:, 0:16], u1[:], u2[:], ALU.subtract, [u1, u2], [krot])
        kb.tt("dve", krot[:, 16:32], u3[:], u4[:], ALU.add, [u3, u4], [krot])
        kb.tt("dve", kn[:], kv3[:, :, 0:64], fap(rk[:, 0:4], [[1, 4], [0, 64]]), ALU.mult, [psb[4], rk], [kn])
        kb.tt("dve", kbf[:, :, 0:64], kn[:], fap(Gkn[:, 0:64], [[0, 4], [1, 64]]), ALU.mult, [kn, Gkn], [kbf])
        kb.tt("dve", kbf[:, :, 64:96], fap(krot[:], [[0, 4], [1, 32]]), fap(rk[:, 0:4], [[1, 4], [0, 32]]), ALU.mult, [krot, rk], [kbf])
        vbt = vb[t % 2]
        kb.cp("act", vbt[:], kv3[:, :, 64:128], [psb[4]], [vbt])
        kb.store("sp", Vd[:, :, t, :], vbt[:], [vbt])
        pq = psb[5]
        pqb = pq[:].bitcast(BF16)
        for h in range(4):
            kb.tr(pqb[:, h * 128:(h + 1) * 128], qb[:, h, :], identb[:], [qb, identb], [pq])
        for h in range(4):
            kb.tr(pqb[:, (4 + h) * 128:(5 + h) * 128], kbf[:, h, :], identb[:], [kbf, identb], [pq])
        qs, ks = QTs[t % 2], KTs[t % 2]
        kb.cp("act", qs[:, :], pqb[:, 0:512], [pq], [qs])
        kb.cp("act", ks[:, :], pqb[:, 512:1024], [pq], [ks])
        kb.store("sp", QTd[:, :, t * 128:(t + 1) * 128], qs[0:96, :].rearrange("p (h t) -> p h t", h=4), [qs])
        kb.store("sp", KTd[:, :, t * 128:(t + 1) * 128], ks[0:96, :].rearrange("p (h t) -> p h t", h=4), [ks])

    kb.dma_barrier("sp")
    KT = sb("a_KT", [96, S], BF16)
    VA = sb("a_VA", [128, NT, 128], BF16)
    QTb = [sb("a_QTb%d" % i, [96, 512], BF16) for i in range(2)]
    PT = [sb("a_PT%d" % i, [128, 512], BF16) for i in range(3)]
    osb = sb("a_osb", [128, 512], F32); rl = sb("a_rl", [128, 512], F32)
    oTt = [sb("a_oTt%d" % i, [64, 512], BF16) for i in range(2)]
    nbias = sb("a_nbias", [128, 1], F32)
    kb.memset("dve", nbias[:], -8.0, [nbias])
    kb.memset("pool", VA[:], 1.0, [VA])
    SPS = [psb[0], psb[1]]; OACC = [psb[2], psb[3]]; bcb = psb[4]
    scale = 96 ** -0.5
    ip = 0
    for h in range(4):
        KC = min(2048, S)
        for c in range(S // KC):
            kb.load("sp", KT[0:96, c * KC:(c + 1) * KC], D["KT_d"][h, :, c * KC:(c + 1) * KC], [KT])
        TC = min(32, NT)
        for c in range(NT // TC):
            kb.load("sp", VA[:, c * TC:(c + 1) * TC, 0:64], D["V_d"][h, :, c * TC:(c + 1) * TC, :], [VA])
        for p in range(NS):
            qt = QTb[p % 2]
            kb.load("sp", qt[0:96, :], D["QT_d"][h, :, p * 512:(p + 1) * 512], [qt])
            oacc = OACC[p % 2]
            nk = 4 * (p + 1)
            for ki in range(nk):
                r = ki - 4 * p
                c0 = 128 * r if r > 0 else 0
                sps = SPS[ip % 2]; pT = PT[ip % 3]; ip += 1
                kb.mm(sps[:, c0:512], KT[0:96, ki * 128:(ki + 1) * 128], qt[0:96, c0:512], True, True, [KT, qt], [sps])
                kb.act(pT[:, c0:512], sps[:, c0:512], AF.Exp, [sps, nbias], [pT], scale=scale, bias=nbias[:, 0:1])
                if r >= 0:
                    kb.tt("pool", pT[:, 128 * r:128 * (r + 1)], pT[:, 128 * r:128 * (r + 1)], cst.triub[:], ALU.mult, [pT, cst.triub], [pT])
                kb.mm(oacc[:, c0:512], VA[:, ki, :], pT[:, c0:512], ki == 0, ki == nk - 1, [VA, pT], [oacc])
            kb.cp("act", osb[:, :], oacc[:, :], [oacc], [osb])
            kb.op("dve", lambda e: e.reciprocal(out=rl[64:128, :], in_=osb[64:128, :]), [osb], [rl])
            kb.mm(bcb[0:64, :], cst.onesf[64:65, 0:64], rl[64:65, :], True, True, [cst.onesf, rl], [bcb])
            ot = oTt[p % 2]
            kb.tt("dve", ot[0:64, :], osb[0:64, :], bcb[0:64, :], ALU.mult, [osb, bcb], [ot])
            kb.store("sp", D["oT"][h * 64:(h + 1) * 64, p * 512:(p + 1) * 512], ot[0:64, :], [ot], final=True)


def phase_P(kb, cst, psb, NTOK, D, peer):
    sb = kb.sb
    Wo = sb("p_Wo", [128, 8, 1024], BF16)
    wo = D["w_o"].rearrange("(c p) n -> p c n", p=128)
    for c in range(8):
        kb.load("pool", Wo[:, c, :], wo[:, c, :], [Wo])
    otb = [sb("p_ot%d" % i, [128, 8, 128], BF16) for i in range(2)]
    hb = [sb("p_h%d" % i, [128, 1024], F32) for i in range(2)]
    oT = D["oT"].rearrange("(c p) s -> p c s", p=128)
    for t in range(NTOK // 128):
        ot, h = otb[t % 2], hb[t % 2]
        kb.load("sp", ot[:], oT[:, :, t * 128:(t + 1) * 128], [ot])
        kb.load("sp", h[:], D["resid"][t * 128:(t + 1) * 128, :], [h])
        for half in range(2):
            bank = psb[7 - half]
            for c in range(8):
                kb.mm(bank[:, :], ot[:, c, :], Wo[:, c, half * 512:(half + 1) * 512], c == 0, c == 7, [ot, Wo], [bank])
        kb.tt("dve", h[:, 0:512], h[:, 0:512], psb[7][:, :], ALU.add, [h, psb[7]], [h])
        kb.tt("dve", h[:, 512:1024], h[:, 512:1024], psb[6][:, :], ALU.add, [h, psb[6]], [h])
        peer.tile(h, psb)
        kb.store("sp", D["out"][t * 128:(t + 1) * 128, :], h[:], [h], final=True)


def phase_G(kb, cst, psb, S, D):
    sb = kb.sb
    NT = S // 128
    Win = sb("g_Win", [128, 8, 896], BF16)
    win = D["w_in"].rearrange("(c p) n -> p c n", p=128)
    for c in range(8):
        kb.load("pool", Win[:, c, :], win[:, c, :], [Win])
    Gat = sb("g_Gat", [128, 1024], F32); Gon = sb("g_Gon", [128, 256], F32)
    kb.load("sp", Gat[:], dram_bcast(D["g_attn"], 128), [Gat])
    kb.load("sp", Gon[:], dram_bcast(D["g_on"], 128), [Gon])
    Wg2 = sb("g_Wg2", [128, 128], BF16); bg = sb("g_bg", [128, 128], F32)
    kb.load("pool", Wg2[:], D["w_g2"], [Wg2])
    kb.load("sp", bg[:], dram_bcast(D["b_g"], 128), [bg])
    zb = sb("g_zb", [128, 128], F32)
    xbuf = [sb("g_x%d" % i, [128, 1024], F32) for i in range(2)]
    junk = sb("g_junk", [128, 1024], F32)
    ssq = sb("g_ssq", [128, 1], F32); rstd = sb("g_rstd", [128, 1], F32)
    hnb = sb("g_hnb", [128, 1024], BF16); hnT = sb("g_hnT", [128, 8, 128], BF16)
    glT = sb("g_glT", [128, 128], BF16)
    ez = sb("g_ez", [128, 128], F32); la = sb("g_la", [128, 128], F32)
    cs = sb("g_cs", [128, 128], F32); dd = sb("g_dd", [128, 128], F32)
    epos = sb("g_epos", [128, 128], F32); eneg = sb("g_eneg", [128, 128], F32); erel = sb("g_erel", [128, 128], F32)
    dec = sb("g_dec", [128, 1], F32)
    qd = sb("g_qd", [128, 128], BF16); ki = sb("g_ki", [128, 128], BF16); kd = sb("g_kd", [128, 128], BF16)
    qdT = sb("g_qdT", [128, 128], BF16); kiT = sb("g_kiT", [128, 128], BF16)
    vb = sb("g_vb", [128, 256], BF16)
    at = sb("g_at", [128, 128], BF16)
    St = sb("g_S", [128, 256], F32); Sb = sb("g_Sb", [128, 256], BF16)
    on = sb("g_onb", [128, 256], F32); sr = sb("g_sr", [128, 256], F32); og = sb("g_og", [128, 256], BF16)
    ogT = [sb("g_ogT%d" % i, [128, 2, 512], BF16) for i in range(2)]
    kb.memset("dve", St[:], 0.0, [St]); kb.memset("dve", Sb[:], 0.0, [Sb])
    identb = cst.identb
    oTd = D["oT"].rearrange("(c p) s -> p c s", p=128)
    for t in range(NT):
        xt = xbuf[t % 2]
        kb.load("sp", xt[:], D["x"][t * 128:(t + 1) * 128, :], [xt])
        rms_rstd(kb, xt[:], 1024, junk, ssq, rstd, [xt])
        kb.stt(hnb[:], xt[:], rstd[:, 0:1], Gat[:], ALU.mult, ALU.mult, [xt, rstd, Gat], [hnb])
        pt = psb[0]
        ptb = pt[:].bitcast(BF16)
        for c in range(8):
            kb.tr(ptb[:, c * 128:(c + 1) * 128], hnb[:, c * 128:(c + 1) * 128], identb[:], [hnb, identb], [pt])
        kb.cp("act", hnT[:].rearrange("p c t -> p (c t)"), ptb, [pt], [hnT])
        pA, pB, pC = psb[1], psb[2], psb[3]
        for c in range(8):
            kb.mm(pA[:, 0:512], hnT[:, c, :], Win[:, c, 0:512], c == 0, c == 7, [hnT, Win], [pA])
        for c in range(8):
            kb.mm(pB[:, 0:256], hnT[:, c, :], Win[:, c, 512:768], c == 0, c == 7, [hnT, Win], [pB])
        for c in range(8):
            kb.mm(pC[:, 0:128], Win[:, c, 768:896], hnT[:, c, :], c == 0, c == 7, [hnT, Win], [pC])
        kb.cp("act", glT[:], pC[:, 0:128], [pC], [glT])
        kb.mm(pC[:, 128:256], glT[:], Wg2[:], True, True, [glT, Wg2], [pC])
        kb.tt("dve", zb[:], pC[:, 128:256], bg[:], ALU.add, [pC, bg], [zb])
        kb.act(ez[:], zb[:], AF.Exp, [zb], [ez], scale=-1.0)
        kb.act(la[:], ez[:], AF.Ln, [ez], [la], bias=1.0)
        pD = psb[4]
        kb.mm(pD[:, 0:128], cst.triuf[:], la[:], True, True, [cst.triuf, la], [pD])
        kb.mm(pD[:, 128:256], cst.onesf[:], la[:], True, True, [cst.onesf, la], [pD])
        kb.mm(pD[:, 256:384], la[:], cst.onesf[:], True, True, [cst.onesf, la], [pD])
        kb.cp("dve", cs[:], pD[:, 0:128], [pD], [cs])
        kb.tt("dve", dd[:], pD[:, 128:256], cs[:], ALU.subtract, [pD, cs], [dd])
        kb.act(epos[:], cs[:], AF.Exp, [cs], [epos], scale=-1.0 / 16)
        kb.act(eneg[:], cs[:], AF.Exp, [cs], [eneg], scale=1.0 / 16)
        kb.act(erel[:], dd[:], AF.Exp, [dd], [erel], scale=-1.0 / 16)
        kb.act(dec[:, 0:1], pD[:, 256:257], AF.Exp, [pD], [dec], scale=-1.0 / 16)
        kb.stt(qd[:], pA[:, 0:128], 128 ** -0.5, epos[:], ALU.mult, ALU.mult, [pA, epos], [qd])
        kb.tt("dve", ki[:], pA[:, 128:256], eneg[:], ALU.mult, [pA, eneg], [ki])
        kb.tt("dve", kd[:], pA[:, 128:256], erel[:], ALU.mult, [pA, erel], [kd])
        kb.cp("act", vb[:], pA[:, 256:512], [pA], [vb])
        pE = psb[5]
        peb = pE[:].bitcast(BF16)
        kb.tr(peb[:, 0:128], qd[:], identb[:], [qd, identb], [pE])
        kb.tr(peb[:, 128:256], ki[:], identb[:], [ki, identb], [pE])
        kb.cp("act", qdT[:], peb[:, 0:128], [pE], [qdT])
        kb.cp("act", kiT[:], peb[:, 128:256], [pE], [kiT])
        pF = psb[6]
        kb.mm(pF[:, 0:128], kiT[:], qdT[:], True, True, [kiT, qdT], [pF])
        kb.tt("dve", at[:], pF[:, 0:128], cst.triuf[:], ALU.mult, [pF, cst.triuf], [at])
        kb.mm(pF[:, 256:512], at[:], vb[:], True, False, [at, vb], [pF])
        kb.mm(pF[:, 256:512], qdT[:], Sb[:], False, True, [qdT, Sb], [pF])
        pG = psb[7]
        kb.mm(pG[:, 0:256], kd[:], vb[:], True, True, [kd, vb], [pG])
        kb.stt(St[:], St[:], dec[:, 0:1], pG[:, 0:256], ALU.mult, ALU.add, [St, dec, pG], [St])
        kb.cp("act", Sb[:], St[:], [St], [Sb])
        rms_rstd(kb, pF[:, 256:512], 256, junk, ssq, rstd, [pF])
        kb.stt(on[:], pF[:, 256:512], rstd[:, 0:1], Gon[:], ALU.mult, ALU.mult, [pF, rstd, Gon], [on])
        kb.act(sr[:], pB[:, 0:256], AF.Silu, [pB], [sr])
        kb.tt("dve", og[:], on[:], sr[:], ALU.mult, [on, sr], [og])
        kb.tr(peb[:, 256:384], og[:, 0:128], identb[:], [og, identb], [pE])
        kb.tr(peb[:, 384:512], og[:, 128:256], identb[:], [og, identb], [pE])
        sp_, j = t // 4, t % 4
        ogs = ogT[sp_ % 2]
        kb.cp("act", ogs[:, 0, j * 128:(j + 1) * 128], peb[:, 256:384], [pE], [ogs])
        kb.cp("act", ogs[:, 1, j * 128:(j + 1) * 128], peb[:, 384:512], [pE], [ogs])
        if j == 3 or t == NT - 1:
            w = (j + 1) * 128
            kb.store("sp", oTd[:, :, sp_ * 512:sp_ * 512 + w], ogs[:, :, 0:w], [ogs], final=True)


def _psum_banks(kb):
    return [kb.ps("psb%d" % i, [128, 512], F32) for i in range(8)]


def build_A(S, debug=False):
    nc = bass.Bass("TRN2", target_bir_lowering=False)
    dt_ = lambda n, sh, dt, kind="ExternalInput": nc.dram_tensor(n, sh, dt, kind=kind).ap()
    D = {
        "x": dt_("x", [S, 1024], F32), "posT": dt_("posT", [128, S // 128], I32),
        "g_attn": dt_("g_attn", [1, 1024], F32), "w_down": dt_("w_down", [1024, 672], F32),
        "g_q": dt_("g_q", [1, 384], F32), "w_uq": dt_("w_uq", [384, 384], F32),
        "g_kv": dt_("g_kv", [1, 256], F32), "w_ukv": dt_("w_ukv", [256, 512], F32),
        "g_qn": dt_("g_qn", [1, 96], F32), "g_kn": dt_("g_kn", [1, 96], F32),
        "oT": dt_("oT", [256, S], BF16, "ExternalOutput"),
    }
    kind = "ExternalOutput" if debug else "Internal"
    D["QT_d"] = dt_("QT_d", [4, 96, S], BF16, kind)
    D["KT_d"] = dt_("KT_d", [4, 96, S], BF16, kind)
    D["V_d"] = dt_("V_d", [4, 128, S // 128, 64], BF16, kind)
    c_d = dt_("cst", [128, 336], F32)
    kb = KB(nc)
    cst = Consts(kb, c_d)
    psb = _psum_banks(kb)
    phase_A(kb, cst, psb, S, D)
    kb.finish()
    return nc


def build_P(NTOK):
    nc = bass.Bass("TRN2", target_bir_lowering=False)
    dt_ = lambda n, sh, dt, kind="ExternalInput": nc.dram_tensor(n, sh, dt, kind=kind).ap()
    D = {
        "oT": dt_("oT", [1024, NTOK], BF16), "resid": dt_("resid", [NTOK, 1024], F32), "w_o": dt_("w_o", [1024, 1024], F32),
        "out": dt_("out", [NTOK, 1024], F32, "ExternalOutput"),
    }
    g_d = dt_("g_ffn", [1, 1024], F32); wq_d = dt_("w_query", [1024, 1024], F32); sk_d = dt_("sub_keys", [2, 128, 64], F32)
    u_d = dt_("u_tab", [16384, 1024], F32); v_d = dt_("v_tab", [16384, 1024], F32)
    c_d = dt_("cst", [128, 336], F32)
    kb = KB(nc)
    cst = Consts(kb, c_d)
    psb = _psum_banks(kb)
    peer = Peer(kb, cst)
    peer.load_weights(g_d, wq_d, sk_d, u_d, v_d, psb[7])
    phase_P(kb, cst, psb, NTOK, D, peer)
    kb.finish()
    return nc


def build_G(S):
    nc = bass.Bass("TRN2", target_bir_lowering=False)
    dt_ = lambda n, sh, dt, kind="ExternalInput": nc.dram_tensor(n, sh, dt, kind=kind).ap()
    D = {
        "x": dt_("x", [S, 1024], F32), "g_attn": dt_("g_attn", [1, 1024], F32), "w_in": dt_("w_in", [1024, 896], F32),
        "w_g2": dt_("w_g2", [128, 128], F32), "b_g": dt_("b_g", [1, 128], F32), "g_on": dt_("g_on", [1, 256], F32),
        "oT": dt_("oT", [256, S], BF16, "ExternalOutput"),
    }
    c_d = dt_("cst", [128, 336], F32)
    kb = KB(nc)
    cst = Consts(kb, c_d)
    psb = _psum_banks(kb)
    phase_G(kb, cst, psb, S, D)
    kb.finish()
    return nc


def _c(a):
    return np.ascontiguousarray(a)


def inputs_A(x_b, pos_b, P, g):
    S = x_b.shape[0]
    hs = slice(4 * g, 4 * g + 4)
    w_uq = P["mla_w_uq"][0].reshape(384, 16, 96)[:, hs, :].reshape(384, 384)
    w_ukv = P["mla_w_ukv"][0].reshape(256, 16, 128)[:, hs, :].reshape(256, 512)
    return dict(x=_c(x_b), posT=_c(pos_b.reshape(S // 128, 128).T.astype(np.int32)),
                g_attn=_c(P["attn_norm_g"][0:1]), w_down=_c(P["mla_w_down"][0]), g_q=_c(P["mla_g_q_lat"][0:1]), w_uq=_c(w_uq),
                g_kv=_c(P["mla_g_kv_lat"][0:1]), w_ukv=_c(w_ukv), g_qn=_c(P["mla_g_qn"][0:1]), g_kn=_c(P["mla_g_kn"][0:1]),
                cst=host_consts())


def inputs_P(oT_tok, resid_tok, w_o, P, layer):
    return dict(oT=_c(oT_tok), resid=_c(resid_tok), w_o=_c(w_o), g_ffn=_c(P["ffn_norm_g"][layer:layer + 1]),
                w_query=_c(P["peer_w_query"][layer]), sub_keys=_c(P["peer_sub_keys"][layer]),
                u_tab=_c(P["peer_u"][layer]), v_tab=_c(P["peer_v"][layer]), cst=host_consts())


def inputs_G(h_b, P, hh):
    w = P["gla_w_in"][0]
    w_in = np.concatenate([w[:, 128 * hh:128 * (hh + 1)], w[:, 512 + 128 * hh:512 + 128 * (hh + 1)],
                           w[:, 1024 + 256 * hh:1024 + 256 * (hh + 1)], w[:, 2048 + 256 * hh:2048 + 256 * (hh + 1)], w[:, 3072:3088],
                           np.zeros((1024, 112), np.float32)], axis=1)
    wg2 = np.zeros((128, 128), np.float32)
    wg2[0:16] = P["gla_w_g2"][0][:, 128 * hh:128 * (hh + 1)]
    return dict(x=_c(h_b), g_attn=_c(P["attn_norm_g"][1:2]), w_in=_c(w_in), w_g2=wg2,
                b_g=_c(P["gla_b_g"][0:1, 128 * hh:128 * (hh + 1)]), g_on=_c(P["gla_g_on"][0:1]), cst=host_consts())


def kernel(**inp):
    P = {k: np.asarray(v) for k, v in inp.items()}
    x = P["x"]
    B, S, _ = x.shape
    ncore = 4 * B
    ids = list(range(ncore))
    TOK = S // 4
    ncA = build_A(S)
    ims = [inputs_A(x[b], P["positions"][b], P, g) for b in range(B) for g in range(4)]
    res = run_bass_kernel_spmd(ncA, ims, core_ids=ids)
    oT = [np.concatenate([np.asarray(res.results[b * 4 + g]["oT"]) for g in range(4)], axis=0) for b in range(B)]
    ncP = build_P(TOK)
    ims = [inputs_P(oT[b][:, j * TOK:(j + 1) * TOK], x[b, j * TOK:(j + 1) * TOK], P["mla_w_o"][0], P, 0) for b in range(B) for j in range(4)]
    res = run_bass_kernel_spmd(ncP, ims, core_ids=ids)
    h1 = np.stack([np.concatenate([np.asarray(res.results[b * 4 + j]["out"]) for j in range(4)], axis=0) for b in range(B)])
    ncG = build_G(S)
    ims = [inputs_G(h1[b], P, hh) for b in range(B) for hh in range(4)]
    res = run_bass_kernel_spmd(ncG, ims, core_ids=ids)
    gT = [np.concatenate([np.asarray(res.results[b * 4 + hh]["oT"]) for hh in range(4)], axis=0) for b in range(B)]
    ncP2 = build_P(TOK)
    ims = [inputs_P(gT[b][:, j * TOK:(j + 1) * TOK], h1[b, j * TOK:(j + 1) * TOK], P["gla_w_o"][0], P, 1) for b in range(B) for j in range(4)]
    res = run_bass_kernel_spmd(ncP2, ims, core_ids=ids)
    out = np.stack([np.concatenate([np.asarray(res.results[b * 4 + j]["out"]) for j in range(4)], axis=0) for b in range(B)])
    return out.astype(np.float32)
```

```python
from contextlib import ExitStack
import numpy as np
import ml_dtypes
import concourse.bass as bass
import concourse.mybir as mybir
from concourse.bass_utils import run_bass_kernel_spmd

AF = mybir.ActivationFunctionType
ALU = mybir.AluOpType
AX = mybir.AxisListType
F32, BF16, I32, U32 = mybir.dt.float32, mybir.dt.bfloat16, mybir.dt.int32, mybir.dt.uint32

ENGS = ("pe", "act", "dve", "pool", "sp")
EPS = 1e-6


class T:
    __slots__ = ("h", "w", "r", "name", "psum")

    def __init__(self, h, name="", psum=False):
        self.h = h
        self.w = None
        self.r = {}
        self.name = name
        self.psum = psum

    def __getitem__(self, idx):
        return self.h[idx]


class KB:
    def __init__(self, nc, n_dma_sems=32):
        self.nc = nc
        self.es = ExitStack()
        self.prog = {e: [] for e in ENGS}
        self.sems = {}
        self.cnt = {}
        for e in ENGS:
            self.sems[e] = self.es.enter_context(nc.semaphore("s_" + e))
            self.cnt[e] = 0
        self.dsems = []
        self.dq = {"sp": [], "pool": [], "act": []}
        for q, n in (("sp", n_dma_sems), ("pool", 16)):
            for i in range(n):
                k = "d%s%d" % (q, i)
                self.sems[k] = self.es.enter_context(nc.semaphore("s_" + k))
                self.cnt[k] = 0
                self.dsems.append(k)
                self.dq[q].append(k)
        self.dnext = {"sp": 0, "pool": 0}
        self.seen = {e: {} for e in ENGS}
        self.final = []
        self.pending = {e: [] for e in ENGS}

    def sb(self, name, shape, dt):
        h = self.es.enter_context(self.nc.sbuf_tensor(name, list(shape), dt))
        return T(h, name)

    def ps(self, name, shape, dt):
        h = self.es.enter_context(self.nc.psum_tensor(name, list(shape), dt))
        return T(h, name, psum=True)

    def _waits(self, eng, reads, writes, relaxed=()):
        deps = {}

        def add(k, v):
            if v > deps.get(k, 0):
                deps[k] = v
        for t in reads:
            if t.w is not None:
                k, v = t.w
                if not (k == eng and eng == "pe"):
                    add(k, v)
            if t.psum:
                for k, v in t.r.items():
                    if k != eng:
                        add(k, v)
        for t in writes:
            same_ok = (eng == "pe") or any(t is r for r in relaxed)
            if t.w is not None:
                k, v = t.w
                if k != eng or not same_ok:
                    add(k, v)
            for k, v in t.r.items():
                if k != eng or not same_ok:
                    add(k, v)
        out = []
        seen = self.seen[eng]
        for k, v in deps.items():
            if seen.get(k, 0) >= v:
                continue
            seen[k] = v
            out.append((k, v))
        return out

    def _commit(self, done, reads, writes):
        k, v = done
        for t in writes:
            t.w = done
            t.r = {}
        for t in reads:
            if t.r.get(k, 0) < v:
                t.r[k] = v

    def dma_barrier(self, eng):
        for k in self.dsems:
            v = self.cnt[k]
            if v > 0 and self.seen[eng].get(k, 0) < v:
                self.seen[eng][k] = v
                self.pending[eng].append((k, v))

    def op(self, eng, fn, reads=(), writes=(), relaxed=()):
        waits = self.pending[eng] + self._waits(eng, reads, writes, relaxed)
        self.pending[eng] = []
        self.cnt[eng] += 1
        done = (eng, self.cnt[eng])
        self.prog[eng].append((waits, fn, (eng, 1)))
        self._commit(done, reads, writes)
        return done

    def dma(self, eng, fn, reads=(), writes=(), final=False):
        k = self.dq[eng][self.dnext[eng]]
        self.dnext[eng] = (self.dnext[eng] + 1) % len(self.dq[eng])
        waits = self.pending[eng] + self._waits(eng, reads, writes)
        self.pending[eng] = []
        prev = self.cnt[k]
        if prev > 0 and self.seen[eng].get(k, 0) < prev:
            self.seen[eng][k] = prev
            waits.append((k, prev))
        self.cnt[k] += 16
        done = (k, self.cnt[k])
        self.prog[eng].append((waits, fn, (k, 16)))
        self._commit(done, reads, writes)
        if final:
            self.final.append(done)
        return done

    def finish(self):
        nc = self.nc
        fw = [(k, self.cnt[k]) for k in self.dsems if self.cnt[k] > 0]
        sems = self.sems
        prog = self.prog

        def replay(name):
            def f(eng):
                for waits, fn, inc in prog[name]:
                    for k, v in waits:
                        eng.wait_ge(sems[k], v)
                    ins = fn(eng)
                    ins.then_inc(sems[inc[0]], inc[1])
                if name == "sp":
                    for k, v in fw:
                        eng.wait_ge(sems[k], v)
            return f
        with nc.Block() as block:
            block.tensor(replay("pe"))
            block.scalar(replay("act"))
            block.vector(replay("dve"))
            block.gpsimd(replay("pool"))
            block.sync(replay("sp"))
        self.es.close()

    def mm(self, out, lhsT, rhs, start, stop, reads, writes):
        return self.op("pe", lambda e: e.matmul(out, lhsT=lhsT, rhs=rhs, start=start, stop=stop), reads, writes)

    def tr(self, out, in_, ident, reads, writes):
        return self.op("pe", lambda e: e.transpose(out, in_, ident), reads, writes)

    def act(self, out, in_, func, reads, writes, bias=None, scale=None, accum=None):
        kw = {}
        if bias is not None:
            kw["bias"] = bias
        if scale is not None:
            kw["scale"] = scale
        if accum is not None:
            kw["accum_out"] = accum
        return self.op("act", lambda e: e.activation(out=out, in_=in_, func=func, **kw), reads, writes)

    def cp(self, eng, out, in_, reads, writes):
        if eng == "act":
            return self.op("act", lambda e: e.copy(out=out, in_=in_), reads, writes)
        return self.op(eng, lambda e: e.tensor_copy(out=out, in_=in_), reads, writes)

    def tt(self, eng, out, in0, in1, op, reads, writes):
        return self.op(eng, lambda e: e.tensor_tensor(out=out, in0=in0, in1=in1, op=op), reads, writes)

    def ts(self, eng, out, in0, s1, s2, op0, op1, reads, writes, accum=None):
        if op1 is None:
            return self.op(eng, lambda e: e.tensor_scalar(out=out, in0=in0, scalar1=s1, scalar2=None, op0=op0), reads, writes)
        if accum is not None:
            return self.op(eng, lambda e: e.tensor_scalar(out=out, in0=in0, scalar1=s1, scalar2=s2, op0=op0, op1=op1, accum_out=accum), reads, writes)
        return self.op(eng, lambda e: e.tensor_scalar(out=out, in0=in0, scalar1=s1, scalar2=s2, op0=op0, op1=op1), reads, writes)

    def stt(self, out, in0, scalar, in1, op0, op1, reads, writes, accum=None, relaxed=()):
        if accum is not None:
            return self.op("dve", lambda e: e.scalar_tensor_tensor(out=out, in0=in0, scalar=scalar, in1=in1, op0=op0, op1=op1, accum_out=accum), reads, writes, relaxed)
        return self.op("dve", lambda e: e.scalar_tensor_tensor(out=out, in0=in0, scalar=scalar, in1=in1, op0=op0, op1=op1), reads, writes)

    def red(self, eng, out, in_, op, reads, writes):
        return self.op(eng, lambda e: e.tensor_reduce(out=out, in_=in_, axis=AX.X, op=op), reads, writes)

    def memset(self, eng, ap, val, writes):
        return self.op(eng, lambda e: e.memset(ap, val), (), writes)

    def load(self, eng, out, in_, writes, reads=()):
        return self.dma(eng, lambda e: e.dma_start(out=out, in_=in_), reads, writes)

    def store(self, eng, out, in_, reads, final=False, writes=()):
        return self.dma(eng, lambda e: e.dma_start(out=out, in_=in_), reads, writes, final=final)


def dram_bcast(row, nparts):
    n = row.shape[-1]
    return bass.AP(row.tensor, row.offset, [[0, nparts], [1, n]])


def fap(ap, dims):
    return bass.AP(ap.tensor, ap.offset, [list(ap.ap[0])] + [list(d) for d in dims])


def rms_rstd(kb, x_ap, n, junk, ssq, rstd, reads, eng="act"):
    kb.act(junk[:, 0:n], x_ap, AF.Square, reads, [junk, ssq], accum=ssq[:, 0:1])
    kb.ts("dve", rstd[:, 0:1], ssq[:, 0:1], 1.0 / n, EPS, ALU.mult, ALU.add, [ssq], [rstd])
    kb.act(rstd[:, 0:1], rstd[:, 0:1], AF.Sqrt, [rstd], [rstd])
    kb.op("dve", lambda e: e.reciprocal(out=rstd[:, 0:1], in_=rstd[:, 0:1]), [rstd], [rstd])


class Peer:
    def __init__(self, kb, cst, NG=6):
        self.kb = kb
        self.cst = cst
        sb, ps = kb.sb, kb.ps
        self.G = sb("pe_G", [128, 1024], F32)
        self.Wq = sb("pe_Wq", [128, 8, 1024], BF16)
        self.KBD = sb("pe_KBD", [128, 256], F32)
        self.SK = sb("pe_SK", [128, 128], F32)
        self.junk = sb("pe_junk", [128, 1024], F32)
        self.junk2 = sb("pe_junk2", [128, 1024], F32)
        self.ssq = sb("pe_ssq", [128, 1], F32)
        self.rstd = sb("pe_rstd", [128, 1], F32)
        self.xn = sb("pe_xn", [128, 1024], F32)
        self.xnb = sb("pe_xnb", [128, 1024], BF16)
        self.xnT = sb("pe_xnT", [128, 8, 128], BF16)
        self.qT = sb("pe_qT", [128, 8, 128], F32)
        self.S = sb("pe_S", [128, 16, 128], F32)
        self.S2 = sb("pe_S2", [128, 16, 128], F32)
        self.V1 = sb("pe_V1", [128, 16, 16], F32)
        self.I1 = sb("pe_I1", [128, 16, 16], U32)
        self.I1f = sb("pe_I1f", [128, 16, 16], F32)
        self.CA = sb("pe_CA", [128, 8, 256], F32)
        self.CA2 = sb("pe_CA2", [128, 8, 256], F32)
        self.BV = sb("pe_BV", [128, 8, 16], F32)
        self.BP = sb("pe_BP", [128, 8, 16], U32)
        self.PA = sb("pe_PA", [128, 8, 16], U32)
        self.PB = sb("pe_PB", [128, 8, 16], U32)
        self.PAf = sb("pe_PAf", [128, 8, 16], F32)
        self.PBf = sb("pe_PBf", [128, 8, 16], F32)
        self.OH = sb("pe_OH", [128, 8, 16, 16], F32)
        self.SEL1 = sb("pe_SEL1", [128, 8, 16], F32)
        self.SEL2 = sb("pe_SEL2", [128, 8, 16], F32)
        self.IDXf = sb("pe_IDXf", [128, 128], F32)
        self.IDX = sb("pe_IDX", [128, 128], I32)
        self.GT = sb("pe_GT", [128, 8, 16], F32)
        self.Z = sb("pe_Z", [128, 8], F32)
        self.HD = sb("pe_HD", [128, 128], F32)
        self.W = sb("pe_W", [128, 128], F32)
        self.NG = NG
        self.gb = [sb("pe_gb%d" % i, [128, 1024], F32) for i in range(NG)]
        self.gi = 0

    def load_weights(self, g_ffn, w_query, sub_keys, u_tab, v_tab, ps_tr):
        kb = self.kb
        self.u_tab, self.v_tab = u_tab, v_tab
        kb.load("sp", self.G[:], dram_bcast(g_ffn, 128), [self.G])
        wq = w_query.rearrange("(c p) n -> p c n", p=128)
        for c in range(8):
            kb.load("pool", self.Wq[:, c, :], wq[:, c, :], [self.Wq])
        kb.load("sp", self.SK[:].rearrange("p (c d) -> p c d", c=2), sub_keys.rearrange("c n d -> n c d"), [self.SK])
        kb.tr(ps_tr[:, 0:128], self.SK[:], self.cst.identf[:], [self.SK, self.cst.identf], [ps_tr])
        kb.memset("dve", self.KBD[:], 0.0, [self.KBD])
        kb.cp("dve", self.KBD[0:64, 0:128], ps_tr[0:64, 0:128], [ps_tr], [self.KBD])
        kb.cp("dve", self.KBD[64:128, 128:256], ps_tr[64:128, 0:128], [ps_tr], [self.KBD])

    def tile(self, h, psb):
        kb, c = self.kb, self.cst
        rms_rstd(kb, h[:], 1024, self.junk, self.ssq, self.rstd, [h])
        kb.stt(self.xn[:], h[:], self.rstd[:, 0:1], self.G[:], ALU.mult, ALU.mult, [h, self.rstd, self.G], [self.xn])
        kb.cp("pool", self.xnb[:], self.xn[:], [self.xn], [self.xnb])
        pt = psb[0]
        ptb = pt[:].bitcast(BF16)
        for ch in range(8):
            kb.tr(ptb[:, ch * 128:(ch + 1) * 128], self.xnb[:, ch * 128:(ch + 1) * 128], c.identb[:], [self.xnb, c.identb], [pt])
        kb.cp("act", self.xnT[:].rearrange("p c t -> p (c t)"), ptb, [pt], [self.xnT])
        for hh in range(8):
            bank = psb[1 + hh // 4]
            o = bank[:, (hh % 4) * 128:(hh % 4 + 1) * 128]
            for ch in range(8):
                kb.mm(o, self.Wq[:, ch, hh * 128:(hh + 1) * 128], self.xnT[:, ch, :], ch == 0, ch == 7, [self.Wq, self.xnT], [bank])
        kb.cp("act", self.qT[:, 0:4, :].rearrange("p h t -> p (h t)"), psb[1][:], [psb[1]], [self.qT])
        kb.cp("act", self.qT[:, 4:8, :].rearrange("p h t -> p (h t)"), psb[2][:], [psb[2]], [self.qT])
        for hh in range(8):
            bank = psb[3 + hh // 2]
            o = bank[:, (hh % 2) * 256:(hh % 2 + 1) * 256]
            kb.mm(o, self.qT[:, hh, :], self.KBD[:], True, True, [self.qT, self.KBD], [bank])
        for b4 in range(4):
            kb.cp("act" if b4 % 2 == 0 else "dve", self.S[:, b4 * 4:(b4 + 1) * 4, :].rearrange("p g n -> p (g n)"), psb[3 + b4][:], [psb[3 + b4]], [self.S])
        S, S2, V1, I1 = self.S, self.S2, self.V1, self.I1
        for g in range(16):
            kb.op("dve", (lambda g: lambda e: e.max(out=V1[:, g, 0:8], in_=S[:, g, :]))(g), [S], [V1])
        for g in range(16):
            kb.op("dve", (lambda g: lambda e: e.match_replace(out=S2[:, g, :], in_to_replace=V1[:, g, 0:8], in_values=S[:, g, :], imm_value=-1e30))(g), [S, V1], [S2])
        for g in range(16):
            kb.op("dve", (lambda g: lambda e: e.max(out=V1[:, g, 8:16], in_=S2[:, g, :]))(g), [S2], [V1])
        for g in range(16):
            kb.op("dve", (lambda g: lambda e: e.max_index(out=I1[:, g, 0:8], in_max=V1[:, g, 0:8], in_values=S[:, g, :]))(g), [S, V1], [I1])
            kb.op("dve", (lambda g: lambda e: e.max_index(out=I1[:, g, 8:16], in_max=V1[:, g, 8:16], in_values=S[:, g, :]))(g), [S, V1], [I1])
        kb.cp("dve", self.I1f[:], I1[:], [I1], [self.I1f])
        v1 = V1[:]
        in0 = fap(v1, [[32, 8], [1, 16], [0, 16]])
        in1 = fap(V1[:, 1:2, :], [[32, 8], [0, 16], [1, 16]])
        CA = self.CA
        kb.tt("dve", CA[:].rearrange("p h (a b) -> p h a b", a=16), in0, in1, ALU.add, [V1], [CA])
        CA2, BV, BP = self.CA2, self.BV, self.BP
        for hh in range(8):
            kb.op("dve", (lambda g: lambda e: e.max(out=BV[:, g, 0:8], in_=CA[:, g, :]))(hh), [CA], [BV])
        for hh in range(8):
            kb.op("dve", (lambda g: lambda e: e.match_replace(out=CA2[:, g, :], in_to_replace=BV[:, g, 0:8], in_values=CA[:, g, :], imm_value=-1e30))(hh), [CA, BV], [CA2])
        for hh in range(8):
            kb.op("dve", (lambda g: lambda e: e.max(out=BV[:, g, 8:16], in_=CA2[:, g, :]))(hh), [CA2], [BV])
        for hh in range(8):
            kb.op("dve", (lambda g: lambda e: e.max_index(out=BP[:, g, 0:8], in_max=BV[:, g, 0:8], in_values=CA[:, g, :]))(hh), [CA, BV], [BP])
            kb.op("dve", (lambda g: lambda e: e.max_index(out=BP[:, g, 8:16], in_max=BV[:, g, 8:16], in_values=CA[:, g, :]))(hh), [CA, BV], [BP])
        BPf = self.SEL1
        kb.cp("dve", BPf[:], BP[:], [BP], [BPf])
        OH = self.OH
        kb.tt("dve", OH[:], fap(BPf[:], [[16, 8], [1, 16], [0, 16]]), fap(c.thr16[:], [[0, 8], [0, 16], [1, 16]]), ALU.is_ge, [BPf, c.thr16], [OH])
        kb.red("dve", self.PAf[:], OH[:], ALU.add, [OH], [self.PAf])
        kb.stt(self.PBf[:].rearrange("p h k -> p (h k)"), self.PAf[:].rearrange("p h k -> p (h k)"), -16.0, BPf[:].rearrange("p h k -> p (h k)"),
               ALU.mult, ALU.add, [self.PAf, BPf], [self.PBf])
        OH = self.OH
        io = fap(c.iota16[:], [[0, 8], [0, 16], [1, 16]])
        for (Pf, SEL, off) in ((self.PAf, self.SEL1, 0), (self.PBf, self.SEL2, 16)):
            pfb = fap(Pf[:], [[16, 8], [1, 16], [0, 16]])
            kb.tt("dve", OH[:], pfb, io, ALU.is_equal, [Pf, c.iota16], [OH])
            i1b = fap(self.I1f[:, off // 16:off // 16 + 1, :], [[32, 8], [0, 16], [1, 16]])
            kb.tt("dve", OH[:], OH[:], i1b, ALU.mult, [OH, self.I1f], [OH])
            kb.red("dve", SEL[:], OH[:], ALU.add, [OH], [SEL])
        kb.stt(self.IDXf[:], self.SEL1[:].rearrange("p h k -> p (h k)"), 128.0, self.SEL2[:].rearrange("p h k -> p (h k)"),
               ALU.mult, ALU.add, [self.SEL1, self.SEL2], [self.IDXf])
        kb.cp("dve", self.IDX[:], self.IDXf[:], [self.IDXf], [self.IDX])
        GT, Z = self.GT, self.Z
        kb.tt("dve", GT[:], BV[:], fap(BV[:], [[16, 8], [0, 16]]), ALU.subtract, [BV], [GT])
        kb.act(GT[:], GT[:], AF.Exp, [GT], [GT])
        kb.red("dve", Z[:], GT[:], ALU.add, [GT], [Z])
        kb.op("dve", lambda e: e.reciprocal(out=Z[:], in_=Z[:]), [Z], [Z])
        kb.tt("dve", GT[:], GT[:], fap(Z[:], [[1, 8], [0, 16]]), ALU.mult, [GT, Z], [GT])
        HD, W = self.HD, self.W
        for s in range(128):
            gb = self.gb[self.gi % self.NG]
            self.gi += 1
            kb.dma("pool", (lambda gb, s: lambda e: e.indirect_dma_start(out=gb[:], out_offset=None, in_=self.u_tab,
                   in_offset=bass.IndirectOffsetOnAxis(ap=self.IDX[:, s:s + 1], axis=0)))(gb, s), [self.IDX], [gb])
            jk = self.junk if s % 2 == 0 else self.junk2
            kb.stt(jk[:], gb[:], 1.0, self.xn[:], ALU.mult, ALU.mult, [gb, self.xn], [jk, HD], accum=HD[:, s:s + 1], relaxed=(HD,))
        kb.act(W[:], HD[:], AF.Gelu, [HD], [W])
        kb.tt("dve", W[:], W[:], GT[:].rearrange("p h k -> p (h k)"), ALU.mult, [W, GT], [W])
        for s in range(128):
            gb = self.gb[self.gi % self.NG]
            self.gi += 1
            kb.dma("pool", (lambda gb, s: lambda e: e.indirect_dma_start(out=gb[:], out_offset=None, in_=self.v_tab,
                   in_offset=bass.IndirectOffsetOnAxis(ap=self.IDX[:, s:s + 1], axis=0)))(gb, s), [self.IDX], [gb])
            kb.stt(h[:], gb[:], W[:, s:s + 1], h[:], ALU.mult, ALU.add, [gb, W, h], [h])
        if getattr(self, "dbg", None):
            d = self.dbg
            for nm, t in (("IDX", self.IDX), ("GT", self.GT), ("HD", self.HD), ("V1", self.V1), ("I1", self.I1), ("BV", self.BV),
                          ("BP", self.BP), ("xn", self.xn), ("S", self.S), ("PAf", self.PAf), ("PBf", self.PBf), ("qT", self.qT), ("W", self.W)):
                ap = t[:]
                if len(ap.shape) == 3:
                    ap = ap.rearrange("p a b -> p (a b)")
                kb.store("sp", d[nm], ap, [t], final=True)


class Consts:
    def __init__(self, kb, cst_dram):
        self.identf = kb.sb("c_identf", [128, 128], F32)
        self.identb = kb.sb("c_identb", [128, 128], BF16)
        self.iota16 = kb.sb("c_iota16", [128, 16], F32)
        self.triuf = kb.sb("c_triuf", [128, 128], F32)
        self.triub = kb.sb("c_triub", [128, 128], BF16)
        self.ropec = kb.sb("c_ropec", [128, 64], F32)
        self.onesf = kb.sb("c_onesf", [128, 128], F32)
        self.onesb = kb.sb("c_onesb", [128, 128], BF16)
        kb.load("sp", self.identf[:], cst_dram[:, 0:128], [self.identf])
        kb.load("sp", self.iota16[:], cst_dram[:, 128:144], [self.iota16])
        kb.load("sp", self.triuf[:], cst_dram[:, 144:272], [self.triuf])
        kb.load("sp", self.ropec[:], cst_dram[:, 272:336], [self.ropec])
        kb.cp("dve", self.identb[:], self.identf[:], [self.identf], [self.identb])
        kb.cp("dve", self.triub[:], self.triuf[:], [self.triuf], [self.triub])
        kb.memset("dve", self.onesf[:], 1.0, [self.onesf])
        kb.memset("dve", self.onesb[:], 1.0, [self.onesb])
        self.thr16 = kb.sb("c_thr16", [128, 16], F32)
        kb.ts("dve", self.thr16[:], self.iota16[:], 1.0, 16.0, ALU.add, ALU.mult, [self.iota16], [self.thr16])


def host_consts():
    c = np.zeros((128, 336), np.float32)
    c[:, 0:128] = np.eye(128, dtype=np.float32)
    c[:, 128:144] = np.arange(16, dtype=np.float32)[None, :]
    c[:, 144:272] = np.triu(np.ones((128, 128), np.float32))
    inv = (10000.0 ** (-np.arange(16, dtype=np.float32) / 16)).astype(np.float32)
    c[:, 272:288] = inv[None, :]
    c[:, 288:304] = inv[None, :]
    c[:, 304:320] = np.float32(np.pi / 2)
    c[:, 320:336] = 0.0
    return c


def build_peer_only(ntiles, dbg=False):
    nc = bass.Bass("TRN2", target_bir_lowering=False)
    h_d = nc.dram_tensor("h", [ntiles * 128, 1024], F32, kind="ExternalInput").ap()
    g_d = nc.dram_tensor("g_ffn", [1, 1024], F32, kind="ExternalInput").ap()
    wq_d = nc.dram_tensor("w_query", [1024, 1024], F32, kind="ExternalInput").ap()
    sk_d = nc.dram_tensor("sub_keys", [2, 128, 64], F32, kind="ExternalInput").ap()
    u_d = nc.dram_tensor("u_tab", [16384, 1024], F32, kind="ExternalInput").ap()
    v_d = nc.dram_tensor("v_tab", [16384, 1024], F32, kind="ExternalInput").ap()
    c_d = nc.dram_tensor("cst", [128, 336], F32, kind="ExternalInput").ap()
    o_d = nc.dram_tensor("out", [ntiles * 128, 1024], F32, kind="ExternalOutput").ap()
    kb = KB(nc)
    cst = Consts(kb, c_d)
    psb = [kb.ps("psb%d" % i, [128, 512], F32) for i in range(8)]
    peer = Peer(kb, cst)
    if dbg:
        peer.dbg = {}
        for nm, w, dt in (("IDX", 128, I32), ("GT", 128, F32), ("HD", 128, F32), ("V1", 256, F32), ("I1", 256, U32), ("BV", 128, F32),
                          ("BP", 128, U32), ("xn", 1024, F32), ("S", 2048, F32), ("PAf", 128, F32), ("PBf", 128, F32), ("qT", 1024, F32), ("W", 128, F32)):
            peer.dbg[nm] = nc.dram_tensor("dbg_" + nm, [128, w], dt, kind="ExternalOutput").ap()
    peer.load_weights(g_d[0:1, :], wq_d, sk_d, u_d, v_d, psb[7])
    hb = [kb.sb("hb%d" % i, [128, 1024], F32) for i in range(2)]
    for t in range(ntiles):
        h = hb[t % 2]
        kb.load("sp", h[:], h_d[t * 128:(t + 1) * 128, :], [h])
        peer.tile(h, psb)
        kb.store("sp", o_d[t * 128:(t + 1) * 128, :], h[:], [h], final=True)
    kb.finish()
    return nc


def phase_A(kb, cst, psb, S, D):
    sb = kb.sb
    NT, NS = S // 128, S // 512
    TWO_PI = float(2 * np.pi)
    Wd = sb("a_Wd", [128, 8, 672], BF16); Wuq = sb("a_Wuq", [128, 3, 384], BF16); Wukv = sb("a_Wukv", [128, 2, 512], BF16)
    Gat = sb("a_Gat", [128, 1024], F32); Gq = sb("a_Gq", [128, 384], F32); Gkv = sb("a_Gkv", [128, 256], F32)
    Gqn = sb("a_Gqn", [128, 96], F32); Gkn = sb("a_Gkn", [128, 96], F32)
    wd = D["w_down"].rearrange("(c p) n -> p c n", p=128)
    for c in range(8):
        kb.load("pool", Wd[:, c, :], wd[:, c, :], [Wd])
    wq = D["w_uq"].rearrange("(c p) n -> p c n", p=128)
    for c in range(3):
        kb.load("pool", Wuq[:, c, :], wq[:, c, :], [Wuq])
    wk = D["w_ukv"].rearrange("(c p) n -> p c n", p=128)
    for c in range(2):
        kb.load("pool", Wukv[:, c, :], wk[:, c, :], [Wukv])
    for (g, src) in ((Gat, "g_attn"), (Gq, "g_q"), (Gkv, "g_kv"), (Gqn, "g_qn"), (Gkn, "g_kn")):
        kb.load("sp", g[:], dram_bcast(D[src], 128), [g])
    POS = sb("a_POS", [128, NT], I32); POSF = sb("a_POSF", [128, NT], F32)
    ANG = sb("a_ANG", [128, NT, 32], F32); KI = sb("a_KI", [128, NT, 32], I32); CS = sb("a_CS", [128, NT, 32], F32)
    kb.load("sp", POS[:], D["posT"], [POS])
    kb.cp("dve", POSF[:], POS[:], [POS], [POSF])
    rc = cst.ropec
    kb.tt("dve", ANG[:], fap(POSF[:], [[1, NT], [0, 32]]), fap(rc[:, 0:32], [[0, NT], [1, 32]]), ALU.mult, [POSF, rc], [ANG])
    kb.tt("dve", ANG[:], ANG[:], fap(rc[:, 32:64], [[0, NT], [1, 32]]), ALU.add, [ANG, rc], [ANG])
    A2 = ANG[:].rearrange("p t f -> p (t f)"); C2 = CS[:].rearrange("p t f -> p (t f)"); K2 = KI[:].rearrange("p t f -> p (t f)")
    kb.ts("dve", C2, A2, 1.0 / TWO_PI, None, ALU.mult, None, [ANG], [CS])
    kb.cp("dve", K2, C2, [CS], [KI])
    kb.cp("dve", C2, K2, [KI], [CS])
    kb.stt(A2, C2, -TWO_PI, A2, ALU.mult, ALU.add, [CS, ANG], [ANG])
    kb.ts("dve", C2, A2, float(np.pi), -TWO_PI, ALU.is_gt, ALU.mult, [ANG], [CS])
    kb.tt("dve", A2, A2, C2, ALU.add, [ANG, CS], [ANG])
    kb.ts("dve", C2, A2, -float(np.pi), TWO_PI, ALU.is_lt, ALU.mult, [ANG], [CS])
    kb.tt("dve", A2, A2, C2, ALU.add, [ANG, CS], [ANG])
    kb.act(C2, A2, AF.Sin, [ANG], [CS])

    xbuf = [sb("a_x%d" % i, [128, 1024], F32) for i in range(2)]
    junk = sb("a_junk", [128, 1024], F32)
    ssq = sb("a_ssq", [128, 1], F32); rstd = sb("a_rstd", [128, 1], F32)
    ssq2 = sb("a_ssq2", [128, 1], F32); rstd2 = sb("a_rstd2", [128, 1], F32)
    ssqr = sb("a_ssqr", [128, 1], F32)
    hnb = sb("a_hnb", [128, 1024], BF16); hnT = sb("a_hnT", [128, 8, 128], BF16)
    cqb = sb("a_cqb", [128, 384], BF16); ckb = sb("a_ckb", [128, 256], BF16); cT = sb("a_cT", [128, 5, 128], BF16)
    krr = sb("a_krr", [128, 32], F32); krg = sb("a_krg", [128, 32], F32); krot = sb("a_krot", [128, 32], F32)
    sq = sb("a_sq", [128, 512], F32)
    rq = sb("a_rq", [128, 4], F32); rk = sb("a_rk", [128, 4], F32)
    qn = sb("a_qn", [128, 4, 96], F32); kn = sb("a_kn", [128, 4, 64], F32)
    t1 = sb("a_t1", [128, 4, 16], F32); t2 = sb("a_t2", [128, 4, 16], F32); t3 = sb("a_t3", [128, 4, 16], F32); t4 = sb("a_t4", [128, 4, 16], F32)
    u1 = sb("a_u1", [128, 16], F32); u2 = sb("a_u2", [128, 16], F32); u3 = sb("a_u3", [128, 16], F32); u4 = sb("a_u4", [128, 16], F32)
    qb = sb("a_qb", [128, 4, 128], BF16); kbf = sb("a_kbf", [128, 4, 128], BF16)
    vb = [sb("a_vb%d" % i, [128, 4, 64], BF16) for i in range(2)]
    QTs = [sb("a_QTs%d" % i, [128, 512], BF16) for i in range(2)]
    KTs = [sb("a_KTs%d" % i, [128, 512], BF16) for i in range(2)]
    kb.memset("pool", qb[:], 0.0, [qb]); kb.memset("pool", kbf[:], 0.0, [kbf])
    identb = cst.identb
    QTd = D["QT_d"].rearrange("h d s -> d h s"); KTd = D["KT_d"].rearrange("h d s -> d h s")
    Vd = D["V_d"].rearrange("h p t d -> p h t d")

    def sqrt_recip(t_ap, tt_):
        kb.act(t_ap, t_ap, AF.Sqrt, [tt_], [tt_])
        kb.op("dve", lambda e: e.reciprocal(out=t_ap, in_=t_ap), [tt_], [tt_])

    for t in range(NT):
        xt = xbuf[t % 2]
        kb.load("sp", xt[:], D["x"][t * 128:(t + 1) * 128, :], [xt])
        rms_rstd(kb, xt[:], 1024, junk, ssq, rstd, [xt])
        kb.stt(hnb[:], xt[:], rstd[:, 0:1], Gat[:], ALU.mult, ALU.mult, [xt, rstd, Gat], [hnb])
        pt = psb[0]
        ptb = pt[:].bitcast(BF16)
        for c in range(8):
            kb.tr(ptb[:, c * 128:(c + 1) * 128], hnb[:, c * 128:(c + 1) * 128], identb[:], [hnb, identb], [pt])
        kb.cp("act", hnT[:].rearrange("p c t -> p (c t)"), ptb, [pt], [hnT])
        for c in range(8):
            kb.mm(psb[1][:, 0:384], hnT[:, c, :], Wd[:, c, 0:384], c == 0, c == 7, [hnT, Wd], [psb[1]])
        for c in range(8):
            kb.mm(psb[2][:, 0:288], hnT[:, c, :], Wd[:, c, 384:672], c == 0, c == 7, [hnT, Wd], [psb[2]])
        rms_rstd(kb, psb[1][:, 0:384], 384, junk, ssq, rstd, [psb[1]])
        kb.stt(cqb[:], psb[1][:, 0:384], rstd[:, 0:1], Gq[:], ALU.mult, ALU.mult, [psb[1], rstd, Gq], [cqb])
        rms_rstd(kb, psb[2][:, 0:256], 256, junk, ssq2, rstd2, [psb[2]])
        kb.stt(ckb[:], psb[2][:, 0:256], rstd2[:, 0:1], Gkv[:], ALU.mult, ALU.mult, [psb[2], rstd2, Gkv], [ckb])
        kb.cp("act", krr[:], psb[2][:, 256:288], [psb[2]], [krr])
        for c in range(3):
            kb.tr(ptb[:, c * 128:(c + 1) * 128], cqb[:, c * 128:(c + 1) * 128], identb[:], [cqb, identb], [pt])
        for c in range(2):
            kb.tr(ptb[:, (3 + c) * 128:(4 + c) * 128], ckb[:, c * 128:(c + 1) * 128], identb[:], [ckb, identb], [pt])
        kb.cp("act", cT[:].rearrange("p c t -> p (c t)"), ptb[:, 0:640], [pt], [cT])
        for c in range(3):
            kb.mm(psb[3][:, 0:384], cT[:, c, :], Wuq[:, c, :], c == 0, c == 2, [cT, Wuq], [psb[3]])
        for c in range(2):
            kb.mm(psb[4][:, 0:512], cT[:, 3 + c, :], Wukv[:, c, :], c == 0, c == 1, [cT, Wukv], [psb[4]])
        cosq = fap(CS[:, t, 0:16], [[0, 4], [1, 16]]); sinq = fap(CS[:, t, 16:32], [[0, 4], [1, 16]])
        q3 = psb[3][:, 0:384].rearrange("p (h d) -> p h d", h=4)
        kb.act(sq[:, 0:384], psb[3][:, 0:384], AF.Square, [psb[3]], [sq])
        kb.red("dve", rq[:, 0:4], sq[:, 0:384].rearrange("p (h d) -> p h d", h=4), ALU.add, [sq], [rq])
        kb.ts("dve", rq[:, 0:4], rq[:, 0:4], 1.0 / 96, EPS, ALU.mult, ALU.add, [rq], [rq])
        sqrt_recip(rq[:, 0:4], rq)
        kb.tt("dve", qn[:], q3, fap(rq[:, 0:4], [[1, 4], [0, 96]]), ALU.mult, [psb[3], rq], [qn])
        kb.tt("dve", qn[:], qn[:], fap(Gqn[:], [[0, 4], [1, 96]]), ALU.mult, [qn, Gqn], [qn])
        kb.tt("dve", t1[:], qn[:, :, 64:80], cosq, ALU.mult, [qn, CS], [t1])
        kb.tt("dve", t2[:], qn[:, :, 80:96], sinq, ALU.mult, [qn, CS], [t2])
        kb.tt("dve", t3[:], qn[:, :, 80:96], cosq, ALU.mult, [qn, CS], [t3])
        kb.tt("dve", t4[:], qn[:, :, 64:80], sinq, ALU.mult, [qn, CS], [t4])
        kb.cp("act", qb[:, :, 0:64], qn[:, :, 0:64], [qn], [qb])
        kb.tt("dve", qb[:, :, 64:80], t1[:], t2[:], ALU.subtract, [t1, t2], [qb])
        kb.tt("dve", qb[:, :, 80:96], t3[:], t4[:], ALU.add, [t3, t4], [qb])
        kv3 = psb[4][:, 0:512].rearrange("p (h d) -> p h d", h=4)
        kb.act(sq[:, 0:512], psb[4][:, 0:512], AF.Square, [psb[4]], [sq])
        kb.red("dve", rk[:, 0:4], sq[:, 0:512].rearrange("p (h d) -> p h d", h=4)[:, :, 0:64], ALU.add, [sq], [rk])
        kb.act(junk[:, 0:32], krr[:], AF.Square, [krr], [junk, ssqr], accum=ssqr[:, 0:1])
        kb.ts("dve", rk[:, 0:4], rk[:, 0:4], ssqr[:, 0:1], 1.0 / 96, ALU.add, ALU.mult, [rk, ssqr], [rk])
        kb.ts("dve", rk[:, 0:4], rk[:, 0:4], EPS, None, ALU.add, None, [rk], [rk])
        sqrt_recip(rk[:, 0:4], rk)
        kb.tt("dve", krg[:], krr[:], Gkn[:, 64:96], ALU.mult, [krr, Gkn], [krg])
        cs_, sn_ = CS[:, t, 0:16], CS[:, t, 16:32]
        kb.tt("dve", u1[:], krg[:, 0:16], cs_, ALU.mult, [krg, CS], [u1])
        kb.tt("dve", u2[:], krg[:, 16:32], sn_, ALU.mult, [krg, CS], [u2])
        kb.tt("dve", u3[:], krg[:, 16:32], cs_, ALU.mult, [krg, CS], [u3])
        kb.tt("dve", u4[:], krg[:, 0:16], sn_, ALU.mult, [krg, CS], [u4])
        kb.tt("dve", krot[:, 0:16], u1[:], u2[:], ALU.subtract, [u1, u2], [krot])
        kb.tt("dve", krot[:, 16:32], u3[:], u4[:], ALU.add, [u3, u4], [krot])
        kb.tt("dve", kn[:], kv3[:, :, 0:64], fap(rk[:, 0:4], [[1, 4], [0, 64]]), ALU.mult, [psb[4], rk], [kn])
        kb.tt("dve", kbf[:, :, 0:64], kn[:], fap(Gkn[:, 0:64], [[0, 4], [1, 64]]), ALU.mult, [kn, Gkn], [kbf])
        kb.tt("dve", kbf[:, :, 64:96], fap(krot[:], [[0, 4], [1, 32]]), fap(rk[:, 0:4], [[1, 4], [0, 32]]), ALU.mult, [krot, rk], [kbf])
        vbt = vb[t % 2]
        kb.cp("act", vbt[:], kv3[:, :, 64:128], [psb[4]], [vbt])
        kb.store("sp", Vd[:, :, t, :], vbt[:], [vbt])
        pq = psb[5]
        pqb = pq[:].bitcast(BF16)
        for h in range(4):
            kb.tr(pqb[:, h * 128:(h + 1) * 128], qb[:, h, :], identb[:], [qb, identb], [pq])
        for h in range(4):
            kb.tr(pqb[:, (4 + h) * 128:(5 + h) * 128], kbf[:, h, :], identb[:], [kbf, identb], [pq])
        qs, ks = QTs[t % 2], KTs[t % 2]
        kb.cp("act", qs[:, :], pqb[:, 0:512], [pq], [qs])
        kb.cp("act", ks[:, :], pqb[:, 512:1024], [pq], [ks])
        kb.store("sp", QTd[:, :, t * 128:(t + 1) * 128], qs[0:96, :].rearrange("p (h t) -> p h t", h=4), [qs])
        kb.store("sp", KTd[:, :, t * 128:(t + 1) * 128], ks[0:96, :].rearrange("p (h t) -> p h t", h=4), [ks])

    kb.dma_barrier("sp")
    KT = sb("a_KT", [96, S], BF16)
    VA = sb("a_VA", [128, NT, 128], BF16)
    QTb = [sb("a_QTb%d" % i, [96, 512], BF16) for i in range(2)]
    PT = [sb("a_PT%d" % i, [128, 512], BF16) for i in range(3)]
    osb = sb("a_osb", [128, 512], F32); rl = sb("a_rl", [128, 512], F32)
    oTt = [sb("a_oTt%d" % i, [64, 512], BF16) for i in range(2)]
    nbias = sb("a_nbias", [128, 1], F32)
    kb.memset("dve", nbias[:], -8.0, [nbias])
    kb.memset("pool", VA[:], 1.0, [VA])
    SPS = [psb[0], psb[1]]; OACC = [psb[2], psb[3]]; bcb = psb[4]
    scale = 96 ** -0.5
    ip = 0
    for h in range(4):
        KC = min(2048, S)
        for c in range(S // KC):
            kb.load("sp", KT[0:96, c * KC:(c + 1) * KC], D["KT_d"][h, :, c * KC:(c + 1) * KC], [KT])
        TC = min(32, NT)
        for c in range(NT // TC):
            kb.load("sp", VA[:, c * TC:(c + 1) * TC, 0:64], D["V_d"][h, :, c * TC:(c + 1) * TC, :], [VA])
        for p in range(NS):
            qt = QTb[p % 2]
            kb.load("sp", qt[0:96, :], D["QT_d"][h, :, p * 512:(p + 1) * 512], [qt])
            oacc = OACC[p % 2]
            nk = 4 * (p + 1)
            slots = {}

            def emit_S(ki):
                nonlocal ip
                r = ki - 4 * p
                c0 = 128 * r if r > 0 else 0
                sps = SPS[ip % 2]; pT = PT[ip % 3]; ip += 1
                slots[ki] = (r, c0, sps, pT)
                kb.mm(sps[:, c0:512], KT[0:96, ki * 128:(ki + 1) * 128], qt[0:96, c0:512], True, True, [KT, qt], [sps])

            emit_S(0)
            for ki in range(nk):
                if ki + 1 < nk:
                    emit_S(ki + 1)
                r, c0, sps, pT = slots.pop(ki)
                kb.act(pT[:, c0:512], sps[:, c0:512], AF.Exp, [sps, nbias], [pT], scale=scale, bias=nbias[:, 0:1])
                if r >= 0:
                    kb.tt("pool", pT[:, 128 * r:128 * (r + 1)], pT[:, 128 * r:128 * (r + 1)], cst.triub[:], ALU.mult, [pT, cst.triub], [pT])
                kb.mm(oacc[:, c0:512], VA[:, ki, :], pT[:, c0:512], ki == 0, ki == nk - 1, [VA, pT], [oacc])
            kb.cp("act", osb[:, :], oacc[:, :], [oacc], [osb])
            kb.op("dve", lambda e: e.reciprocal(out=rl[64:128, :], in_=osb[64:128, :]), [osb], [rl])
            kb.mm(bcb[0:64, :], cst.onesf[64:65, 0:64], rl[64:65, :], True, True, [cst.onesf, rl], [bcb])
            ot = oTt[p % 2]
            kb.tt("dve", ot[0:64, :], osb[0:64, :], bcb[0:64, :], ALU.mult, [osb, bcb], [ot])
            kb.store("sp", D["oT"][h * 64:(h + 1) * 64, p * 512:(p + 1) * 512], ot[0:64, :], [ot], final=True)


def phase_P(kb, cst, psb, NTOK, D, peer):
    sb = kb.sb
    Wo = sb("p_Wo", [128, 8, 1024], BF16)
    wo = D["w_o"].rearrange("(c p) n -> p c n", p=128)
    for c in range(8):
        kb.load("pool", Wo[:, c, :], wo[:, c, :], [Wo])
    otb = [sb("p_ot%d" % i, [128, 8, 128], BF16) for i in range(2)]
    hb = [sb("p_h%d" % i, [128, 1024], F32) for i in range(2)]
    oT = D["oT"].rearrange("(c p) s -> p c s", p=128)
    for t in range(NTOK // 128):
        ot, h = otb[t % 2], hb[t % 2]
        kb.load("sp", ot[:], oT[:, :, t * 128:(t + 1) * 128], [ot])
        kb.load("sp", h[:], D["resid"][t * 128:(t + 1) * 128, :], [h])
        for half in range(2):
            bank = psb[7 - half]
            for c in range(8):
                kb.mm(bank[:, :], ot[:, c, :], Wo[:, c, half * 512:(half + 1) * 512], c == 0, c == 7, [ot, Wo], [bank])
        kb.tt("dve", h[:, 0:512], h[:, 0:512], psb[7][:, :], ALU.add, [h, psb[7]], [h])
        kb.tt("dve", h[:, 512:1024], h[:, 512:1024], psb[6][:, :], ALU.add, [h, psb[6]], [h])
        peer.tile(h, psb)
        kb.store("sp", D["out"][t * 128:(t + 1) * 128, :], h[:], [h], final=True)


def phase_G(kb, cst, psb, S, D):
    sb = kb.sb
    NT = S // 128
    Win = sb("g_Win", [128, 8, 896], BF16)
    win = D["w_in"].rearrange("(c p) n -> p c n", p=128)
    for c in range(8):
        kb.load("pool", Win[:, c, :], win[:, c, :], [Win])
    Gat = sb("g_Gat", [128, 1024], F32); Gon = sb("g_Gon", [128, 256], F32)
    kb.load("sp", Gat[:], dram_bcast(D["g_attn"], 128), [Gat])
    kb.load("sp", Gon[:], dram_bcast(D["g_on"], 128), [Gon])
    Wg2 = sb("g_Wg2", [128, 128], BF16); bg = sb("g_bg", [128, 128], F32)
    kb.load("pool", Wg2[:], D["w_g2"], [Wg2])
    kb.load("sp", bg[:], dram_bcast(D["b_g"], 128), [bg])
    zb = sb("g_zb", [128, 128], F32)
    xbuf = [sb("g_x%d" % i, [128, 1024], F32) for i in range(2)]
    junk = sb("g_junk", [128, 1024], F32)
    ssq = sb("g_ssq", [128, 1], F32); rstd = sb("g_rstd", [128, 1], F32)
    hnb = sb("g_hnb", [128, 1024], BF16); hnT = sb("g_hnT", [128, 8, 128], BF16)
    glT = sb("g_glT", [128, 128], BF16)
    ez = sb("g_ez", [128, 128], F32); la = sb("g_la", [128, 128], F32)
    cs = sb("g_cs", [128, 128], F32); dd = sb("g_dd", [128, 128], F32)
    epos = sb("g_epos", [128, 128], F32); eneg = sb("g_eneg", [128, 128], F32); erel = sb("g_erel", [128, 128], F32)
    dec = sb("g_dec", [128, 1], F32)
    qd = sb("g_qd", [128, 128], BF16); ki = sb("g_ki", [128, 128], BF16); kd = sb("g_kd", [128, 128], BF16)
    qdT = sb("g_qdT", [128, 128], BF16); kiT = sb("g_kiT", [128, 128], BF16)
    vb = sb("g_vb", [128, 256], BF16)
    at = sb("g_at", [128, 128], BF16)
    St = sb("g_S", [128, 256], F32); Sb = sb("g_Sb", [128, 256], BF16)
    on = sb("g_onb", [128, 256], F32); sr = sb("g_sr", [128, 256], F32); og = sb("g_og", [128, 256], BF16)
    ogT = [sb("g_ogT%d" % i, [128, 2, 512], BF16) for i in range(2)]
    kb.memset("dve", St[:], 0.0, [St]); kb.memset("dve", Sb[:], 0.0, [Sb])
    identb = cst.identb
    oTd = D["oT"].rearrange("(c p) s -> p c s", p=128)
    for t in range(NT):
        xt = xbuf[t % 2]
        kb.load("sp", xt[:], D["x"][t * 128:(t + 1) * 128, :], [xt])
        rms_rstd(kb, xt[:], 1024, junk, ssq, rstd, [xt])
        kb.stt(hnb[:], xt[:], rstd[:, 0:1], Gat[:], ALU.mult, ALU.mult, [xt, rstd, Gat], [hnb])
        pt = psb[0]
        ptb = pt[:].bitcast(BF16)
        for c in range(8):
            kb.tr(ptb[:, c * 128:(c + 1) * 128], hnb[:, c * 128:(c + 1) * 128], identb[:], [hnb, identb], [pt])
        kb.cp("act", hnT[:].rearrange("p c t -> p (c t)"), ptb, [pt], [hnT])
        pA, pB, pC = psb[1], psb[2], psb[3]
        for c in range(8):
            kb.mm(pA[:, 0:512], hnT[:, c, :], Win[:, c, 0:512], c == 0, c == 7, [hnT, Win], [pA])
        for c in range(8):
            kb.mm(pB[:, 0:256], hnT[:, c, :], Win[:, c, 512:768], c == 0, c == 7, [hnT, Win], [pB])
        for c in range(8):
            kb.mm(pC[:, 0:128], Win[:, c, 768:896], hnT[:, c, :], c == 0, c == 7, [hnT, Win], [pC])
        kb.cp("act", glT[:], pC[:, 0:128], [pC], [glT])
        kb.mm(pC[:, 128:256], glT[:], Wg2[:], True, True, [glT, Wg2], [pC])
        kb.tt("dve", zb[:], pC[:, 128:256], bg[:], ALU.add, [pC, bg], [zb])
        kb.act(ez[:], zb[:], AF.Exp, [zb], [ez], scale=-1.0)
        kb.act(la[:], ez[:], AF.Ln, [ez], [la], bias=1.0)
        pD = psb[4]
        kb.mm(pD[:, 0:128], cst.triuf[:], la[:], True, True, [cst.triuf, la], [pD])
        kb.mm(pD[:, 128:256], cst.onesf[:], la[:], True, True, [cst.onesf, la], [pD])
        kb.mm(pD[:, 256:384], la[:], cst.onesf[:], True, True, [cst.onesf, la], [pD])
        kb.cp("dve", cs[:], pD[:, 0:128], [pD], [cs])
        kb.tt("dve", dd[:], pD[:, 128:256], cs[:], ALU.subtract, [pD, cs], [dd])
        kb.act(epos[:], cs[:], AF.Exp, [cs], [epos], scale=-1.0 / 16)
        kb.act(eneg[:], cs[:], AF.Exp, [cs], [eneg], scale=1.0 / 16)
        kb.act(erel[:], dd[:], AF.Exp, [dd], [erel], scale=-1.0 / 16)
        kb.act(dec[:, 0:1], pD[:, 256:257], AF.Exp, [pD], [dec], scale=-1.0 / 16)
        kb.stt(qd[:], pA[:, 0:128], 128 ** -0.5, epos[:], ALU.mult, ALU.mult, [pA, epos], [qd])
        kb.tt("dve", ki[:], pA[:, 128:256], eneg[:], ALU.mult, [pA, eneg], [ki])
        kb.tt("dve", kd[:], pA[:, 128:256], erel[:], ALU.mult, [pA, erel], [kd])
        kb.cp("act", vb[:], pA[:, 256:512], [pA], [vb])
        pE = psb[5]
        peb = pE[:].bitcast(BF16)
        kb.tr(peb[:, 0:128], qd[:], identb[:], [qd, identb], [pE])
        kb.tr(peb[:, 128:256], ki[:], identb[:], [ki, identb], [pE])
        kb.cp("act", qdT[:], peb[:, 0:128], [pE], [qdT])
        kb.cp("act", kiT[:], peb[:, 128:256], [pE], [kiT])
        pF = psb[6]
        kb.mm(pF[:, 0:128], kiT[:], qdT[:], True, True, [kiT, qdT], [pF])
        kb.tt("dve", at[:], pF[:, 0:128], cst.triuf[:], ALU.mult, [pF, cst.triuf], [at])
        kb.mm(pF[:, 256:512], at[:], vb[:], True, False, [at, vb], [pF])
        kb.mm(pF[:, 256:512], qdT[:], Sb[:], False, True, [qdT, Sb], [pF])
        pG = psb[7]
        kb.mm(pG[:, 0:256], kd[:], vb[:], True, True, [kd, vb], [pG])
        kb.stt(St[:], St[:], dec[:, 0:1], pG[:, 0:256], ALU.mult, ALU.add, [St, dec, pG], [St])
        kb.cp("act", Sb[:], St[:], [St], [Sb])
        rms_rstd(kb, pF[:, 256:512], 256, junk, ssq, rstd, [pF])
        kb.stt(on[:], pF[:, 256:512], rstd[:, 0:1], Gon[:], ALU.mult, ALU.mult, [pF, rstd, Gon], [on])
        kb.act(sr[:], pB[:, 0:256], AF.Silu, [pB], [sr])
        kb.tt("dve", og[:], on[:], sr[:], ALU.mult, [on, sr], [og])
        kb.tr(peb[:, 256:384], og[:, 0:128], identb[:], [og, identb], [pE])
        kb.tr(peb[:, 384:512], og[:, 128:256], identb[:], [og, identb], [pE])
        sp_, j = t // 4, t % 4
        ogs = ogT[sp_ % 2]
        kb.cp("act", ogs[:, 0, j * 128:(j + 1) * 128], peb[:, 256:384], [pE], [ogs])
        kb.cp("act", ogs[:, 1, j * 128:(j + 1) * 128], peb[:, 384:512], [pE], [ogs])
        if j == 3 or t == NT - 1:
            w = (j + 1) * 128
            kb.store("sp", oTd[:, :, sp_ * 512:sp_ * 512 + w], ogs[:, :, 0:w], [ogs], final=True)


def _psum_banks(kb):
    return [kb.ps("psb%d" % i, [128, 512], F32) for i in range(8)]


def build_A(S, debug=False):
    nc = bass.Bass("TRN2", target_bir_lowering=False)
    dt_ = lambda n, sh, dt, kind="ExternalInput": nc.dram_tensor(n, sh, dt, kind=kind).ap()
    D = {
        "x": dt_("x", [S, 1024], F32), "posT": dt_("posT", [128, S // 128], I32),
        "g_attn": dt_("g_attn", [1, 1024], F32), "w_down": dt_("w_down", [1024, 672], F32),
        "g_q": dt_("g_q", [1, 384], F32), "w_uq": dt_("w_uq", [384, 384], F32),
        "g_kv": dt_("g_kv", [1, 256], F32), "w_ukv": dt_("w_ukv", [256, 512], F32),
        "g_qn": dt_("g_qn", [1, 96], F32), "g_kn": dt_("g_kn", [1, 96], F32),
        "oT": dt_("oT", [256, S], BF16, "ExternalOutput"),
    }
    kind = "ExternalOutput" if debug else "Internal"
    D["QT_d"] = dt_("QT_d", [4, 96, S], BF16, kind)
    D["KT_d"] = dt_("KT_d", [4, 96, S], BF16, kind)
    D["V_d"] = dt_("V_d", [4, 128, S // 128, 64], BF16, kind)
    c_d = dt_("cst", [128, 336], F32)
    kb = KB(nc)
    cst = Consts(kb, c_d)
    psb = _psum_banks(kb)
    phase_A(kb, cst, psb, S, D)
    kb.finish()
    return nc


def build_P(NTOK):
    nc = bass.Bass("TRN2", target_bir_lowering=False)
    dt_ = lambda n, sh, dt, kind="ExternalInput": nc.dram_tensor(n, sh, dt, kind=kind).ap()
    D = {
        "oT": dt_("oT", [1024, NTOK], BF16), "resid": dt_("resid", [NTOK, 1024], F32), "w_o": dt_("w_o", [1024, 1024], F32),
        "out": dt_("out", [NTOK, 1024], F32, "ExternalOutput"),
    }
    g_d = dt_("g_ffn", [1, 1024], F32); wq_d = dt_("w_query", [1024, 1024], F32); sk_d = dt_("sub_keys", [2, 128, 64], F32)
    u_d = dt_("u_tab", [16384, 1024], F32); v_d = dt_("v_tab", [16384, 1024], F32)
    c_d = dt_("cst", [128, 336], F32)
    kb = KB(nc)
    cst = Consts(kb, c_d)
    psb = _psum_banks(kb)
    peer = Peer(kb, cst)
    peer.load_weights(g_d, wq_d, sk_d, u_d, v_d, psb[7])
    phase_P(kb, cst, psb, NTOK, D, peer)
    kb.finish()
    return nc


def build_G(S):
    nc = bass.Bass("TRN2", target_bir_lowering=False)
    dt_ = lambda n, sh, dt, kind="ExternalInput": nc.dram_tensor(n, sh, dt, kind=kind).ap()
    D = {
        "x": dt_("x", [S, 1024], F32), "g_attn": dt_("g_attn", [1, 1024], F32), "w_in": dt_("w_in", [1024, 896], F32),
        "w_g2": dt_("w_g2", [128, 128], F32), "b_g": dt_("b_g", [1, 128], F32), "g_on": dt_("g_on", [1, 256], F32),
        "oT": dt_("oT", [256, S], BF16, "ExternalOutput"),
    }
    c_d = dt_("cst", [128, 336], F32)
    kb = KB(nc)
    cst = Consts(kb, c_d)
    psb = _psum_banks(kb)
    phase_G(kb, cst, psb, S, D)
    kb.finish()
    return nc


def _c(a):
    return np.ascontiguousarray(a)


def inputs_A(x_b, pos_b, P, g):
    S = x_b.shape[0]
    hs = slice(4 * g, 4 * g + 4)
    w_uq = P["mla_w_uq"][0].reshape(384, 16, 96)[:, hs, :].reshape(384, 384)
    w_ukv = P["mla_w_ukv"][0].reshape(256, 16, 128)[:, hs, :].reshape(256, 512)
    return dict(x=_c(x_b), posT=_c(pos_b.reshape(S // 128, 128).T.astype(np.int32)),
                g_attn=_c(P["attn_norm_g"][0:1]), w_down=_c(P["mla_w_down"][0]), g_q=_c(P["mla_g_q_lat"][0:1]), w_uq=_c(w_uq),
                g_kv=_c(P["mla_g_kv_lat"][0:1]), w_ukv=_c(w_ukv), g_qn=_c(P["mla_g_qn"][0:1]), g_kn=_c(P["mla_g_kn"][0:1]),
                cst=host_consts())


def inputs_P(oT_tok, resid_tok, w_o, P, layer):
    return dict(oT=_c(oT_tok), resid=_c(resid_tok), w_o=_c(w_o), g_ffn=_c(P["ffn_norm_g"][layer:layer + 1]),
                w_query=_c(P["peer_w_query"][layer]), sub_keys=_c(P["peer_sub_keys"][layer]),
                u_tab=_c(P["peer_u"][layer]), v_tab=_c(P["peer_v"][layer]), cst=host_consts())


def inputs_G(h_b, P, hh):
    w = P["gla_w_in"][0]
    w_in = np.concatenate([w[:, 128 * hh:128 * (hh + 1)], w[:, 512 + 128 * hh:512 + 128 * (hh + 1)],
                           w[:, 1024 + 256 * hh:1024 + 256 * (hh + 1)], w[:, 2048 + 256 * hh:2048 + 256 * (hh + 1)], w[:, 3072:3088],
                           np.zeros((1024, 112), np.float32)], axis=1)
    wg2 = np.zeros((128, 128), np.float32)
    wg2[0:16] = P["gla_w_g2"][0][:, 128 * hh:128 * (hh + 1)]
    return dict(x=_c(h_b), g_attn=_c(P["attn_norm_g"][1:2]), w_in=_c(w_in), w_g2=wg2,
                b_g=_c(P["gla_b_g"][0:1, 128 * hh:128 * (hh + 1)]), g_on=_c(P["gla_g_on"][0:1]), cst=host_consts())


def kernel(**inp):
    P = {k: np.asarray(v) for k, v in inp.items()}
    x = P["x"]
    B, S, _ = x.shape
    ncore = 4 * B
    ids = list(range(ncore))
    TOK = S // 4
    ncA = build_A(S)
    ims = [inputs_A(x[b], P["positions"][b], P, g) for b in range(B) for g in range(4)]
    res = run_bass_kernel_spmd(ncA, ims, core_ids=ids)
    oT = [np.concatenate([np.asarray(res.results[b * 4 + g]["oT"]) for g in range(4)], axis=0) for b in range(B)]
    ncP = build_P(TOK)
    ims = [inputs_P(oT[b][:, j * TOK:(j + 1) * TOK], x[b, j * TOK:(j + 1) * TOK], P["mla_w_o"][0], P, 0) for b in range(B) for j in range(4)]
    res = run_bass_kernel_spmd(ncP, ims, core_ids=ids)
    h1 = np.stack([np.concatenate([np.asarray(res.results[b * 4 + j]["out"]) for j in range(4)], axis=0) for b in range(B)])
    ncG = build_G(S)
    ims = [inputs_G(h1[b], P, hh) for b in range(B) for hh in range(4)]
    res = run_bass_kernel_spmd(ncG, ims, core_ids=ids)
    gT = [np.concatenate([np.asarray(res.results[b * 4 + hh]["oT"]) for hh in range(4)], axis=0) for b in range(B)]
    ncP2 = build_P(TOK)
    ims = [inputs_P(gT[b][:, j * TOK:(j + 1) * TOK], h1[b, j * TOK:(j + 1) * TOK], P["gla_w_o"][0], P, 1) for b in range(B) for j in range(4)]
    res = run_bass_kernel_spmd(ncP2, ims, core_ids=ids)
    out = np.stack([np.concatenate([np.asarray(res.results[b * 4 + j]["out"]) for j in range(4)], axis=0) for b in range(B)])
    return out.astype(np.float32)
```

```python
from contextlib import ExitStack
import numpy as np
import ml_dtypes
import concourse.bass as bass
import concourse.mybir as mybir
from concourse.bass_utils import run_bass_kernel_spmd

AF = mybir.ActivationFunctionType
ALU = mybir.AluOpType
AX = mybir.AxisListType
F32, BF16, I32, U32 = mybir.dt.float32, mybir.dt.bfloat16, mybir.dt.int32, mybir.dt.uint32

ENGS = ("pe", "act", "dve", "pool", "sp")
EPS = 1e-6


class T:
    __slots__ = ("h", "w", "r", "name", "psum")

    def __init__(self, h, name="", psum=False):
        self.h = h
        self.w = None
        self.r = {}
        self.name = name
        self.psum = psum

    def __getitem__(self, idx):
        return self.h[idx]


class KB:
    def __init__(self, nc, n_dma_sems=32):
        self.nc = nc
        self.es = ExitStack()
        self.prog = {e: [] for e in ENGS}
        self.sems = {}
        self.cnt = {}
        for e in ENGS:
            self.sems[e] = self.es.enter_context(nc.semaphore("s_" + e))
            self.cnt[e] = 0
        self.dsems = []
        self.dq = {"sp": [], "pool": [], "act": []}
        for q, n in (("sp", n_dma_sems), ("pool", 16)):
            for i in range(n):
                k = "d%s%d" % (q, i)
                self.sems[k] = self.es.enter_context(nc.semaphore("s_" + k))
                self.cnt[k] = 0
                self.dsems.append(k)
                self.dq[q].append(k)
        self.dnext = {"sp": 0, "pool": 0}
        self.seen = {e: {} for e in ENGS}
        self.final = []
        self.pending = {e: [] for e in ENGS}

    def sb(self, name, shape, dt):
        h = self.es.enter_context(self.nc.sbuf_tensor(name, list(shape), dt))
        return T(h, name)

    def ps(self, name, shape, dt):
        h = self.es.enter_context(self.nc.psum_tensor(name, list(shape), dt))
        return T(h, name, psum=True)

    def _waits(self, eng, reads, writes, relaxed=()):
        deps = {}

        def add(k, v):
            if v > deps.get(k, 0):
                deps[k] = v
        for t in reads:
            if t.w is not None:
                k, v = t.w
                if not (k == eng and eng == "pe"):
                    add(k, v)
            if t.psum:
                for k, v in t.r.items():
                    if k != eng:
                        add(k, v)
        for t in writes:
            same_ok = (eng == "pe") or any(t is r for r in relaxed)
            if t.w is not None:
                k, v = t.w
                if k != eng or not same_ok:
                    add(k, v)
            for k, v in t.r.items():
                if k != eng or not same_ok:
                    add(k, v)
        out = []
        seen = self.seen[eng]
        for k, v in deps.items():
            if seen.get(k, 0) >= v:
                continue
            seen[k] = v
            out.append((k, v))
        return out

    def _commit(self, done, reads, writes):
        k, v = done
        for t in writes:
            t.w = done
            t.r = {}
        for t in reads:
            if t.r.get(k, 0) < v:
                t.r[k] = v

    def dma_barrier(self, eng):
        for k in self.dsems:
            v = self.cnt[k]
            if v > 0 and self.seen[eng].get(k, 0) < v:
                self.seen[eng][k] = v
                self.pending[eng].append((k, v))

    def op(self, eng, fn, reads=(), writes=(), relaxed=()):
        waits = self.pending[eng] + self._waits(eng, reads, writes, relaxed)
        self.pending[eng] = []
        self.cnt[eng] += 1
        done = (eng, self.cnt[eng])
        self.prog[eng].append((waits, fn, (eng, 1)))
        self._commit(done, reads, writes)
        return done

    def dma(self, eng, fn, reads=(), writes=(), final=False):
        k = self.dq[eng][self.dnext[eng]]
        self.dnext[eng] = (self.dnext[eng] + 1) % len(self.dq[eng])
        waits = self.pending[eng] + self._waits(eng, reads, writes)
        self.pending[eng] = []
        prev = self.cnt[k]
        if prev > 0 and self.seen[eng].get(k, 0) < prev:
            self.seen[eng][k] = prev
            waits.append((k, prev))
        self.cnt[k] += 16
        done = (k, self.cnt[k])
        self.prog[eng].append((waits, fn, (k, 16)))
        self._commit(done, reads, writes)
        if final:
            self.final.append(done)
        return done

    def finish(self):
        nc = self.nc
        fw = [(k, self.cnt[k]) for k in self.dsems if self.cnt[k] > 0]
        sems = self.sems
        prog = self.prog

        def replay(name):
            def f(eng):
                for waits, fn, inc in prog[name]:
                    for k, v in waits:
                        eng.wait_ge(sems[k], v)
                    ins = fn(eng)
                    ins.then_inc(sems[inc[0]], inc[1])
                if name == "sp":
                    for k, v in fw:
                        eng.wait_ge(sems[k], v)
            return f
        with nc.Block() as block:
            block.tensor(replay("pe"))
            block.scalar(replay("act"))
            block.vector(replay("dve"))
            block.gpsimd(replay("pool"))
            block.sync(replay("sp"))
        self.es.close()

    def mm(self, out, lhsT, rhs, start, stop, reads, writes):
        return self.op("pe", lambda e: e.matmul(out, lhsT=lhsT, rhs=rhs, start=start, stop=stop), reads, writes)

    def tr(self, out, in_, ident, reads, writes):
        return self.op("pe", lambda e: e.transpose(out, in_, ident), reads, writes)

    def act(self, out, in_, func, reads, writes, bias=None, scale=None, accum=None):
        kw = {}
        if bias is not None:
            kw["bias"] = bias
        if scale is not None:
            kw["scale"] = scale
        if accum is not None:
            kw["accum_out"] = accum
        return self.op("act", lambda e: e.activation(out=out, in_=in_, func=func, **kw), reads, writes)

    def cp(self, eng, out, in_, reads, writes):
        if eng == "act":
            return self.op("act", lambda e: e.copy(out=out, in_=in_), reads, writes)
        return self.op(eng, lambda e: e.tensor_copy(out=out, in_=in_), reads, writes)

    def tt(self, eng, out, in0, in1, op, reads, writes):
        return self.op(eng, lambda e: e.tensor_tensor(out=out, in0=in0, in1=in1, op=op), reads, writes)

    def ts(self, eng, out, in0, s1, s2, op0, op1, reads, writes, accum=None):
        if op1 is None:
            return self.op(eng, lambda e: e.tensor_scalar(out=out, in0=in0, scalar1=s1, scalar2=None, op0=op0), reads, writes)
        if accum is not None:
            return self.op(eng, lambda e: e.tensor_scalar(out=out, in0=in0, scalar1=s1, scalar2=s2, op0=op0, op1=op1, accum_out=accum), reads, writes)
        return self.op(eng, lambda e: e.tensor_scalar(out=out, in0=in0, scalar1=s1, scalar2=s2, op0=op0, op1=op1), reads, writes)

    def stt(self, out, in0, scalar, in1, op0, op1, reads, writes, accum=None, relaxed=()):
        if accum is not None:
            return self.op("dve", lambda e: e.scalar_tensor_tensor(out=out, in0=in0, scalar=scalar, in1=in1, op0=op0, op1=op1, accum_out=accum), reads, writes, relaxed)
        return self.op("dve", lambda e: e.scalar_tensor_tensor(out=out, in0=in0, scalar=scalar, in1=in1, op0=op0, op1=op1), reads, writes)

    def red(self, eng, out, in_, op, reads, writes):
        return self.op(eng, lambda e: e.tensor_reduce(out=out, in_=in_, axis=AX.X, op=op), reads, writes)

    def memset(self, eng, ap, val, writes):
        return self.op(eng, lambda e: e.memset(ap, val), (), writes)

    def load(self, eng, out, in_, writes, reads=()):
        return self.dma(eng, lambda e: e.dma_start(out=out, in_=in_), reads, writes)

    def store(self, eng, out, in_, reads, final=False, writes=()):
        return self.dma(eng, lambda e: e.dma_start(out=out, in_=in_), reads, writes, final=final)


def dram_bcast(row, nparts):
    n = row.shape[-1]
    return bass.AP(row.tensor, row.offset, [[0, nparts], [1, n]])


def fap(ap, dims):
    return bass.AP(ap.tensor, ap.offset, [list(ap.ap[0])] + [list(d) for d in dims])


def rms_rstd(kb, x_ap, n, junk, ssq, rstd, reads, eng="act"):
    kb.act(junk[:, 0:n], x_ap, AF.Square, reads, [junk, ssq], accum=ssq[:, 0:1])
    kb.ts("dve", rstd[:, 0:1], ssq[:, 0:1], 1.0 / n, EPS, ALU.mult, ALU.add, [ssq], [rstd])
    kb.act(rstd[:, 0:1], rstd[:, 0:1], AF.Sqrt, [rstd], [rstd])
    kb.op("dve", lambda e: e.reciprocal(out=rstd[:, 0:1], in_=rstd[:, 0:1]), [rstd], [rstd])


class Peer:
    def __init__(self, kb, cst, NG=6):
        self.kb = kb
        self.cst = cst
        sb, ps = kb.sb, kb.ps
        self.G = sb("pe_G", [128, 1024], F32)
        self.Wq = sb("pe_Wq", [128, 8, 1024], BF16)
        self.KBD = sb("pe_KBD", [128, 256], F32)
        self.SK = sb("pe_SK", [128, 128], F32)
        self.junk = sb("pe_junk", [128, 1024], F32)
        self.junk2 = sb("pe_junk2", [128, 1024], F32)
        self.ssq = sb("pe_ssq", [128, 1], F32)
        self.rstd = sb("pe_rstd", [128, 1], F32)
        self.xn = sb("pe_xn", [128, 1024], F32)
        self.xnb = sb("pe_xnb", [128, 1024], BF16)
        self.xnT = sb("pe_xnT", [128, 8, 128], BF16)
        self.qT = sb("pe_qT", [128, 8, 128], F32)
        self.S = sb("pe_S", [128, 16, 128], F32)
        self.S2 = sb("pe_S2", [128, 16, 128], F32)
        self.V1 = sb("pe_V1", [128, 16, 16], F32)
        self.I1 = sb("pe_I1", [128, 16, 16], U32)
        self.I1f = sb("pe_I1f", [128, 16, 16], F32)
        self.CA = sb("pe_CA", [128, 8, 256], F32)
        self.CA2 = sb("pe_CA2", [128, 8, 256], F32)
        self.BV = sb("pe_BV", [128, 8, 16], F32)
        self.BP = sb("pe_BP", [128, 8, 16], U32)
        self.PA = sb("pe_PA", [128, 8, 16], U32)
        self.PB = sb("pe_PB", [128, 8, 16], U32)
        self.PAf = sb("pe_PAf", [128, 8, 16], F32)
        self.PBf = sb("pe_PBf", [128, 8, 16], F32)
        self.OH = sb("pe_OH", [128, 8, 16, 16], F32)
        self.SEL1 = sb("pe_SEL1", [128, 8, 16], F32)
        self.SEL2 = sb("pe_SEL2", [128, 8, 16], F32)
        self.IDXf = sb("pe_IDXf", [128, 128], F32)
        self.IDX = sb("pe_IDX", [128, 128], I32)
        self.GT = sb("pe_GT", [128, 8, 16], F32)
        self.Z = sb("pe_Z", [128, 8], F32)
        self.HD = sb("pe_HD", [128, 128], F32)
        self.W = sb("pe_W", [128, 128], F32)
        self.NG = NG
        self.NG = NG = 12
        self.gb = [sb("pe_gb%d" % i, [128, 1024], BF16) for i in range(NG)]
        self.gi = 0
        self.DG = [sb("pe_DG%d" % i, [128, 128], BF16) for i in range(4)]
        self.stg = [sb("pe_stg%d" % i, [128, 8, 1024], BF16) for i in range(2)]

    def load_weights(self, g_ffn, w_query, sub_keys, u_tab, v_tab, ps_tr):
        kb = self.kb
        nc = kb.nc
        self.u_tab = nc.dram_tensor("pe_u_bf", [16384, 1024], BF16, kind="Internal").ap()
        self.v_tab = nc.dram_tensor("pe_v_bf", [16384, 1024], BF16, kind="Internal").ap()
        ci = 0
        for src, dst in ((u_tab, self.u_tab), (v_tab, self.v_tab)):
            sv = src.rearrange("(c p r) d -> c p r d", p=128, r=8)
            dv = dst.rearrange("(c p r) d -> c p r d", p=128, r=8)
            for c in range(16):
                st = self.stg[ci % 2]
                ci += 1
                kb.dma("pool", (lambda st, a: lambda e: e.dma_start(out=st[:], in_=a, max_dma_last_dim=4096))(st, sv[c]), (), [st])
                kb.store("sp", dv[c], st[:], [st])
        kb.dma_barrier("pool")
        kb.load("sp", self.G[:], dram_bcast(g_ffn, 128), [self.G])
        wq = w_query.rearrange("(c p) n -> p c n", p=128)
        for c in range(8):
            kb.load("pool", self.Wq[:, c, :], wq[:, c, :], [self.Wq])
        kb.load("sp", self.SK[:].rearrange("p (c d) -> p c d", c=2), sub_keys.rearrange("c n d -> n c d"), [self.SK])
        kb.tr(ps_tr[:, 0:128], self.SK[:], self.cst.identf[:], [self.SK, self.cst.identf], [ps_tr])
        kb.memset("dve", self.KBD[:], 0.0, [self.KBD])
        kb.cp("dve", self.KBD[0:64, 0:128], ps_tr[0:64, 0:128], [ps_tr], [self.KBD])
        kb.cp("dve", self.KBD[64:128, 128:256], ps_tr[64:128, 0:128], [ps_tr], [self.KBD])

    def tile(self, h, psb):
        kb, c = self.kb, self.cst
        rms_rstd(kb, h[:], 1024, self.junk, self.ssq, self.rstd, [h])
        kb.stt(self.xn[:], h[:], self.rstd[:, 0:1], self.G[:], ALU.mult, ALU.mult, [h, self.rstd, self.G], [self.xn])
        kb.cp("pool", self.xnb[:], self.xn[:], [self.xn], [self.xnb])
        pt = psb[0]
        ptb = pt[:].bitcast(BF16)
        for ch in range(8):
            kb.tr(ptb[:, ch * 128:(ch + 1) * 128], self.xnb[:, ch * 128:(ch + 1) * 128], c.identb[:], [self.xnb, c.identb], [pt])
        kb.cp("act", self.xnT[:].rearrange("p c t -> p (c t)"), ptb, [pt], [self.xnT])
        for hh in range(8):
            bank = psb[1 + hh // 4]
            o = bank[:, (hh % 4) * 128:(hh % 4 + 1) * 128]
            for ch in range(8):
                kb.mm(o, self.Wq[:, ch, hh * 128:(hh + 1) * 128], self.xnT[:, ch, :], ch == 0, ch == 7, [self.Wq, self.xnT], [bank])
        kb.cp("act", self.qT[:, 0:4, :].rearrange("p h t -> p (h t)"), psb[1][:], [psb[1]], [self.qT])
        kb.cp("act", self.qT[:, 4:8, :].rearrange("p h t -> p (h t)"), psb[2][:], [psb[2]], [self.qT])
        for hh in range(8):
            bank = psb[3 + hh // 2]
            o = bank[:, (hh % 2) * 256:(hh % 2 + 1) * 256]
            kb.mm(o, self.qT[:, hh, :], self.KBD[:], True, True, [self.qT, self.KBD], [bank])
        for b4 in range(4):
            kb.cp("act" if b4 % 2 == 0 else "dve", self.S[:, b4 * 4:(b4 + 1) * 4, :].rearrange("p g n -> p (g n)"), psb[3 + b4][:], [psb[3 + b4]], [self.S])
        S, S2, V1, I1 = self.S, self.S2, self.V1, self.I1
        for g in range(16):
            kb.op("dve", (lambda g: lambda e: e.max(out=V1[:, g, 0:8], in_=S[:, g, :]))(g), [S], [V1])
        for g in range(16):
            kb.op("dve", (lambda g: lambda e: e.match_replace(out=S2[:, g, :], in_to_replace=V1[:, g, 0:8], in_values=S[:, g, :], imm_value=-1e30))(g), [S, V1], [S2])
        for g in range(16):
            kb.op("dve", (lambda g: lambda e: e.max(out=V1[:, g, 8:16], in_=S2[:, g, :]))(g), [S2], [V1])
        for g in range(16):
            kb.op("dve", (lambda g: lambda e: e.max_index(out=I1[:, g, 0:8], in_max=V1[:, g, 0:8], in_values=S[:, g, :]))(g), [S, V1], [I1])
            kb.op("dve", (lambda g: lambda e: e.max_index(out=I1[:, g, 8:16], in_max=V1[:, g, 8:16], in_values=S[:, g, :]))(g), [S, V1], [I1])
        kb.cp("dve", self.I1f[:], I1[:], [I1], [self.I1f])
        v1 = V1[:]
        in0 = fap(v1, [[32, 8], [1, 16], [0, 16]])
        in1 = fap(V1[:, 1:2, :], [[32, 8], [0, 16], [1, 16]])
        CA = self.CA
        kb.tt("dve", CA[:].rearrange("p h (a b) -> p h a b", a=16), in0, in1, ALU.add, [V1], [CA])
        CA2, BV, BP = self.CA2, self.BV, self.BP
        for hh in range(8):
            kb.op("dve", (lambda g: lambda e: e.max(out=BV[:, g, 0:8], in_=CA[:, g, :]))(hh), [CA], [BV])
        for hh in range(8):
            kb.op("dve", (lambda g: lambda e: e.match_replace(out=CA2[:, g, :], in_to_replace=BV[:, g, 0:8], in_values=CA[:, g, :], imm_value=-1e30))(hh), [CA, BV], [CA2])
        for hh in range(8):
            kb.op("dve", (lambda g: lambda e: e.max(out=BV[:, g, 8:16], in_=CA2[:, g, :]))(hh), [CA2], [BV])
        for hh in range(8):
            kb.op("dve", (lambda g: lambda e: e.max_index(out=BP[:, g, 0:8], in_max=BV[:, g, 0:8], in_values=CA[:, g, :]))(hh), [CA, BV], [BP])
            kb.op("dve", (lambda g: lambda e: e.max_index(out=BP[:, g, 8:16], in_max=BV[:, g, 8:16], in_values=CA[:, g, :]))(hh), [CA, BV], [BP])
        BPf = self.SEL1
        kb.cp("dve", BPf[:], BP[:], [BP], [BPf])
        OH = self.OH
        kb.tt("dve", OH[:], fap(BPf[:], [[16, 8], [1, 16], [0, 16]]), fap(c.thr16[:], [[0, 8], [0, 16], [1, 16]]), ALU.is_ge, [BPf, c.thr16], [OH])
        kb.red("dve", self.PAf[:], OH[:], ALU.add, [OH], [self.PAf])
        kb.stt(self.PBf[:].rearrange("p h k -> p (h k)"), self.PAf[:].rearrange("p h k -> p (h k)"), -16.0, BPf[:].rearrange("p h k -> p (h k)"),
               ALU.mult, ALU.add, [self.PAf, BPf], [self.PBf])
        OH = self.OH
        io = fap(c.iota16[:], [[0, 8], [0, 16], [1, 16]])
        for (Pf, SEL, off) in ((self.PAf, self.SEL1, 0), (self.PBf, self.SEL2, 16)):
            pfb = fap(Pf[:], [[16, 8], [1, 16], [0, 16]])
            kb.tt("dve", OH[:], pfb, io, ALU.is_equal, [Pf, c.iota16], [OH])
            i1b = fap(self.I1f[:, off // 16:off // 16 + 1, :], [[32, 8], [0, 16], [1, 16]])
            kb.tt("dve", OH[:], OH[:], i1b, ALU.mult, [OH, self.I1f], [OH])
            kb.red("dve", SEL[:], OH[:], ALU.add, [OH], [SEL])
        kb.stt(self.IDXf[:], self.SEL1[:].rearrange("p h k -> p (h k)"), 128.0, self.SEL2[:].rearrange("p h k -> p (h k)"),
               ALU.mult, ALU.add, [self.SEL1, self.SEL2], [self.IDXf])
        kb.cp("dve", self.IDX[:], self.IDXf[:], [self.IDXf], [self.IDX])
        GT, Z = self.GT, self.Z
        kb.tt("dve", GT[:], BV[:], fap(BV[:], [[16, 8], [0, 16]]), ALU.subtract, [BV], [GT])
        kb.act(GT[:], GT[:], AF.Exp, [GT], [GT])
        kb.red("dve", Z[:], GT[:], ALU.add, [GT], [Z])
        kb.op("dve", lambda e: e.reciprocal(out=Z[:], in_=Z[:]), [Z], [Z])
        kb.tt("dve", GT[:], GT[:], fap(Z[:], [[1, 8], [0, 16]]), ALU.mult, [GT, Z], [GT])
        HD, W = self.HD, self.W
        for s in range(128):
            gb = self.gb[self.gi % self.NG]
            self.gi += 1
            kb.dma("pool", (lambda gb, s: lambda e: e.indirect_dma_start(out=gb[:], out_offset=None, in_=self.u_tab,
                   in_offset=bass.IndirectOffsetOnAxis(ap=self.IDX[:, s:s + 1], axis=0)))(gb, s), [self.IDX], [gb])
            jk = self.junk if s % 2 == 0 else self.junk2
            kb.stt(jk[:], gb[:], 1.0, self.xn[:], ALU.mult, ALU.mult, [gb, self.xn], [jk, HD], accum=HD[:, s:s + 1], relaxed=(HD,))
        kb.act(W[:], HD[:], AF.Gelu, [HD], [W])
        kb.tt("dve", W[:], W[:], GT[:].rearrange("p h k -> p (h k)"), ALU.mult, [W, GT], [W])
        pa, pb = psb[1], psb[2]
        for s in range(128):
            gb = self.gb[self.gi % self.NG]
            self.gi += 1
            dg = self.DG[s % 4]
            kb.dma("pool", (lambda gb, s: lambda e: e.indirect_dma_start(out=gb[:], out_offset=None, in_=self.v_tab,
                   in_offset=bass.IndirectOffsetOnAxis(ap=self.IDX[:, s:s + 1], axis=0)))(gb, s), [self.IDX], [gb])
            kb.act(dg[:], c.identb[:], AF.Identity, [c.identb, W], [dg], scale=W[:, s:s + 1])
            kb.mm(pa[:, :], dg[:], gb[:, 0:512], s == 0, s == 127, [dg, gb], [pa])
            kb.mm(pb[:, :], dg[:], gb[:, 512:1024], s == 0, s == 127, [dg, gb], [pb])
        kb.tt("dve", h[:, 0:512], h[:, 0:512], pa[:, :], ALU.add, [h, pa], [h])
        kb.tt("dve", h[:, 512:1024], h[:, 512:1024], pb[:, :], ALU.add, [h, pb], [h])


class Consts:
    def __init__(self, kb, cst_dram):
        self.identf = kb.sb("c_identf", [128, 128], F32)
        self.identb = kb.sb("c_identb", [128, 128], BF16)
        self.iota16 = kb.sb("c_iota16", [128, 16], F32)
        self.triuf = kb.sb("c_triuf", [128, 128], F32)
        self.triub = kb.sb("c_triub", [128, 128], BF16)
        self.ropec = kb.sb("c_ropec", [128, 64], F32)
        self.onesf = kb.sb("c_onesf", [128, 128], F32)
        self.onesb = kb.sb("c_onesb", [128, 128], BF16)
        kb.load("sp", self.identf[:], cst_dram[:, 0:128], [self.identf])
        kb.load("sp", self.iota16[:], cst_dram[:, 128:144], [self.iota16])
        kb.load("sp", self.triuf[:], cst_dram[:, 144:272], [self.triuf])
        kb.load("sp", self.ropec[:], cst_dram[:, 272:336], [self.ropec])
        kb.cp("dve", self.identb[:], self.identf[:], [self.identf], [self.identb])
        kb.cp("dve", self.triub[:], self.triuf[:], [self.triuf], [self.triub])
        kb.memset("dve", self.onesf[:], 1.0, [self.onesf])
        kb.memset("dve", self.onesb[:], 1.0, [self.onesb])
        self.thr16 = kb.sb("c_thr16", [128, 16], F32)
        kb.ts("dve", self.thr16[:], self.iota16[:], 1.0, 16.0, ALU.add, ALU.mult, [self.iota16], [self.thr16])


def host_consts():
    c = np.zeros((128, 336), np.float32)
    c[:, 0:128] = np.eye(128, dtype=np.float32)
    c[:, 128:144] = np.arange(16, dtype=np.float32)[None, :]
    c[:, 144:272] = np.triu(np.ones((128, 128), np.float32))
    inv = (10000.0 ** (-np.arange(16, dtype=np.float32) / 16)).astype(np.float32)
    c[:, 272:288] = inv[None, :]
    c[:, 288:304] = inv[None, :]
    c[:, 304:320] = np.float32(np.pi / 2)
    c[:, 320:336] = 0.0
    return c


def build_peer_only(ntiles, dbg=False):
    nc = bass.Bass("TRN2", target_bir_lowering=False)
    h_d = nc.dram_tensor("h", [ntiles * 128, 1024], F32, kind="ExternalInput").ap()
    g_d = nc.dram_tensor("g_ffn", [1, 1024], F32, kind="ExternalInput").ap()
    wq_d = nc.dram_tensor("w_query", [1024, 1024], F32, kind="ExternalInput").ap()
    sk_d = nc.dram_tensor("sub_keys", [2, 128, 64], F32, kind="ExternalInput").ap()
    u_d = nc.dram_tensor("u_tab", [16384, 1024], F32, kind="ExternalInput").ap()
    v_d = nc.dram_tensor("v_tab", [16384, 1024], F32, kind="ExternalInput").ap()
    c_d = nc.dram_tensor("cst", [128, 336], F32, kind="ExternalInput").ap()
    o_d = nc.dram_tensor("out", [ntiles * 128, 1024], F32, kind="ExternalOutput").ap()
    kb = KB(nc)
    cst = Consts(kb, c_d)
    psb = [kb.ps("psb%d" % i, [128, 512], F32) for i in range(8)]
    peer = Peer(kb, cst)
    if dbg:
        peer.dbg = {}
        for nm, w, dt in (("IDX", 128, I32), ("GT", 128, F32), ("HD", 128, F32), ("V1", 256, F32), ("I1", 256, U32), ("BV", 128, F32),
                          ("BP", 128, U32), ("xn", 1024, F32), ("S", 2048, F32), ("PAf", 128, F32), ("PBf", 128, F32), ("qT", 1024, F32), ("W", 128, F32)):
            peer.dbg[nm] = nc.dram_tensor("dbg_" + nm, [128, w], dt, kind="ExternalOutput").ap()
    peer.load_weights(g_d[0:1, :], wq_d, sk_d, u_d, v_d, psb[7])
    hb = [kb.sb("hb%d" % i, [128, 1024], F32) for i in range(2)]
    for t in range(ntiles):
        h = hb[t % 2]
        kb.load("sp", h[:], h_d[t * 128:(t + 1) * 128, :], [h])
        peer.tile(h, psb)
        kb.store("sp", o_d[t * 128:(t + 1) * 128, :], h[:], [h], final=True)
    kb.finish()
    return nc


def phase_A(kb, cst, psb, S, D):
    sb = kb.sb
    NT, NS = S // 128, S // 512
    TWO_PI = float(2 * np.pi)
    Wd = sb("a_Wd", [128, 8, 672], BF16); Wuq = sb("a_Wuq", [128, 3, 384], BF16); Wukv = sb("a_Wukv", [128, 2, 512], BF16)
    Gat = sb("a_Gat", [128, 1024], F32); Gq = sb("a_Gq", [128, 384], F32); Gkv = sb("a_Gkv", [128, 256], F32)
    Gqn = sb("a_Gqn", [128, 96], F32); Gkn = sb("a_Gkn", [128, 96], F32)
    wd = D["w_down"].rearrange("(c p) n -> p c n", p=128)
    for c in range(8):
        kb.load("pool", Wd[:, c, :], wd[:, c, :], [Wd])
    wq = D["w_uq"].rearrange("(c p) n -> p c n", p=128)
    for c in range(3):
        kb.load("pool", Wuq[:, c, :], wq[:, c, :], [Wuq])
    wk = D["w_ukv"].rearrange("(c p) n -> p c n", p=128)
    for c in range(2):
        kb.load("pool", Wukv[:, c, :], wk[:, c, :], [Wukv])
    for (g, src) in ((Gat, "g_attn"), (Gq, "g_q"), (Gkv, "g_kv"), (Gqn, "g_qn"), (Gkn, "g_kn")):
        kb.load("sp", g[:], dram_bcast(D[src], 128), [g])
    POS = sb("a_POS", [128, NT], I32); POSF = sb("a_POSF", [128, NT], F32)
    ANG = sb("a_ANG", [128, NT, 32], F32); KI = sb("a_KI", [128, NT, 32], I32); CS = sb("a_CS", [128, NT, 32], F32)
    kb.load("sp", POS[:], D["posT"], [POS])
    kb.cp("dve", POSF[:], POS[:], [POS], [POSF])
    rc = cst.ropec
    kb.tt("dve", ANG[:], fap(POSF[:], [[1, NT], [0, 32]]), fap(rc[:, 0:32], [[0, NT], [1, 32]]), ALU.mult, [POSF, rc], [ANG])
    kb.tt("dve", ANG[:], ANG[:], fap(rc[:, 32:64], [[0, NT], [1, 32]]), ALU.add, [ANG, rc], [ANG])
    A2 = ANG[:].rearrange("p t f -> p (t f)"); C2 = CS[:].rearrange("p t f -> p (t f)"); K2 = KI[:].rearrange("p t f -> p (t f)")
    kb.ts("dve", C2, A2, 1.0 / TWO_PI, None, ALU.mult, None, [ANG], [CS])
    kb.cp("dve", K2, C2, [CS], [KI])
    kb.cp("dve", C2, K2, [KI], [CS])
    kb.stt(A2, C2, -TWO_PI, A2, ALU.mult, ALU.add, [CS, ANG], [ANG])
    kb.ts("dve", C2, A2, float(np.pi), -TWO_PI, ALU.is_gt, ALU.mult, [ANG], [CS])
    kb.tt("dve", A2, A2, C2, ALU.add, [ANG, CS], [ANG])
    kb.ts("dve", C2, A2, -float(np.pi), TWO_PI, ALU.is_lt, ALU.mult, [ANG], [CS])
    kb.tt("dve", A2, A2, C2, ALU.add, [ANG, CS], [ANG])
    kb.act(C2, A2, AF.Sin, [ANG], [CS])

    xbuf = [sb("a_x%d" % i, [128, 1024], F32) for i in range(2)]
    junk = sb("a_junk", [128, 1024], F32)
    ssq = sb("a_ssq", [128, 1], F32); rstd = sb("a_rstd", [128, 1], F32)
    ssq2 = sb("a_ssq2", [128, 1], F32); rstd2 = sb("a_rstd2", [128, 1], F32)
    ssqr = sb("a_ssqr", [128, 1], F32)
    hnb = sb("a_hnb", [128, 1024], BF16); hnT = sb("a_hnT", [128, 8, 128], BF16)
    cqb = sb("a_cqb", [128, 384], BF16); ckb = sb("a_ckb", [128, 256], BF16); cT = sb("a_cT", [128, 5, 128], BF16)
    krr = sb("a_krr", [128, 32], F32); krg = sb("a_krg", [128, 32], F32); krot = sb("a_krot", [128, 32], F32)
    sq = sb("a_sq", [128, 512], F32)
    rq = sb("a_rq", [128, 4], F32); rk = sb("a_rk", [128, 4], F32)
    qn = sb("a_qn", [128, 4, 96], F32); kn = sb("a_kn", [128, 4, 64], F32)
    t1 = sb("a_t1", [128, 4, 16], F32); t2 = sb("a_t2", [128, 4, 16], F32); t3 = sb("a_t3", [128, 4, 16], F32); t4 = sb("a_t4", [128, 4, 16], F32)
    u1 = sb("a_u1", [128, 16], F32); u2 = sb("a_u2", [128, 16], F32); u3 = sb("a_u3", [128, 16], F32); u4 = sb("a_u4", [128, 16], F32)
    qb = sb("a_qb", [128, 4, 128], BF16); kbf = sb("a_kbf", [128, 4, 128], BF16)
    vb = [sb("a_vb%d" % i, [128, 4, 64], BF16) for i in range(2)]
    QTs = [sb("a_QTs%d" % i, [128, 512], BF16) for i in range(2)]
    KTs = [sb("a_KTs%d" % i, [128, 512], BF16) for i in range(2)]
    kb.memset("pool", qb[:], 0.0, [qb]); kb.memset("pool", kbf[:], 0.0, [kbf])
    identb = cst.identb
    QTd = D["QT_d"].rearrange("h d s -> d h s"); KTd = D["KT_d"].rearrange("h d s -> d h s")
    Vd = D["V_d"].rearrange("h p t d -> p h t d")

    def sqrt_recip(t_ap, tt_):
        kb.act(t_ap, t_ap, AF.Sqrt, [tt_], [tt_])
        kb.op("dve", lambda e: e.reciprocal(out=t_ap, in_=t_ap), [tt_], [tt_])

    for t in range(NT):
        xt = xbuf[t % 2]
        kb.load("sp", xt[:], D["x"][t * 128:(t + 1) * 128, :], [xt])
        rms_rstd(kb, xt[:], 1024, junk, ssq, rstd, [xt])
        kb.stt(hnb[:], xt[:], rstd[:, 0:1], Gat[:], ALU.mult, ALU.mult, [xt, rstd, Gat], [hnb])
        pt = psb[0]
        ptb = pt[:].bitcast(BF16)
        for c in range(8):
            kb.tr(ptb[:, c * 128:(c + 1) * 128], hnb[:, c * 128:(c + 1) * 128], identb[:], [hnb, identb], [pt])
        kb.cp("act", hnT[:].rearrange("p c t -> p (c t)"), ptb, [pt], [hnT])
        for c in range(8):
            kb.mm(psb[1][:, 0:384], hnT[:, c, :], Wd[:, c, 0:384], c == 0, c == 7, [hnT, Wd], [psb[1]])
        for c in range(8):
            kb.mm(psb[2][:, 0:288], hnT[:, c, :], Wd[:, c, 384:672], c == 0, c == 7, [hnT, Wd], [psb[2]])
        rms_rstd(kb, psb[1][:, 0:384], 384, junk, ssq, rstd, [psb[1]])
        kb.stt(cqb[:], psb[1][:, 0:384], rstd[:, 0:1], Gq[:], ALU.mult, ALU.mult, [psb[1], rstd, Gq], [cqb])
        rms_rstd(kb, psb[2][:, 0:256], 256, junk, ssq2, rstd2, [psb[2]])
        kb.stt(ckb[:], psb[2][:, 0:256], rstd2[:, 0:1], Gkv[:], ALU.mult, ALU.mult, [psb[2], rstd2, Gkv], [ckb])
        kb.cp("act", krr[:], psb[2][:, 256:288], [psb[2]], [krr])
        for c in range(3):
            kb.tr(ptb[:, c * 128:(c + 1) * 128], cqb[:, c * 128:(c + 1) * 128], identb[:], [cqb, identb], [pt])
        for c in range(2):
            kb.tr(ptb[:, (3 + c) * 128:(4 + c) * 128], ckb[:, c * 128:(c + 1) * 128], identb[:], [ckb, identb], [pt])
        kb.cp("act", cT[:].rearrange("p c t -> p (c t)"), ptb[:, 0:640], [pt], [cT])
        for c in range(3):
            kb.mm(psb[3][:, 0:384], cT[:, c, :], Wuq[:, c, :], c == 0, c == 2, [cT, Wuq], [psb[3]])
        for c in range(2):
            kb.mm(psb[4][:, 0:512], cT[:, 3 + c, :], Wukv[:, c, :], c == 0, c == 1, [cT, Wukv], [psb[4]])
        cosq = fap(CS[:, t, 0:16], [[0, 4], [1, 16]]); sinq = fap(CS[:, t, 16:32], [[0, 4], [1, 16]])
        q3 = psb[3][:, 0:384].rearrange("p (h d) -> p h d", h=4)
        kb.act(sq[:, 0:384], psb[3][:, 0:384], AF.Square, [psb[3]], [sq])
        kb.red("dve", rq[:, 0:4], sq[:, 0:384].rearrange("p (h d) -> p h d", h=4), ALU.add, [sq], [rq])
        kb.ts("dve", rq[:, 0:4], rq[:, 0:4], 1.0 / 96, EPS, ALU.mult, ALU.add, [rq], [rq])
        sqrt_recip(rq[:, 0:4], rq)
        kb.tt("dve", qn[:], q3, fap(rq[:, 0:4], [[1, 4], [0, 96]]), ALU.mult, [psb[3], rq], [qn])
        kb.tt("dve", qn[:], qn[:], fap(Gqn[:], [[0, 4], [1, 96]]), ALU.mult, [qn, Gqn], [qn])
        kb.tt("dve", t1[:], qn[:, :, 64:80], cosq, ALU.mult, [qn, CS], [t1])
        kb.tt("dve", t2[:], qn[:, :, 80:96], sinq, ALU.mult, [qn, CS], [t2])
        kb.tt("dve", t3[:], qn[:, :, 80:96], cosq, ALU.mult, [qn, CS], [t3])
        kb.tt("dve", t4[:], qn[:, :, 64:80], sinq, ALU.mult, [qn, CS], [t4])
        kb.cp("act", qb[:, :, 0:64], qn[:, :, 0:64], [qn], [qb])
        kb.tt("dve", qb[:, :, 64:80], t1[:], t2[:], ALU.subtract, [t1, t2], [qb])
        kb.tt("dve", qb[:, :, 80:96], t3[:], t4[:], ALU.add, [t3, t4], [qb])
        kv3 = psb[4][:, 0:512].rearrange("p (h d) -> p h d", h=4)
        kb.act(sq[:, 0:512], psb[4][:, 0:512], AF.Square, [psb[4]], [sq])
        kb.red("dve", rk[:, 0:4], sq[:, 0:512].rearrange("p (h d) -> p h d", h=4)[:, :, 0:64], ALU.add, [sq], [rk])
        kb.act(junk[:, 0:32], krr[:], AF.Square, [krr], [junk, ssqr], accum=ssqr[:, 0:1])
        kb.ts("dve", rk[:, 0:4], rk[:, 0:4], ssqr[:, 0:1], 1.0 / 96, ALU.add, ALU.mult, [rk, ssqr], [rk])
        kb.ts("dve", rk[:, 0:4], rk[:, 0:4], EPS, None, ALU.add, None, [rk], [rk])
        sqrt_recip(rk[:, 0:4], rk)
        kb.tt("dve", krg[:], krr[:], Gkn[:, 64:96], ALU.mult, [krr, Gkn], [krg])
        cs_, sn_ = CS[:, t, 0:16], CS[:, t, 16:32]
        kb.tt("dve", u1[:], krg[:, 0:16], cs_, ALU.mult, [krg, CS], [u1])
        kb.tt("dve", u2[:], krg[:, 16:32], sn_, ALU.mult, [krg, CS], [u2])
        kb.tt("dve", u3[:], krg[:, 16:32], cs_, ALU.mult, [krg, CS], [u3])
        kb.tt("dve", u4[:], krg[:, 0:16], sn_, ALU.mult, [krg, CS], [u4])
        kb.tt("dve", krot[:, 0:16], u1[:], u2[:], ALU.subtract, [u1, u2], [krot])
        kb.tt("dve", krot[:, 16:32], u3[:], u4[:], ALU.add, [u3, u4], [krot])
        kb.tt("dve", kn[:], kv3[:, :, 0:64], fap(rk[:, 0:4], [[1, 4], [0, 64]]), ALU.mult, [psb[4], rk], [kn])
        kb.tt("dve", kbf[:, :, 0:64], kn[:], fap(Gkn[:, 0:64], [[0, 4], [1, 64]]), ALU.mult, [kn, Gkn], [kbf])
        kb.tt("dve", kbf[:, :, 64:96], fap(krot[:], [[0, 4], [1, 32]]), fap(rk[:, 0:4], [[1, 4], [0, 32]]), ALU.mult, [krot, rk], [kbf])
        vbt = vb[t % 2]
        kb.cp("act", vbt[:], kv3[:, :, 64:128], [psb[4]], [vbt])
        kb.store("sp", Vd[:, :, t, :], vbt[:], [vbt])
        pq = psb[5]
        pqb = pq[:].bitcast(BF16)
        for h in range(4):
            kb.tr(pqb[:, h * 128:(h + 1) * 128], qb[:, h, :], identb[:], [qb, identb], [pq])
        for h in range(4):
            kb.tr(pqb[:, (4 + h) * 128:(5 + h) * 128], kbf[:, h, :], identb[:], [kbf, identb], [pq])
        qs, ks = QTs[t % 2], KTs[t % 2]
        kb.cp("act", qs[:, :], pqb[:, 0:512], [pq], [qs])
        kb.cp("act", ks[:, :], pqb[:, 512:1024], [pq], [ks])
        kb.store("sp", QTd[:, :, t * 128:(t + 1) * 128], qs[0:96, :].rearrange("p (h t) -> p h t", h=4), [qs])
        kb.store("sp", KTd[:, :, t * 128:(t + 1) * 128], ks[0:96, :].rearrange("p (h t) -> p h t", h=4), [ks])

    kb.dma_barrier("sp")
    KT = sb("a_KT", [96, S], BF16)
    VA = sb("a_VA", [128, NT, 128], BF16)
    QTb = [sb("a_QTb%d" % i, [96, 512], BF16) for i in range(2)]
    PT = [sb("a_PT%d" % i, [128, 512], BF16) for i in range(3)]
    osb = sb("a_osb", [128, 512], F32); rl = sb("a_rl", [128, 512], F32)
    oTt = [sb("a_oTt%d" % i, [64, 512], BF16) for i in range(2)]
    nbias = sb("a_nbias", [128, 1], F32)
    kb.memset("dve", nbias[:], -8.0, [nbias])
    kb.memset("pool", VA[:], 1.0, [VA])
    SPS = [psb[0], psb[1]]; OACC = [psb[2], psb[3]]; bcb = psb[4]
    scale = 96 ** -0.5
    ip = 0
    for h in range(4):
        KC = min(2048, S)
        for c in range(S // KC):
            kb.load("sp", KT[0:96, c * KC:(c + 1) * KC], D["KT_d"][h, :, c * KC:(c + 1) * KC], [KT])
        TC = min(32, NT)
        for c in range(NT // TC):
            kb.load("sp", VA[:, c * TC:(c + 1) * TC, 0:64], D["V_d"][h, :, c * TC:(c + 1) * TC, :], [VA])
        for p in range(NS):
            qt = QTb[p % 2]
            kb.load("sp", qt[0:96, :], D["QT_d"][h, :, p * 512:(p + 1) * 512], [qt])
            oacc = OACC[p % 2]
            nk = 4 * (p + 1)
            slots = {}

            def emit_S(ki):
                nonlocal ip
                r = ki - 4 * p
                c0 = 128 * r if r > 0 else 0
                sps = SPS[ip % 2]; pT = PT[ip % 3]; ip += 1
                slots[ki] = (r, c0, sps, pT)
                kb.mm(sps[:, c0:512], KT[0:96, ki * 128:(ki + 1) * 128], qt[0:96, c0:512], True, True, [KT, qt], [sps])

            emit_S(0)
            for ki in range(nk):
                if ki + 1 < nk:
                    emit_S(ki + 1)
                r, c0, sps, pT = slots.pop(ki)
                kb.act(pT[:, c0:512], sps[:, c0:512], AF.Exp, [sps, nbias], [pT], scale=scale, bias=nbias[:, 0:1])
                if r >= 0:
                    kb.tt("pool", pT[:, 128 * r:128 * (r + 1)], pT[:, 128 * r:128 * (r + 1)], cst.triub[:], ALU.mult, [pT, cst.triub], [pT])
                kb.mm(oacc[:, c0:512], VA[:, ki, :], pT[:, c0:512], ki == 0, ki == nk - 1, [VA, pT], [oacc])
            kb.cp("act", osb[:, :], oacc[:, :], [oacc], [osb])
            kb.op("dve", lambda e: e.reciprocal(out=rl[64:128, :], in_=osb[64:128, :]), [osb], [rl])
            kb.mm(bcb[0:64, :], cst.onesf[64:65, 0:64], rl[64:65, :], True, True, [cst.onesf, rl], [bcb])
            ot = oTt[p % 2]
            kb.tt("dve", ot[0:64, :], osb[0:64, :], bcb[0:64, :], ALU.mult, [osb, bcb], [ot])
            kb.store("sp", D["oT"][h * 64:(h + 1) * 64, p * 512:(p + 1) * 512], ot[0:64, :], [ot], final=True)


def phase_P(kb, cst, psb, NTOK, D, peer):
    sb = kb.sb
    Wo = sb("p_Wo", [128, 8, 1024], BF16)
    wo = D["w_o"].rearrange("(c p) n -> p c n", p=128)
    for c in range(8):
        kb.load("pool", Wo[:, c, :], wo[:, c, :], [Wo])
    otb = [sb("p_ot%d" % i, [128, 8, 128], BF16) for i in range(2)]
    hb = [sb("p_h%d" % i, [128, 1024], F32) for i in range(2)]
    oT = D["oT"].rearrange("(c p) s -> p c s", p=128)
    for t in range(NTOK // 128):
        ot, h = otb[t % 2], hb[t % 2]
        kb.load("sp", ot[:], oT[:, :, t * 128:(t + 1) * 128], [ot])
        kb.load("sp", h[:], D["resid"][t * 128:(t + 1) * 128, :], [h])
        for half in range(2):
            bank = psb[7 - half]
            for c in range(8):
                kb.mm(bank[:, :], ot[:, c, :], Wo[:, c, half * 512:(half + 1) * 512], c == 0, c == 7, [ot, Wo], [bank])
        kb.tt("dve", h[:, 0:512], h[:, 0:512], psb[7][:, :], ALU.add, [h, psb[7]], [h])
        kb.tt("dve", h[:, 512:1024], h[:, 512:1024], psb[6][:, :], ALU.add, [h, psb[6]], [h])
        peer.tile(h, psb)
        kb.store("sp", D["out"][t * 128:(t + 1) * 128, :], h[:], [h], final=True)


def phase_G(kb, cst, psb, S, D):
    sb = kb.sb
    NT = S // 128
    Win = sb("g_Win", [128, 8, 896], BF16)
    win = D["w_in"].rearrange("(c p) n -> p c n", p=128)
    for c in range(8):
        kb.load("pool", Win[:, c, :], win[:, c, :], [Win])
    Gat = sb("g_Gat", [128, 1024], F32); Gon = sb("g_Gon", [128, 256], F32)
    kb.load("sp", Gat[:], dram_bcast(D["g_attn"], 128), [Gat])
    kb.load("sp", Gon[:], dram_bcast(D["g_on"], 128), [Gon])
    Wg2 = sb("g_Wg2", [128, 128], BF16); bg = sb("g_bg", [128, 128], F32)
    kb.load("pool", Wg2[:], D["w_g2"], [Wg2])
    kb.load("sp", bg[:], dram_bcast(D["b_g"], 128), [bg])
    zb = sb("g_zb", [128, 128], F32)
    xbuf = [sb("g_x%d" % i, [128, 1024], F32) for i in range(2)]
    junk = sb("g_junk", [128, 1024], F32)
    ssq = sb("g_ssq", [128, 1], F32); rstd = sb("g_rstd", [128, 1], F32)
    hnb = sb("g_hnb", [128, 1024], BF16); hnT = sb("g_hnT", [128, 8, 128], BF16)
    glT = sb("g_glT", [128, 128], BF16)
    ez = sb("g_ez", [128, 128], F32); la = sb("g_la", [128, 128], F32)
    cs = sb("g_cs", [128, 128], F32); dd = sb("g_dd", [128, 128], F32)
    epos = sb("g_epos", [128, 128], F32); eneg = sb("g_eneg", [128, 128], F32); erel = sb("g_erel", [128, 128], F32)
    dec = sb("g_dec", [128, 1], F32)
    qd = sb("g_qd", [128, 128], BF16); ki = sb("g_ki", [128, 128], BF16); kd = sb("g_kd", [128, 128], BF16)
    qdT = sb("g_qdT", [128, 128], BF16); kiT = sb("g_kiT", [128, 128], BF16)
    vb = sb("g_vb", [128, 256], BF16)
    at = sb("g_at", [128, 128], BF16)
    St = sb("g_S", [128, 256], F32); Sb = sb("g_Sb", [128, 256], BF16)
    on = sb("g_onb", [128, 256], F32); sr = sb("g_sr", [128, 256], F32); og = sb("g_og", [128, 256], BF16)
    ogT = [sb("g_ogT%d" % i, [128, 2, 512], BF16) for i in range(2)]
    kb.memset("dve", St[:], 0.0, [St]); kb.memset("dve", Sb[:], 0.0, [Sb])
    identb = cst.identb
    oTd = D["oT"].rearrange("(c p) s -> p c s", p=128)
    for t in range(NT):
        xt = xbuf[t % 2]
        kb.load("sp", xt[:], D["x"][t * 128:(t + 1) * 128, :], [xt])
        rms_rstd(kb, xt[:], 1024, junk, ssq, rstd, [xt])
        kb.stt(hnb[:], xt[:], rstd[:, 0:1], Gat[:], ALU.mult, ALU.mult, [xt, rstd, Gat], [hnb])
        pt = psb[0]
        ptb = pt[:].bitcast(BF16)
        for c in range(8):
            kb.tr(ptb[:, c * 128:(c + 1) * 128], hnb[:, c * 128:(c + 1) * 128], identb[:], [hnb, identb], [pt])
        kb.cp("act", hnT[:].rearrange("p c t -> p (c t)"), ptb, [pt], [hnT])
        pA, pB, pC = psb[1], psb[2], psb[3]
        for c in range(8):
            kb.mm(pA[:, 0:512], hnT[:, c, :], Win[:, c, 0:512], c == 0, c == 7, [hnT, Win], [pA])
        for c in range(8):
            kb.mm(pB[:, 0:256], hnT[:, c, :], Win[:, c, 512:768], c == 0, c == 7, [hnT, Win], [pB])
        for c in range(8):
            kb.mm(pC[:, 0:128], Win[:, c, 768:896], hnT[:, c, :], c == 0, c == 7, [hnT, Win], [pC])
        kb.cp("act", glT[:], pC[:, 0:128], [pC], [glT])
        kb.mm(pC[:, 128:256], glT[:], Wg2[:], True, True, [glT, Wg2], [pC])
        kb.tt("dve", zb[:], pC[:, 128:256], bg[:], ALU.add, [pC, bg], [zb])
        kb.act(ez[:], zb[:], AF.Exp, [zb], [ez], scale=-1.0)
        kb.act(la[:], ez[:], AF.Ln, [ez], [la], bias=1.0)
        pD = psb[4]
        kb.mm(pD[:, 0:128], cst.triuf[:], la[:], True, True, [cst.triuf, la], [pD])
        kb.mm(pD[:, 128:256], cst.onesf[:], la[:], True, True, [cst.onesf, la], [pD])
        kb.mm(pD[:, 256:384], la[:], cst.onesf[:], True, True, [cst.onesf, la], [pD])
        kb.cp("dve", cs[:], pD[:, 0:128], [pD], [cs])
        kb.tt("dve", dd[:], pD[:, 128:256], cs[:], ALU.subtract, [pD, cs], [dd])
        kb.act(epos[:], cs[:], AF.Exp, [cs], [epos], scale=-1.0 / 16)
        kb.act(eneg[:], cs[:], AF.Exp, [cs], [eneg], scale=1.0 / 16)
        kb.act(erel[:], dd[:], AF.Exp, [dd], [erel], scale=-1.0 / 16)
        kb.act(dec[:, 0:1], pD[:, 256:257], AF.Exp, [pD], [dec], scale=-1.0 / 16)
        kb.stt(qd[:], pA[:, 0:128], 128 ** -0.5, epos[:], ALU.mult, ALU.mult, [pA, epos], [qd])
        kb.tt("dve", ki[:], pA[:, 128:256], eneg[:], ALU.mult, [pA, eneg], [ki])
        kb.tt("dve", kd[:], pA[:, 128:256], erel[:], ALU.mult, [pA, erel], [kd])
        kb.cp("act", vb[:], pA[:, 256:512], [pA], [vb])
        pE = psb[5]
        peb = pE[:].bitcast(BF16)
        kb.tr(peb[:, 0:128], qd[:], identb[:], [qd, identb], [pE])
        kb.tr(peb[:, 128:256], ki[:], identb[:], [ki, identb], [pE])
        kb.cp("act", qdT[:], peb[:, 0:128], [pE], [qdT])
        kb.cp("act", kiT[:], peb[:, 128:256], [pE], [kiT])
        pF = psb[6]
        kb.mm(pF[:, 0:128], kiT[:], qdT[:], True, True, [kiT, qdT], [pF])
        kb.tt("dve", at[:], pF[:, 0:128], cst.triuf[:], ALU.mult, [pF, cst.triuf], [at])
        kb.mm(pF[:, 256:512], at[:], vb[:], True, False, [at, vb], [pF])
        kb.mm(pF[:, 256:512], qdT[:], Sb[:], False, True, [qdT, Sb], [pF])
        pG = psb[7]
        kb.mm(pG[:, 0:256], kd[:], vb[:], True, True, [kd, vb], [pG])
        kb.stt(St[:], St[:], dec[:, 0:1], pG[:, 0:256], ALU.mult, ALU.add, [St, dec, pG], [St])
        kb.cp("act", Sb[:], St[:], [St], [Sb])
        rms_rstd(kb, pF[:, 256:512], 256, junk, ssq, rstd, [pF])
        kb.stt(on[:], pF[:, 256:512], rstd[:, 0:1], Gon[:], ALU.mult, ALU.mult, [pF, rstd, Gon], [on])
        kb.act(sr[:], pB[:, 0:256], AF.Silu, [pB], [sr])
        kb.tt("dve", og[:], on[:], sr[:], ALU.mult, [on, sr], [og])
        kb.tr(peb[:, 256:384], og[:, 0:128], identb[:], [og, identb], [pE])
        kb.tr(peb[:, 384:512], og[:, 128:256], identb[:], [og, identb], [pE])
        sp_, j = t // 4, t % 4
        ogs = ogT[sp_ % 2]
        kb.cp("act", ogs[:, 0, j * 128:(j + 1) * 128], peb[:, 256:384], [pE], [ogs])
        kb.cp("act", ogs[:, 1, j * 128:(j + 1) * 128], peb[:, 384:512], [pE], [ogs])
        if j == 3 or t == NT - 1:
            w = (j + 1) * 128
            kb.store("sp", oTd[:, :, sp_ * 512:sp_ * 512 + w], ogs[:, :, 0:w], [ogs], final=True)


def _psum_banks(kb):
    return [kb.ps("psb%d" % i, [128, 512], F32) for i in range(8)]


def build_A(S, debug=False):
    nc = bass.Bass("TRN2", target_bir_lowering=False)
    dt_ = lambda n, sh, dt, kind="ExternalInput": nc.dram_tensor(n, sh, dt, kind=kind).ap()
    D = {
        "x": dt_("x", [S, 1024], F32), "posT": dt_("posT", [128, S // 128], I32),
        "g_attn": dt_("g_attn", [1, 1024], F32), "w_down": dt_("w_down", [1024, 672], F32),
        "g_q": dt_("g_q", [1, 384], F32), "w_uq": dt_("w_uq", [384, 384], F32),
        "g_kv": dt_("g_kv", [1, 256], F32), "w_ukv": dt_("w_ukv", [256, 512], F32),
        "g_qn": dt_("g_qn", [1, 96], F32), "g_kn": dt_("g_kn", [1, 96], F32),
        "oT": dt_("oT", [256, S], BF16, "ExternalOutput"),
    }
    kind = "ExternalOutput" if debug else "Internal"
    D["QT_d"] = dt_("QT_d", [4, 96, S], BF16, kind)
    D["KT_d"] = dt_("KT_d", [4, 96, S], BF16, kind)
    D["V_d"] = dt_("V_d", [4, 128, S // 128, 64], BF16, kind)
    c_d = dt_("cst", [128, 336], F32)
    kb = KB(nc)
    cst = Consts(kb, c_d)
    psb = _psum_banks(kb)
    phase_A(kb, cst, psb, S, D)
    kb.finish()
    return nc


def build_P(NTOK):
    nc = bass.Bass("TRN2", target_bir_lowering=False)
    dt_ = lambda n, sh, dt, kind="ExternalInput": nc.dram_tensor(n, sh, dt, kind=kind).ap()
    D = {
        "oT": dt_("oT", [1024, NTOK], BF16), "resid": dt_("resid", [NTOK, 1024], F32), "w_o": dt_("w_o", [1024, 1024], F32),
        "out": dt_("out", [NTOK, 1024], F32, "ExternalOutput"),
    }
    g_d = dt_("g_ffn", [1, 1024], F32); wq_d = dt_("w_query", [1024, 1024], F32); sk_d = dt_("sub_keys", [2, 128, 64], F32)
    u_d = dt_("u_tab", [16384, 1024], F32); v_d = dt_("v_tab", [16384, 1024], F32)
    c_d = dt_("cst", [128, 336], F32)
    kb = KB(nc)
    cst = Consts(kb, c_d)
    psb = _psum_banks(kb)
    peer = Peer(kb, cst)
    peer.load_weights(g_d, wq_d, sk_d, u_d, v_d, psb[7])
    phase_P(kb, cst, psb, NTOK, D, peer)
    kb.finish()
    return nc


def build_G(S):
    nc = bass.Bass("TRN2", target_bir_lowering=False)
    dt_ = lambda n, sh, dt, kind="ExternalInput": nc.dram_tensor(n, sh, dt, kind=kind).ap()
    D = {
        "x": dt_("x", [S, 1024], F32), "g_attn": dt_("g_attn", [1, 1024], F32), "w_in": dt_("w_in", [1024, 896], F32),
        "w_g2": dt_("w_g2", [128, 128], F32), "b_g": dt_("b_g", [1, 128], F32), "g_on": dt_("g_on", [1, 256], F32),
        "oT": dt_("oT", [256, S], BF16, "ExternalOutput"),
    }
    c_d = dt_("cst", [128, 336], F32)
    kb = KB(nc)
    cst = Consts(kb, c_d)
    psb = _psum_banks(kb)
    phase_G(kb, cst, psb, S, D)
    kb.finish()
    return nc


def _c(a):
    return np.ascontiguousarray(a)


def inputs_A(x_b, pos_b, P, g):
    S = x_b.shape[0]
    hs = slice(4 * g, 4 * g + 4)
    w_uq = P["mla_w_uq"][0].reshape(384, 16, 96)[:, hs, :].reshape(384, 384)
    w_ukv = P["mla_w_ukv"][0].reshape(256, 16, 128)[:, hs, :].reshape(256, 512)
    return dict(x=_c(x_b), posT=_c(pos_b.reshape(S // 128, 128).T.astype(np.int32)),
                g_attn=_c(P["attn_norm_g"][0:1]), w_down=_c(P["mla_w_down"][0]), g_q=_c(P["mla_g_q_lat"][0:1]), w_uq=_c(w_uq),
                g_kv=_c(P["mla_g_kv_lat"][0:1]), w_ukv=_c(w_ukv), g_qn=_c(P["mla_g_qn"][0:1]), g_kn=_c(P["mla_g_kn"][0:1]),
                cst=host_consts())


def inputs_P(oT_tok, resid_tok, w_o, P, layer):
    return dict(oT=_c(oT_tok), resid=_c(resid_tok), w_o=_c(w_o), g_ffn=_c(P["ffn_norm_g"][layer:layer + 1]),
                w_query=_c(P["peer_w_query"][layer]), sub_keys=_c(P["peer_sub_keys"][layer]),
                u_tab=_c(P["peer_u"][layer]), v_tab=_c(P["peer_v"][layer]), cst=host_consts())


def inputs_G(h_b, P, hh):
    w = P["gla_w_in"][0]
    w_in = np.concatenate([w[:, 128 * hh:128 * (hh + 1)], w[:, 512 + 128 * hh:512 + 128 * (hh + 1)],
                           w[:, 1024 + 256 * hh:1024 + 256 * (hh + 1)], w[:, 2048 + 256 * hh:2048 + 256 * (hh + 1)], w[:, 3072:3088],
                           np.zeros((1024, 112), np.float32)], axis=1)
    wg2 = np.zeros((128, 128), np.float32)
    wg2[0:16] = P["gla_w_g2"][0][:, 128 * hh:128 * (hh + 1)]
    return dict(x=_c(h_b), g_attn=_c(P["attn_norm_g"][1:2]), w_in=_c(w_in), w_g2=wg2,
                b_g=_c(P["gla_b_g"][0:1, 128 * hh:128 * (hh + 1)]), g_on=_c(P["gla_g_on"][0:1]), cst=host_consts())


def kernel(**inp):
    P = {k: np.asarray(v) for k, v in inp.items()}
    x = P["x"]
    B, S, _ = x.shape
    ncore = 4 * B
    ids = list(range(ncore))
    TOK = S // 4
    ncA = build_A(S)
    ims = [inputs_A(x[b], P["positions"][b], P, g) for b in range(B) for g in range(4)]
    res = run_bass_kernel_spmd(ncA, ims, core_ids=ids)
    oT = [np.concatenate([np.asarray(res.results[b * 4 + g]["oT"]) for g in range(4)], axis=0) for b in range(B)]
    ncP = build_P(TOK)
    ims = [inputs_P(oT[b][:, j * TOK:(j + 1) * TOK], x[b, j * TOK:(j + 1) * TOK], P["mla_w_o"][0], P, 0) for b in range(B) for j in range(4)]
    res = run_bass_kernel_spmd(ncP, ims, core_ids=ids)
    h1 = np.stack([np.concatenate([np.asarray(res.results[b * 4 + j]["out"]) for j in range(4)], axis=0) for b in range(B)])
    ncG = build_G(S)
    ims = [inputs_G(h1[b], P, hh) for b in range(B) for hh in range(4)]
    res = run_bass_kernel_spmd(ncG, ims, core_ids=ids)
    gT = [np.concatenate([np.asarray(res.results[b * 4 + hh]["oT"]) for hh in range(4)], axis=0) for b in range(B)]
    ncP2 = build_P(TOK)
    ims = [inputs_P(gT[b][:, j * TOK:(j + 1) * TOK], h1[b, j * TOK:(j + 1) * TOK], P["gla_w_o"][0], P, 1) for b in range(B) for j in range(4)]
    res = run_bass_kernel_spmd(ncP2, ims, core_ids=ids)
    out = np.stack([np.concatenate([np.asarray(res.results[b * 4 + j]["out"]) for j in range(4)], axis=0) for b in range(B)])
    return out.astype(np.float32)
```

```python
from contextlib import ExitStack
import numpy as np
import ml_dtypes
import concourse.bass as bass
import concourse.mybir as mybir
from concourse.bass_utils import run_bass_kernel_spmd

AF = mybir.ActivationFunctionType
ALU = mybir.AluOpType
AX = mybir.AxisListType
F32, BF16, I32, U32 = mybir.dt.float32, mybir.dt.bfloat16, mybir.dt.int32, mybir.dt.uint32

ENGS = ("pe", "act", "dve", "pool", "sp")
EPS = 1e-6


class T:
    __slots__ = ("h", "w", "r", "name", "psum")

    def __init__(self, h, name="", psum=False):
        self.h = h
        self.w = None
        self.r = {}
        self.name = name
        self.psum = psum

    def __getitem__(self, idx):
        return self.h[idx]


class KB:
    def __init__(self, nc, n_dma_sems=32):
        self.nc = nc
        self.es = ExitStack()
        self.prog = {e: [] for e in ENGS}
        self.sems = {}
        self.cnt = {}
        for e in ENGS:
            self.sems[e] = self.es.enter_context(nc.semaphore("s_" + e))
            self.cnt[e] = 0
        self.dsems = []
        self.dq = {"sp": [], "pool": [], "act": []}
        for q, n in (("sp", n_dma_sems), ("pool", 16)):
            for i in range(n):
                k = "d%s%d" % (q, i)
                self.sems[k] = self.es.enter_context(nc.semaphore("s_" + k))
                self.cnt[k] = 0
                self.dsems.append(k)
                self.dq[q].append(k)
        self.dnext = {"sp": 0, "pool": 0}
        self.seen = {e: {} for e in ENGS}
        self.final = []
        self.pending = {e: [] for e in ENGS}

    def sb(self, name, shape, dt):
        h = self.es.enter_context(self.nc.sbuf_tensor(name, list(shape), dt))
        return T(h, name)

    def ps(self, name, shape, dt):
        h = self.es.enter_context(self.nc.psum_tensor(name, list(shape), dt))
        return T(h, name, psum=True)

    def _waits(self, eng, reads, writes, relaxed=()):
        deps = {}

        def add(k, v):
            if v > deps.get(k, 0):
                deps[k] = v
        for t in reads:
            if t.w is not None:
                k, v = t.w
                if not (k == eng and eng == "pe"):
                    add(k, v)
            if t.psum:
                for k, v in t.r.items():
                    if k != eng:
                        add(k, v)
        for t in writes:
            same_ok = (eng == "pe") or any(t is r for r in relaxed)
            if t.w is not None:
                k, v = t.w
                if k != eng or not same_ok:
                    add(k, v)
            for k, v in t.r.items():
                if k != eng or not same_ok:
                    add(k, v)
        out = []
        seen = self.seen[eng]
        for k, v in deps.items():
            if seen.get(k, 0) >= v:
                continue
            seen[k] = v
            out.append((k, v))
        return out

    def _commit(self, done, reads, writes):
        k, v = done
        for t in writes:
            t.w = done
            t.r = {}
        for t in reads:
            if t.r.get(k, 0) < v:
                t.r[k] = v

    def dma_barrier(self, eng):
        for k in self.dsems:
            v = self.cnt[k]
            if v > 0 and self.seen[eng].get(k, 0) < v:
                self.seen[eng][k] = v
                self.pending[eng].append((k, v))

    def op(self, eng, fn, reads=(), writes=(), relaxed=()):
        waits = self.pending[eng] + self._waits(eng, reads, writes, relaxed)
        self.pending[eng] = []
        self.cnt[eng] += 1
        done = (eng, self.cnt[eng])
        self.prog[eng].append((waits, fn, (eng, 1)))
        self._commit(done, reads, writes)
        return done

    def dma(self, eng, fn, reads=(), writes=(), final=False):
        k = self.dq[eng][self.dnext[eng]]
        self.dnext[eng] = (self.dnext[eng] + 1) % len(self.dq[eng])
        waits = self.pending[eng] + self._waits(eng, reads, writes)
        self.pending[eng] = []
        prev = self.cnt[k]
        if prev > 0 and self.seen[eng].get(k, 0) < prev:
            self.seen[eng][k] = prev
            waits.append((k, prev))
        self.cnt[k] += 16
        done = (k, self.cnt[k])
        self.prog[eng].append((waits, fn, (k, 16)))
        self._commit(done, reads, writes)
        if final:
            self.final.append(done)
        return done

    def finish(self):
        nc = self.nc
        fw = [(k, self.cnt[k]) for k in self.dsems if self.cnt[k] > 0]
        sems = self.sems
        prog = self.prog

        def replay(name):
            def f(eng):
                for waits, fn, inc in prog[name]:
                    for k, v in waits:
                        eng.wait_ge(sems[k], v)
                    ins = fn(eng)
                    ins.then_inc(sems[inc[0]], inc[1])
                if name == "sp":
                    for k, v in fw:
                        eng.wait_ge(sems[k], v)
            return f
        with nc.Block() as block:
            block.tensor(replay("pe"))
            block.scalar(replay("act"))
            block.vector(replay("dve"))
            block.gpsimd(replay("pool"))
            block.sync(replay("sp"))
        self.es.close()

    def mm(self, out, lhsT, rhs, start, stop, reads, writes):
        return self.op("pe", lambda e: e.matmul(out, lhsT=lhsT, rhs=rhs, start=start, stop=stop), reads, writes)

    def tr(self, out, in_, ident, reads, writes):
        return self.op("pe", lambda e: e.transpose(out, in_, ident), reads, writes)

    def act(self, out, in_, func, reads, writes, bias=None, scale=None, accum=None):
        kw = {}
        if bias is not None:
            kw["bias"] = bias
        if scale is not None:
            kw["scale"] = scale
        if accum is not None:
            kw["accum_out"] = accum
        return self.op("act", lambda e: e.activation(out=out, in_=in_, func=func, **kw), reads, writes)

    def cp(self, eng, out, in_, reads, writes):
        if eng == "act":
            return self.op("act", lambda e: e.copy(out=out, in_=in_), reads, writes)
        return self.op(eng, lambda e: e.tensor_copy(out=out, in_=in_), reads, writes)

    def tt(self, eng, out, in0, in1, op, reads, writes):
        return self.op(eng, lambda e: e.tensor_tensor(out=out, in0=in0, in1=in1, op=op), reads, writes)

    def ts(self, eng, out, in0, s1, s2, op0, op1, reads, writes, accum=None):
        if op1 is None:
            return self.op(eng, lambda e: e.tensor_scalar(out=out, in0=in0, scalar1=s1, scalar2=None, op0=op0), reads, writes)
        if accum is not None:
            return self.op(eng, lambda e: e.tensor_scalar(out=out, in0=in0, scalar1=s1, scalar2=s2, op0=op0, op1=op1, accum_out=accum), reads, writes)
        return self.op(eng, lambda e: e.tensor_scalar(out=out, in0=in0, scalar1=s1, scalar2=s2, op0=op0, op1=op1), reads, writes)

    def stt(self, out, in0, scalar, in1, op0, op1, reads, writes, accum=None, relaxed=()):
        if accum is not None:
            return self.op("dve", lambda e: e.scalar_tensor_tensor(out=out, in0=in0, scalar=scalar, in1=in1, op0=op0, op1=op1, accum_out=accum), reads, writes, relaxed)
        return self.op("dve", lambda e: e.scalar_tensor_tensor(out=out, in0=in0, scalar=scalar, in1=in1, op0=op0, op1=op1), reads, writes)

    def red(self, eng, out, in_, op, reads, writes):
        return self.op(eng, lambda e: e.tensor_reduce(out=out, in_=in_, axis=AX.X, op=op), reads, writes)

    def memset(self, eng, ap, val, writes):
        return self.op(eng, lambda e: e.memset(ap, val), (), writes)

    def load(self, eng, out, in_, writes, reads=()):
        return self.dma(eng, lambda e: e.dma_start(out=out, in_=in_), reads, writes)

    def store(self, eng, out, in_, reads, final=False, writes=()):
        return self.dma(eng, lambda e: e.dma_start(out=out, in_=in_), reads, writes, final=final)


def dram_bcast(row, nparts):
    n = row.shape[-1]
    return bass.AP(row.tensor, row.offset, [[0, nparts], [1, n]])


def fap(ap, dims):
    return bass.AP(ap.tensor, ap.offset, [list(ap.ap[0])] + [list(d) for d in dims])


def rms_rstd(kb, x_ap, n, junk, ssq, rstd, reads, eng="act"):
    kb.act(junk[:, 0:n], x_ap, AF.Square, reads, [junk, ssq], accum=ssq[:, 0:1])
    kb.ts("dve", rstd[:, 0:1], ssq[:, 0:1], 1.0 / n, EPS, ALU.mult, ALU.add, [ssq], [rstd])
    kb.act(rstd[:, 0:1], rstd[:, 0:1], AF.Sqrt, [rstd], [rstd])
    kb.op("dve", lambda e: e.reciprocal(out=rstd[:, 0:1], in_=rstd[:, 0:1]), [rstd], [rstd])


class Peer:
    def __init__(self, kb, cst, NG=6):
        self.kb = kb
        self.cst = cst
        sb, ps = kb.sb, kb.ps
        self.G = sb("pe_G", [128, 1024], F32)
        self.Wq = sb("pe_Wq", [128, 8, 1024], BF16)
        self.KBD = sb("pe_KBD", [128, 256], F32)
        self.SK = sb("pe_SK", [128, 128], F32)
        self.junk = sb("pe_junk", [128, 1024], F32)
        self.junk2 = sb("pe_junk2", [128, 1024], F32)
        self.ssq = sb("pe_ssq", [128, 1], F32)
        self.rstd = sb("pe_rstd", [128, 1], F32)
        self.xn = sb("pe_xn", [128, 1024], F32)
        self.xnb = sb("pe_xnb", [128, 1024], BF16)
        self.xnT = sb("pe_xnT", [128, 8, 128], BF16)
        self.qT = sb("pe_qT", [128, 8, 128], F32)
        self.S = sb("pe_S", [128, 16, 128], F32)
        self.S2 = sb("pe_S2", [128, 16, 128], F32)
        self.V1 = sb("pe_V1", [128, 16, 16], F32)
        self.I1 = sb("pe_I1", [128, 16, 16], U32)
        self.I1f = sb("pe_I1f", [128, 16, 16], F32)
        self.CA = sb("pe_CA", [128, 8, 256], F32)
        self.CA2 = sb("pe_CA2", [128, 8, 256], F32)
        self.BV = sb("pe_BV", [128, 8, 16], F32)
        self.BP = sb("pe_BP", [128, 8, 16], U32)
        self.PA = sb("pe_PA", [128, 8, 16], U32)
        self.PB = sb("pe_PB", [128, 8, 16], U32)
        self.PAf = sb("pe_PAf", [128, 8, 16], F32)
        self.PBf = sb("pe_PBf", [128, 8, 16], F32)
        self.OH = sb("pe_OH", [128, 8, 16, 16], F32)
        self.SEL1 = sb("pe_SEL1", [128, 8, 16], F32)
        self.SEL2 = sb("pe_SEL2", [128, 8, 16], F32)
        self.IDXf = sb("pe_IDXf", [128, 128], F32)
        self.IDX = sb("pe_IDX", [128, 128], I32)
        self.GT = sb("pe_GT", [128, 8, 16], F32)
        self.Z = sb("pe_Z", [128, 8], F32)
        self.HD = sb("pe_HD", [128, 128], F32)
        self.W = sb("pe_W", [128, 128], F32)
        self.NG = NG
        self.gi = 0
        self.DG = [sb("pe_DG%d" % i, [128, 128], BF16) for i in range(4)]
        self.stg = [sb("pe_stg%d" % i, [128, 8, 1024], BF16) for i in range(2)]
        self.gb = []
        for i in range(2):
            flat = self.stg[i][:].rearrange("p r d -> p (r d)")
            for j in range(4):
                self.gb.append((T(None, "pe_gbv%d_%d" % (i, j)), flat[:, j * 2048:(j + 1) * 2048]))
        for i in range(4):
            t_ = sb("pe_gbx%d" % i, [128, 2048], BF16)
            self.gb.append((t_, t_[:]))
        self.NG = len(self.gb)

    def load_weights(self, g_ffn, w_query, sub_keys, u_tab, v_tab, ps_tr):
        kb = self.kb
        nc = kb.nc
        self.uv = nc.dram_tensor("pe_uv_bf", [16384, 2, 1024], BF16, kind="Internal").ap()
        ci = 0
        dv = self.uv.rearrange("(c p r) t d -> c p r t d", p=128, r=8)
        for ti, src in enumerate((u_tab, v_tab)):
            sv = src.rearrange("(c p r) d -> c p r d", p=128, r=8)
            for c in range(16):
                st = self.stg[ci % 2]
                ci += 1
                kb.dma("pool", (lambda st, a: lambda e: e.dma_start(out=st[:], in_=a, max_dma_last_dim=4096))(st, sv[c]), (), [st])
                kb.store("sp", dv[c][:, :, ti, :], st[:], [st])
        kb.dma_barrier("pool")
        kb.dma_barrier("pool")
        kb.load("sp", self.G[:], dram_bcast(g_ffn, 128), [self.G])
        wq = w_query.rearrange("(c p) n -> p c n", p=128)
        for c in range(8):
            kb.load("pool", self.Wq[:, c, :], wq[:, c, :], [self.Wq])
        kb.load("sp", self.SK[:].rearrange("p (c d) -> p c d", c=2), sub_keys.rearrange("c n d -> n c d"), [self.SK])
        kb.tr(ps_tr[:, 0:128], self.SK[:], self.cst.identf[:], [self.SK, self.cst.identf], [ps_tr])
        kb.memset("dve", self.KBD[:], 0.0, [self.KBD])
        kb.cp("dve", self.KBD[0:64, 0:128], ps_tr[0:64, 0:128], [ps_tr], [self.KBD])
        kb.cp("dve", self.KBD[64:128, 128:256], ps_tr[64:128, 0:128], [ps_tr], [self.KBD])

    def tile(self, h, psb):
        kb, c = self.kb, self.cst
        rms_rstd(kb, h[:], 1024, self.junk, self.ssq, self.rstd, [h])
        kb.stt(self.xn[:], h[:], self.rstd[:, 0:1], self.G[:], ALU.mult, ALU.mult, [h, self.rstd, self.G], [self.xn])
        kb.cp("pool", self.xnb[:], self.xn[:], [self.xn], [self.xnb])
        pt = psb[0]
        ptb = pt[:].bitcast(BF16)
        for ch in range(8):
            kb.tr(ptb[:, ch * 128:(ch + 1) * 128], self.xnb[:, ch * 128:(ch + 1) * 128], c.identb[:], [self.xnb, c.identb], [pt])
        kb.cp("act", self.xnT[:].rearrange("p c t -> p (c t)"), ptb, [pt], [self.xnT])
        for hh in range(8):
            bank = psb[1 + hh // 4]
            o = bank[:, (hh % 4) * 128:(hh % 4 + 1) * 128]
            for ch in range(8):
                kb.mm(o, self.Wq[:, ch, hh * 128:(hh + 1) * 128], self.xnT[:, ch, :], ch == 0, ch == 7, [self.Wq, self.xnT], [bank])
        kb.cp("act", self.qT[:, 0:4, :].rearrange("p h t -> p (h t)"), psb[1][:], [psb[1]], [self.qT])
        kb.cp("act", self.qT[:, 4:8, :].rearrange("p h t -> p (h t)"), psb[2][:], [psb[2]], [self.qT])
        for hh in range(8):
            bank = psb[3 + hh // 2]
            o = bank[:, (hh % 2) * 256:(hh % 2 + 1) * 256]
            kb.mm(o, self.qT[:, hh, :], self.KBD[:], True, True, [self.qT, self.KBD], [bank])
        for b4 in range(4):
            kb.cp("act" if b4 % 2 == 0 else "dve", self.S[:, b4 * 4:(b4 + 1) * 4, :].rearrange("p g n -> p (g n)"), psb[3 + b4][:], [psb[3 + b4]], [self.S])
        S, S2, V1, I1 = self.S, self.S2, self.V1, self.I1
        for g in range(16):
            kb.op("dve", (lambda g: lambda e: e.max(out=V1[:, g, 0:8], in_=S[:, g, :]))(g), [S], [V1])
        for g in range(16):
            kb.op("dve", (lambda g: lambda e: e.match_replace(out=S2[:, g, :], in_to_replace=V1[:, g, 0:8], in_values=S[:, g, :], imm_value=-1e30))(g), [S, V1], [S2])
        for g in range(16):
            kb.op("dve", (lambda g: lambda e: e.max(out=V1[:, g, 8:16], in_=S2[:, g, :]))(g), [S2], [V1])
        for g in range(16):
            kb.op("dve", (lambda g: lambda e: e.max_index(out=I1[:, g, 0:8], in_max=V1[:, g, 0:8], in_values=S[:, g, :]))(g), [S, V1], [I1])
            kb.op("dve", (lambda g: lambda e: e.max_index(out=I1[:, g, 8:16], in_max=V1[:, g, 8:16], in_values=S[:, g, :]))(g), [S, V1], [I1])
        kb.cp("dve", self.I1f[:], I1[:], [I1], [self.I1f])
        v1 = V1[:]
        in0 = fap(v1, [[32, 8], [1, 16], [0, 16]])
        in1 = fap(V1[:, 1:2, :], [[32, 8], [0, 16], [1, 16]])
        CA = self.CA
        kb.tt("dve", CA[:].rearrange("p h (a b) -> p h a b", a=16), in0, in1, ALU.add, [V1], [CA])
        CA2, BV, BP = self.CA2, self.BV, self.BP
        for hh in range(8):
            kb.op("dve", (lambda g: lambda e: e.max(out=BV[:, g, 0:8], in_=CA[:, g, :]))(hh), [CA], [BV])
        for hh in range(8):
            kb.op("dve", (lambda g: lambda e: e.match_replace(out=CA2[:, g, :], in_to_replace=BV[:, g, 0:8], in_values=CA[:, g, :], imm_value=-1e30))(hh), [CA, BV], [CA2])
        for hh in range(8):
            kb.op("dve", (lambda g: lambda e: e.max(out=BV[:, g, 8:16], in_=CA2[:, g, :]))(hh), [CA2], [BV])
        for hh in range(8):
            kb.op("dve", (lambda g: lambda e: e.max_index(out=BP[:, g, 0:8], in_max=BV[:, g, 0:8], in_values=CA[:, g, :]))(hh), [CA, BV], [BP])
            kb.op("dve", (lambda g: lambda e: e.max_index(out=BP[:, g, 8:16], in_max=BV[:, g, 8:16], in_values=CA[:, g, :]))(hh), [CA, BV], [BP])
        BPf = self.SEL1
        kb.cp("dve", BPf[:], BP[:], [BP], [BPf])
        OH = self.OH
        kb.tt("dve", OH[:], fap(BPf[:], [[16, 8], [1, 16], [0, 16]]), fap(c.thr16[:], [[0, 8], [0, 16], [1, 16]]), ALU.is_ge, [BPf, c.thr16], [OH])
        kb.red("dve", self.PAf[:], OH[:], ALU.add, [OH], [self.PAf])
        kb.stt(self.PBf[:].rearrange("p h k -> p (h k)"), self.PAf[:].rearrange("p h k -> p (h k)"), -16.0, BPf[:].rearrange("p h k -> p (h k)"),
               ALU.mult, ALU.add, [self.PAf, BPf], [self.PBf])
        OH = self.OH
        io = fap(c.iota16[:], [[0, 8], [0, 16], [1, 16]])
        for (Pf, SEL, off) in ((self.PAf, self.SEL1, 0), (self.PBf, self.SEL2, 16)):
            pfb = fap(Pf[:], [[16, 8], [1, 16], [0, 16]])
            kb.tt("dve", OH[:], pfb, io, ALU.is_equal, [Pf, c.iota16], [OH])
            i1b = fap(self.I1f[:, off // 16:off // 16 + 1, :], [[32, 8], [0, 16], [1, 16]])
            kb.tt("dve", OH[:], OH[:], i1b, ALU.mult, [OH, self.I1f], [OH])
            kb.red("dve", SEL[:], OH[:], ALU.add, [OH], [SEL])
        kb.stt(self.IDXf[:], self.SEL1[:].rearrange("p h k -> p (h k)"), 128.0, self.SEL2[:].rearrange("p h k -> p (h k)"),
               ALU.mult, ALU.add, [self.SEL1, self.SEL2], [self.IDXf])
        kb.cp("dve", self.IDX[:], self.IDXf[:], [self.IDXf], [self.IDX])
        GT, Z = self.GT, self.Z
        kb.tt("dve", GT[:], BV[:], fap(BV[:], [[16, 8], [0, 16]]), ALU.subtract, [BV], [GT])
        kb.act(GT[:], GT[:], AF.Exp, [GT], [GT])
        kb.red("dve", Z[:], GT[:], ALU.add, [GT], [Z])
        kb.op("dve", lambda e: e.reciprocal(out=Z[:], in_=Z[:]), [Z], [Z])
        kb.tt("dve", GT[:], GT[:], fap(Z[:], [[1, 8], [0, 16]]), ALU.mult, [GT, Z], [GT])
        HD, W, GT = self.HD, self.W, self.GT
        uvrows = self.uv.rearrange("e t d -> e (t d)")
        pa, pb = psb[1], psb[2]
        GS = 4
        gt2 = GT[:].rearrange("p h k -> p (h k)")
        for g in range(128 // GS):
            bufs = []
            for s in range(g * GS, (g + 1) * GS):
                gt_, gap = self.gb[self.gi % self.NG]
                self.gi += 1
                bufs.append((gt_, gap))
                kb.dma("pool", (lambda gap, s: lambda e: e.indirect_dma_start(out=gap, out_offset=None, in_=uvrows,
                       in_offset=bass.IndirectOffsetOnAxis(ap=self.IDX[:, s:s + 1], axis=0)))(gap, s), [self.IDX], [gt_])
                jk = self.junk if s % 2 == 0 else self.junk2
                kb.stt(jk[:], gap[:, 0:1024], 1.0, self.xn[:], ALU.mult, ALU.mult, [gt_, self.xn], [jk, HD], accum=HD[:, s:s + 1], relaxed=(HD,))
            sl = slice(g * GS, (g + 1) * GS)
            kb.act(W[:, sl], HD[:, sl], AF.Gelu, [HD], [W])
            kb.tt("dve", W[:, sl], W[:, sl], gt2[:, sl], ALU.mult, [W, GT], [W])
            for i, s in enumerate(range(g * GS, (g + 1) * GS)):
                gt_, gap = bufs[i]
                dg = self.DG[s % 4]
                kb.act(dg[:], c.identb[:], AF.Identity, [c.identb, W], [dg], scale=W[:, s:s + 1])
                kb.mm(pa[:, :], dg[:], gap[:, 1024:1536], s == 0, s == 127, [dg, gt_], [pa])
                kb.mm(pb[:, :], dg[:], gap[:, 1536:2048], s == 0, s == 127, [dg, gt_], [pb])
        kb.tt("dve", h[:, 0:512], h[:, 0:512], pa[:, :], ALU.add, [h, pa], [h])
        kb.tt("dve", h[:, 512:1024], h[:, 512:1024], pb[:, :], ALU.add, [h, pb], [h])


class Consts:
    def __init__(self, kb, cst_dram):
        self.identf = kb.sb("c_identf", [128, 128], F32)
        self.identb = kb.sb("c_identb", [128, 128], BF16)
        self.iota16 = kb.sb("c_iota16", [128, 16], F32)
        self.triuf = kb.sb("c_triuf", [128, 128], F32)
        self.triub = kb.sb("c_triub", [128, 128], BF16)
        self.ropec = kb.sb("c_ropec", [128, 64], F32)
        self.onesf = kb.sb("c_onesf", [128, 128], F32)
        self.onesb = kb.sb("c_onesb", [128, 128], BF16)
        kb.load("sp", self.identf[:], cst_dram[:, 0:128], [self.identf])
        kb.load("sp", self.iota16[:], cst_dram[:, 128:144], [self.iota16])
        kb.load("sp", self.triuf[:], cst_dram[:, 144:272], [self.triuf])
        kb.load("sp", self.ropec[:], cst_dram[:, 272:336], [self.ropec])
        kb.cp("dve", self.identb[:], self.identf[:], [self.identf], [self.identb])
        kb.cp("dve", self.triub[:], self.triuf[:], [self.triuf], [self.triub])
        kb.memset("dve", self.onesf[:], 1.0, [self.onesf])
        kb.memset("dve", self.onesb[:], 1.0, [self.onesb])
        self.thr16 = kb.sb("c_thr16", [128, 16], F32)
        kb.ts("dve", self.thr16[:], self.iota16[:], 1.0, 16.0, ALU.add, ALU.mult, [self.iota16], [self.thr16])


def host_consts():
    c = np.zeros((128, 336), np.float32)
    c[:, 0:128] = np.eye(128, dtype=np.float32)
    c[:, 128:144] = np.arange(16, dtype=np.float32)[None, :]
    c[:, 144:272] = np.triu(np.ones((128, 128), np.float32))
    inv = (10000.0 ** (-np.arange(16, dtype=np.float32) / 16)).astype(np.float32)
    c[:, 272:288] = inv[None, :]
    c[:, 288:304] = inv[None, :]
    c[:, 304:320] = np.float32(np.pi / 2)
    c[:, 320:336] = 0.0
    return c


def build_peer_only(ntiles, dbg=False):
    nc = bass.Bass("TRN2", target_bir_lowering=False)
    h_d = nc.dram_tensor("h", [ntiles * 128, 1024], F32, kind="ExternalInput").ap()
    g_d = nc.dram_tensor("g_ffn", [1, 1024], F32, kind="ExternalInput").ap()
    wq_d = nc.dram_tensor("w_query", [1024, 1024], F32, kind="ExternalInput").ap()
    sk_d = nc.dram_tensor("sub_keys", [2, 128, 64], F32, kind="ExternalInput").ap()
    u_d = nc.dram_tensor("u_tab", [16384, 1024], F32, kind="ExternalInput").ap()
    v_d = nc.dram_tensor("v_tab", [16384, 1024], F32, kind="ExternalInput").ap()
    c_d = nc.dram_tensor("cst", [128, 336], F32, kind="ExternalInput").ap()
    o_d = nc.dram_tensor("out", [ntiles * 128, 1024], F32, kind="ExternalOutput").ap()
    kb = KB(nc)
    cst = Consts(kb, c_d)
    psb = [kb.ps("psb%d" % i, [128, 512], F32) for i in range(8)]
    peer = Peer(kb, cst)
    if dbg:
        peer.dbg = {}
        for nm, w, dt in (("IDX", 128, I32), ("GT", 128, F32), ("HD", 128, F32), ("V1", 256, F32), ("I1", 256, U32), ("BV", 128, F32),
                          ("BP", 128, U32), ("xn", 1024, F32), ("S", 2048, F32), ("PAf", 128, F32), ("PBf", 128, F32), ("qT", 1024, F32), ("W", 128, F32)):
            peer.dbg[nm] = nc.dram_tensor("dbg_" + nm, [128, w], dt, kind="ExternalOutput").ap()
    peer.load_weights(g_d[0:1, :], wq_d, sk_d, u_d, v_d, psb[7])
    hb = [kb.sb("hb%d" % i, [128, 1024], F32) for i in range(2)]
    for t in range(ntiles):
        h = hb[t % 2]
        kb.load("sp", h[:], h_d[t * 128:(t + 1) * 128, :], [h])
        peer.tile(h, psb)
        kb.store("sp", o_d[t * 128:(t + 1) * 128, :], h[:], [h], final=True)
    kb.finish()
    return nc


def phase_A(kb, cst, psb, S, D):
    sb = kb.sb
    NT, NS = S // 128, S // 512
    TWO_PI = float(2 * np.pi)
    Wd = sb("a_Wd", [128, 8, 672], BF16); Wuq = sb("a_Wuq", [128, 3, 384], BF16); Wukv = sb("a_Wukv", [128, 2, 512], BF16)
    Gat = sb("a_Gat", [128, 1024], F32); Gq = sb("a_Gq", [128, 384], F32); Gkv = sb("a_Gkv", [128, 256], F32)
    Gqn = sb("a_Gqn", [128, 96], F32); Gkn = sb("a_Gkn", [128, 96], F32)
    wd = D["w_down"].rearrange("(c p) n -> p c n", p=128)
    for c in range(8):
        kb.load("pool", Wd[:, c, :], wd[:, c, :], [Wd])
    wq = D["w_uq"].rearrange("(c p) n -> p c n", p=128)
    for c in range(3):
        kb.load("pool", Wuq[:, c, :], wq[:, c, :], [Wuq])
    wk = D["w_ukv"].rearrange("(c p) n -> p c n", p=128)
    for c in range(2):
        kb.load("pool", Wukv[:, c, :], wk[:, c, :], [Wukv])
    for (g, src) in ((Gat, "g_attn"), (Gq, "g_q"), (Gkv, "g_kv"), (Gqn, "g_qn"), (Gkn, "g_kn")):
        kb.load("sp", g[:], dram_bcast(D[src], 128), [g])
    POS = sb("a_POS", [128, NT], I32); POSF = sb("a_POSF", [128, NT], F32)
    ANG = sb("a_ANG", [128, NT, 32], F32); KI = sb("a_KI", [128, NT, 32], I32); CS = sb("a_CS", [128, NT, 32], F32)
    kb.load("sp", POS[:], D["posT"], [POS])
    kb.cp("dve", POSF[:], POS[:], [POS], [POSF])
    rc = cst.ropec
    kb.tt("dve", ANG[:], fap(POSF[:], [[1, NT], [0, 32]]), fap(rc[:, 0:32], [[0, NT], [1, 32]]), ALU.mult, [POSF, rc], [ANG])
    kb.tt("dve", ANG[:], ANG[:], fap(rc[:, 32:64], [[0, NT], [1, 32]]), ALU.add, [ANG, rc], [ANG])
    A2 = ANG[:].rearrange("p t f -> p (t f)"); C2 = CS[:].rearrange("p t f -> p (t f)"); K2 = KI[:].rearrange("p t f -> p (t f)")
    kb.ts("dve", C2, A2, 1.0 / TWO_PI, None, ALU.mult, None, [ANG], [CS])
    kb.cp("dve", K2, C2, [CS], [KI])
    kb.cp("dve", C2, K2, [KI], [CS])
    kb.stt(A2, C2, -TWO_PI, A2, ALU.mult, ALU.add, [CS, ANG], [ANG])
    kb.ts("dve", C2, A2, float(np.pi), -TWO_PI, ALU.is_gt, ALU.mult, [ANG], [CS])
    kb.tt("dve", A2, A2, C2, ALU.add, [ANG, CS], [ANG])
    kb.ts("dve", C2, A2, -float(np.pi), TWO_PI, ALU.is_lt, ALU.mult, [ANG], [CS])
    kb.tt("dve", A2, A2, C2, ALU.add, [ANG, CS], [ANG])
    kb.act(C2, A2, AF.Sin, [ANG], [CS])

    xbuf = [sb("a_x%d" % i, [128, 1024], F32) for i in range(2)]
    junk = sb("a_junk", [128, 1024], F32)
    ssq = sb("a_ssq", [128, 1], F32); rstd = sb("a_rstd", [128, 1], F32)
    ssq2 = sb("a_ssq2", [128, 1], F32); rstd2 = sb("a_rstd2", [128, 1], F32)
    ssqr = sb("a_ssqr", [128, 1], F32)
    hnb = sb("a_hnb", [128, 1024], BF16); hnT = sb("a_hnT", [128, 8, 128], BF16)
    cqb = sb("a_cqb", [128, 384], BF16); ckb = sb("a_ckb", [128, 256], BF16); cT = sb("a_cT", [128, 5, 128], BF16)
    krr = sb("a_krr", [128, 32], F32); krg = sb("a_krg", [128, 32], F32); krot = sb("a_krot", [128, 32], F32)
    sq = sb("a_sq", [128, 512], F32)
    rq = sb("a_rq", [128, 4], F32); rk = sb("a_rk", [128, 4], F32)
    qn = sb("a_qn", [128, 4, 96], F32); kn = sb("a_kn", [128, 4, 64], F32)
    t1 = sb("a_t1", [128, 4, 16], F32); t2 = sb("a_t2", [128, 4, 16], F32); t3 = sb("a_t3", [128, 4, 16], F32); t4 = sb("a_t4", [128, 4, 16], F32)
    u1 = sb("a_u1", [128, 16], F32); u2 = sb("a_u2", [128, 16], F32); u3 = sb("a_u3", [128, 16], F32); u4 = sb("a_u4", [128, 16], F32)
    qb = sb("a_qb", [128, 4, 128], BF16); kbf = sb("a_kbf", [128, 4, 128], BF16)
    vb = [sb("a_vb%d" % i, [128, 4, 64], BF16) for i in range(2)]
    QTs = [sb("a_QTs%d" % i, [128, 512], BF16) for i in range(2)]
    KTs = [sb("a_KTs%d" % i, [128, 512], BF16) for i in range(2)]
    kb.memset("pool", qb[:], 0.0, [qb]); kb.memset("pool", kbf[:], 0.0, [kbf])
    identb = cst.identb
    QTd = D["QT_d"].rearrange("h d s -> d h s"); KTd = D["KT_d"].rearrange("h d s -> d h s")
    Vd = D["V_d"].rearrange("h p t d -> p h t d")

    def sqrt_recip(t_ap, tt_):
        kb.act(t_ap, t_ap, AF.Sqrt, [tt_], [tt_])
        kb.op("dve", lambda e: e.reciprocal(out=t_ap, in_=t_ap), [tt_], [tt_])

    for t in range(NT):
        xt = xbuf[t % 2]
        kb.load("sp", xt[:], D["x"][t * 128:(t + 1) * 128, :], [xt])
        rms_rstd(kb, xt[:], 1024, junk, ssq, rstd, [xt])
        kb.stt(hnb[:], xt[:], rstd[:, 0:1], Gat[:], ALU.mult, ALU.mult, [xt, rstd, Gat], [hnb])
        pt = psb[0]
        ptb = pt[:].bitcast(BF16)
        for c in range(8):
            kb.tr(ptb[:, c * 128:(c + 1) * 128], hnb[:, c * 128:(c + 1) * 128], identb[:], [hnb, identb], [pt])
        kb.cp("act", hnT[:].rearrange("p c t -> p (c t)"), ptb, [pt], [hnT])
        for c in range(8):
            kb.mm(psb[1][:, 0:384], hnT[:, c, :], Wd[:, c, 0:384], c == 0, c == 7, [hnT, Wd], [psb[1]])
        for c in range(8):
            kb.mm(psb[2][:, 0:288], hnT[:, c, :], Wd[:, c, 384:672], c == 0, c == 7, [hnT, Wd], [psb[2]])
        rms_rstd(kb, psb[1][:, 0:384], 384, junk, ssq, rstd, [psb[1]])
        kb.stt(cqb[:], psb[1][:, 0:384], rstd[:, 0:1], Gq[:], ALU.mult, ALU.mult, [psb[1], rstd, Gq], [cqb])
        rms_rstd(kb, psb[2][:, 0:256], 256, junk, ssq2, rstd2, [psb[2]])
        kb.stt(ckb[:], psb[2][:, 0:256], rstd2[:, 0:1], Gkv[:], ALU.mult, ALU.mult, [psb[2], rstd2, Gkv], [ckb])
        kb.cp("act", krr[:], psb[2][:, 256:288], [psb[2]], [krr])
        for c in range(3):
            kb.tr(ptb[:, c * 128:(c + 1) * 128], cqb[:, c * 128:(c + 1) * 128], identb[:], [cqb, identb], [pt])
        for c in range(2):
            kb.tr(ptb[:, (3 + c) * 128:(4 + c) * 128], ckb[:, c * 128:(c + 1) * 128], identb[:], [ckb, identb], [pt])
        kb.cp("act", cT[:].rearrange("p c t -> p (c t)"), ptb[:, 0:640], [pt], [cT])
        for c in range(3):
            kb.mm(psb[3][:, 0:384], cT[:, c, :], Wuq[:, c, :], c == 0, c == 2, [cT, Wuq], [psb[3]])
        for c in range(2):
            kb.mm(psb[4][:, 0:512], cT[:, 3 + c, :], Wukv[:, c, :], c == 0, c == 1, [cT, Wukv], [psb[4]])
        cosq = fap(CS[:, t, 0:16], [[0, 4], [1, 16]]); sinq = fap(CS[:, t, 16:32], [[0, 4], [1, 16]])
        q3 = psb[3][:, 0:384].rearrange("p (h d) -> p h d", h=4)
        kb.act(sq[:, 0:384], psb[3][:, 0:384], AF.Square, [psb[3]], [sq])
        kb.red("dve", rq[:, 0:4], sq[:, 0:384].rearrange("p (h d) -> p h d", h=4), ALU.add, [sq], [rq])
        kb.ts("dve", rq[:, 0:4], rq[:, 0:4], 1.0 / 96, EPS, ALU.mult, ALU.add, [rq], [rq])
        sqrt_recip(rq[:, 0:4], rq)
        kb.tt("dve", qn[:], q3, fap(rq[:, 0:4], [[1, 4], [0, 96]]), ALU.mult, [psb[3], rq], [qn])
        kb.tt("dve", qn[:], qn[:], fap(Gqn[:], [[0, 4], [1, 96]]), ALU.mult, [qn, Gqn], [qn])
        kb.tt("dve", t1[:], qn[:, :, 64:80], cosq, ALU.mult, [qn, CS], [t1])
        kb.tt("dve", t2[:], qn[:, :, 80:96], sinq, ALU.mult, [qn, CS], [t2])
        kb.tt("dve", t3[:], qn[:, :, 80:96], cosq, ALU.mult, [qn, CS], [t3])
        kb.tt("dve", t4[:], qn[:, :, 64:80], sinq, ALU.mult, [qn, CS], [t4])
        kb.cp("act", qb[:, :, 0:64], qn[:, :, 0:64], [qn], [qb])
        kb.tt("dve", qb[:, :, 64:80], t1[:], t2[:], ALU.subtract, [t1, t2], [qb])
        kb.tt("dve", qb[:, :, 80:96], t3[:], t4[:], ALU.add, [t3, t4], [qb])
        kv3 = psb[4][:, 0:512].rearrange("p (h d) -> p h d", h=4)
        kb.act(sq[:, 0:512], psb[4][:, 0:512], AF.Square, [psb[4]], [sq])
        kb.red("dve", rk[:, 0:4], sq[:, 0:512].rearrange("p (h d) -> p h d", h=4)[:, :, 0:64], ALU.add, [sq], [rk])
        kb.act(junk[:, 0:32], krr[:], AF.Square, [krr], [junk, ssqr], accum=ssqr[:, 0:1])
        kb.ts("dve", rk[:, 0:4], rk[:, 0:4], ssqr[:, 0:1], 1.0 / 96, ALU.add, ALU.mult, [rk, ssqr], [rk])
        kb.ts("dve", rk[:, 0:4], rk[:, 0:4], EPS, None, ALU.add, None, [rk], [rk])
        sqrt_recip(rk[:, 0:4], rk)
        kb.tt("dve", krg[:], krr[:], Gkn[:, 64:96], ALU.mult, [krr, Gkn], [krg])
        cs_, sn_ = CS[:, t, 0:16], CS[:, t, 16:32]
        kb.tt("dve", u1[:], krg[:, 0:16], cs_, ALU.mult, [krg, CS], [u1])
        kb.tt("dve", u2[:], krg[:, 16:32], sn_, ALU.mult, [krg, CS], [u2])
        kb.tt("dve", u3[:], krg[:, 16:32], cs_, ALU.mult, [krg, CS], [u3])
        kb.tt("dve", u4[:], krg[:, 0:16], sn_, ALU.mult, [krg, CS], [u4])
        kb.tt("dve", krot[:, 0:16], u1[:], u2[:], ALU.subtract, [u1, u2], [krot])
        kb.tt("dve", krot[:, 16:32], u3[:], u4[:], ALU.add, [u3, u4], [krot])
        kb.tt("dve", kn[:], kv3[:, :, 0:64], fap(rk[:, 0:4], [[1, 4], [0, 64]]), ALU.mult, [psb[4], rk], [kn])
        kb.tt("dve", kbf[:, :, 0:64], kn[:], fap(Gkn[:, 0:64], [[0, 4], [1, 64]]), ALU.mult, [kn, Gkn], [kbf])
        kb.tt("dve", kbf[:, :, 64:96], fap(krot[:], [[0, 4], [1, 32]]), fap(rk[:, 0:4], [[1, 4], [0, 32]]), ALU.mult, [krot, rk], [kbf])
        vbt = vb[t % 2]
        kb.cp("act", vbt[:], kv3[:, :, 64:128], [psb[4]], [vbt])
        kb.store("sp", Vd[:, :, t, :], vbt[:], [vbt])
        pq = psb[5]
        pqb = pq[:].bitcast(BF16)
        for h in range(4):
            kb.tr(pqb[:, h * 128:(h + 1) * 128], qb[:, h, :], identb[:], [qb, identb], [pq])
        for h in range(4):
            kb.tr(pqb[:, (4 + h) * 128:(5 + h) * 128], kbf[:, h, :], identb[:], [kbf, identb], [pq])
        qs, ks = QTs[t % 2], KTs[t % 2]
        kb.cp("act", qs[:, :], pqb[:, 0:512], [pq], [qs])
        kb.cp("act", ks[:, :], pqb[:, 512:1024], [pq], [ks])
        kb.store("sp", QTd[:, :, t * 128:(t + 1) * 128], qs[0:96, :].rearrange("p (h t) -> p h t", h=4), [qs])
        kb.store("sp", KTd[:, :, t * 128:(t + 1) * 128], ks[0:96, :].rearrange("p (h t) -> p h t", h=4), [ks])

    kb.dma_barrier("sp")
    KT = sb("a_KT", [96, S], BF16)
    VA = sb("a_VA", [128, NT, 128], BF16)
    QTb = [sb("a_QTb%d" % i, [96, 512], BF16) for i in range(2)]
    PT = [sb("a_PT%d" % i, [128, 512], BF16) for i in range(3)]
    osb = sb("a_osb", [128, 512], F32); rl = sb("a_rl", [128, 512], F32)
    oTt = [sb("a_oTt%d" % i, [64, 512], BF16) for i in range(2)]
    nbias = sb("a_nbias", [128, 1], F32)
    kb.memset("dve", nbias[:], -8.0, [nbias])
    kb.memset("pool", VA[:], 1.0, [VA])
    SPS = [psb[0], psb[1]]; OACC = [psb[2], psb[3]]; bcb = psb[4]
    scale = 96 ** -0.5
    ip = 0
    for h in range(4):
        KC = min(2048, S)
        for c in range(S // KC):
            kb.load("sp", KT[0:96, c * KC:(c + 1) * KC], D["KT_d"][h, :, c * KC:(c + 1) * KC], [KT])
        TC = min(32, NT)
        for c in range(NT // TC):
            kb.load("sp", VA[:, c * TC:(c + 1) * TC, 0:64], D["V_d"][h, :, c * TC:(c + 1) * TC, :], [VA])
        for p in range(NS):
            qt = QTb[p % 2]
            kb.load("sp", qt[0:96, :], D["QT_d"][h, :, p * 512:(p + 1) * 512], [qt])
            oacc = OACC[p % 2]
            nk = 4 * (p + 1)
            slots = {}

            def emit_S(ki):
                nonlocal ip
                r = ki - 4 * p
                c0 = 128 * r if r > 0 else 0
                sps = SPS[ip % 2]; pT = PT[ip % 3]; ip += 1
                slots[ki] = (r, c0, sps, pT)
                kb.mm(sps[:, c0:512], KT[0:96, ki * 128:(ki + 1) * 128], qt[0:96, c0:512], True, True, [KT, qt], [sps])

            emit_S(0)
            for ki in range(nk):
                if ki + 1 < nk:
                    emit_S(ki + 1)
                r, c0, sps, pT = slots.pop(ki)
                kb.act(pT[:, c0:512], sps[:, c0:512], AF.Exp, [sps, nbias], [pT], scale=scale, bias=nbias[:, 0:1])
                if r >= 0:
                    kb.tt("pool", pT[:, 128 * r:128 * (r + 1)], pT[:, 128 * r:128 * (r + 1)], cst.triub[:], ALU.mult, [pT, cst.triub], [pT])
                kb.mm(oacc[:, c0:512], VA[:, ki, :], pT[:, c0:512], ki == 0, ki == nk - 1, [VA, pT], [oacc])
            kb.cp("act", osb[:, :], oacc[:, :], [oacc], [osb])
            kb.op("dve", lambda e: e.reciprocal(out=rl[64:128, :], in_=osb[64:128, :]), [osb], [rl])
            kb.mm(bcb[0:64, :], cst.onesf[64:65, 0:64], rl[64:65, :], True, True, [cst.onesf, rl], [bcb])
            ot = oTt[p % 2]
            kb.tt("dve", ot[0:64, :], osb[0:64, :], bcb[0:64, :], ALU.mult, [osb, bcb], [ot])
            kb.store("sp", D["oT"][h * 64:(h + 1) * 64, p * 512:(p + 1) * 512], ot[0:64, :], [ot], final=True)


def phase_P(kb, cst, psb, NTOK, D, peer):
    sb = kb.sb
    Wo = sb("p_Wo", [128, 8, 1024], BF16)
    wo = D["w_o"].rearrange("(c p) n -> p c n", p=128)
    for c in range(8):
        kb.load("pool", Wo[:, c, :], wo[:, c, :], [Wo])
    otb = [sb("p_ot%d" % i, [128, 8, 128], BF16) for i in range(2)]
    hb = [sb("p_h%d" % i, [128, 1024], F32) for i in range(2)]
    oT = D["oT"].rearrange("(c p) s -> p c s", p=128)
    for t in range(NTOK // 128):
        ot, h = otb[t % 2], hb[t % 2]
        kb.load("sp", ot[:], oT[:, :, t * 128:(t + 1) * 128], [ot])
        kb.load("sp", h[:], D["resid"][t * 128:(t + 1) * 128, :], [h])
        for half in range(2):
            bank = psb[7 - half]
            for c in range(8):
                kb.mm(bank[:, :], ot[:, c, :], Wo[:, c, half * 512:(half + 1) * 512], c == 0, c == 7, [ot, Wo], [bank])
        kb.tt("dve", h[:, 0:512], h[:, 0:512], psb[7][:, :], ALU.add, [h, psb[7]], [h])
        kb.tt("dve", h[:, 512:1024], h[:, 512:1024], psb[6][:, :], ALU.add, [h, psb[6]], [h])
        peer.tile(h, psb)
        kb.store("sp", D["out"][t * 128:(t + 1) * 128, :], h[:], [h], final=True)


def phase_G(kb, cst, psb, S, D):
    sb = kb.sb
    NT = S // 128
    Win = sb("g_Win", [128, 8, 896], BF16)
    win = D["w_in"].rearrange("(c p) n -> p c n", p=128)
    for c in range(8):
        kb.load("pool", Win[:, c, :], win[:, c, :], [Win])
    Gat = sb("g_Gat", [128, 1024], F32); Gon = sb("g_Gon", [128, 256], F32)
    kb.load("sp", Gat[:], dram_bcast(D["g_attn"], 128), [Gat])
    kb.load("sp", Gon[:], dram_bcast(D["g_on"], 128), [Gon])
    Wg2 = sb("g_Wg2", [128, 128], BF16); bg = sb("g_bg", [128, 128], F32)
    kb.load("pool", Wg2[:], D["w_g2"], [Wg2])
    kb.load("sp", bg[:], dram_bcast(D["b_g"], 128), [bg])
    zb = sb("g_zb", [128, 128], F32)
    xbuf = [sb("g_x%d" % i, [128, 1024], F32) for i in range(2)]
    junk = sb("g_junk", [128, 1024], F32)
    ssq = sb("g_ssq", [128, 1], F32); rstd = sb("g_rstd", [128, 1], F32)
    hnb = sb("g_hnb", [128, 1024], BF16); hnT = sb("g_hnT", [128, 8, 128], BF16)
    glT = sb("g_glT", [128, 128], BF16)
    ez = sb("g_ez", [128, 128], F32); la = sb("g_la", [128, 128], F32)
    cs = sb("g_cs", [128, 128], F32); dd = sb("g_dd", [128, 128], F32)
    epos = sb("g_epos", [128, 128], F32); eneg = sb("g_eneg", [128, 128], F32); erel = sb("g_erel", [128, 128], F32)
    dec = sb("g_dec", [128, 1], F32)
    qd = sb("g_qd", [128, 128], BF16); ki = sb("g_ki", [128, 128], BF16); kd = sb("g_kd", [128, 128], BF16)
    qdT = sb("g_qdT", [128, 128], BF16); kiT = sb("g_kiT", [128, 128], BF16)
    vb = sb("g_vb", [128, 256], BF16)
    at = sb("g_at", [128, 128], BF16)
    St = sb("g_S", [128, 256], F32); Sb = sb("g_Sb", [128, 256], BF16)
    on = sb("g_onb", [128, 256], F32); sr = sb("g_sr", [128, 256], F32); og = sb("g_og", [128, 256], BF16)
    ogT = [sb("g_ogT%d" % i, [128, 2, 512], BF16) for i in range(2)]
    kb.memset("dve", St[:], 0.0, [St]); kb.memset("dve", Sb[:], 0.0, [Sb])
    identb = cst.identb
    oTd = D["oT"].rearrange("(c p) s -> p c s", p=128)
    for t in range(NT):
        xt = xbuf[t % 2]
        kb.load("sp", xt[:], D["x"][t * 128:(t + 1) * 128, :], [xt])
        rms_rstd(kb, xt[:], 1024, junk, ssq, rstd, [xt])
        kb.stt(hnb[:], xt[:], rstd[:, 0:1], Gat[:], ALU.mult, ALU.mult, [xt, rstd, Gat], [hnb])
        pt = psb[0]
        ptb = pt[:].bitcast(BF16)
        for c in range(8):
            kb.tr(ptb[:, c * 128:(c + 1) * 128], hnb[:, c * 128:(c + 1) * 128], identb[:], [hnb, identb], [pt])
        kb.cp("act", hnT[:].rearrange("p c t -> p (c t)"), ptb, [pt], [hnT])
        pA, pB, pC = psb[1], psb[2], psb[3]
        for c in range(8):
            kb.mm(pA[:, 0:512], hnT[:, c, :], Win[:, c, 0:512], c == 0, c == 7, [hnT, Win], [pA])
        for c in range(8):
            kb.mm(pB[:, 0:256], hnT[:, c, :], Win[:, c, 512:768], c == 0, c == 7, [hnT, Win], [pB])
        for c in range(8):
            kb.mm(pC[:, 0:128], Win[:, c, 768:896], hnT[:, c, :], c == 0, c == 7, [hnT, Win], [pC])
        kb.cp("act", glT[:], pC[:, 0:128], [pC], [glT])
        kb.mm(pC[:, 128:256], glT[:], Wg2[:], True, True, [glT, Wg2], [pC])
        kb.tt("dve", zb[:], pC[:, 128:256], bg[:], ALU.add, [pC, bg], [zb])
        kb.act(ez[:], zb[:], AF.Exp, [zb], [ez], scale=-1.0)
        kb.act(la[:], ez[:], AF.Ln, [ez], [la], bias=1.0)
        pD = psb[4]
        kb.mm(pD[:, 0:128], cst.triuf[:], la[:], True, True, [cst.triuf, la], [pD])
        kb.mm(pD[:, 128:256], cst.onesf[:], la[:], True, True, [cst.onesf, la], [pD])
        kb.mm(pD[:, 256:384], la[:], cst.onesf[:], True, True, [cst.onesf, la], [pD])
        kb.cp("dve", cs[:], pD[:, 0:128], [pD], [cs])
        kb.tt("dve", dd[:], pD[:, 128:256], cs[:], ALU.subtract, [pD, cs], [dd])
        kb.act(epos[:], cs[:], AF.Exp, [cs], [epos], scale=-1.0 / 16)
        kb.act(eneg[:], cs[:], AF.Exp, [cs], [eneg], scale=1.0 / 16)
        kb.act(erel[:], dd[:], AF.Exp, [dd], [erel], scale=-1.0 / 16)
        kb.act(dec[:, 0:1], pD[:, 256:257], AF.Exp, [pD], [dec], scale=-1.0 / 16)
        kb.stt(qd[:], pA[:, 0:128], 128 ** -0.5, epos[:], ALU.mult, ALU.mult, [pA, epos], [qd])
        kb.tt("dve", ki[:], pA[:, 128:256], eneg[:], ALU.mult, [pA, eneg], [ki])
        kb.tt("dve", kd[:], pA[:, 128:256], erel[:], ALU.mult, [pA, erel], [kd])
        kb.cp("act", vb[:], pA[:, 256:512], [pA], [vb])
        pE = psb[5]
        peb = pE[:].bitcast(BF16)
        kb.tr(peb[:, 0:128], qd[:], identb[:], [qd, identb], [pE])
        kb.tr(peb[:, 128:256], ki[:], identb[:], [ki, identb], [pE])
        kb.cp("act", qdT[:], peb[:, 0:128], [pE], [qdT])
        kb.cp("act", kiT[:], peb[:, 128:256], [pE], [kiT])
        pF = psb[6]
        kb.mm(pF[:, 0:128], kiT[:], qdT[:], True, True, [kiT, qdT], [pF])
        kb.tt("dve", at[:], pF[:, 0:128], cst.triuf[:], ALU.mult, [pF, cst.triuf], [at])
        kb.mm(pF[:, 256:512], at[:], vb[:], True, False, [at, vb], [pF])
        kb.mm(pF[:, 256:512], qdT[:], Sb[:], False, True, [qdT, Sb], [pF])
        pG = psb[7]
        kb.mm(pG[:, 0:256], kd[:], vb[:], True, True, [kd, vb], [pG])
        kb.stt(St[:], St[:], dec[:, 0:1], pG[:, 0:256], ALU.mult, ALU.add, [St, dec, pG], [St])
        kb.cp("act", Sb[:], St[:], [St], [Sb])
        rms_rstd(kb, pF[:, 256:512], 256, junk, ssq, rstd, [pF])
        kb.stt(on[:], pF[:, 256:512], rstd[:, 0:1], Gon[:], ALU.mult, ALU.mult, [pF, rstd, Gon], [on])
        kb.act(sr[:], pB[:, 0:256], AF.Silu, [pB], [sr])
        kb.tt("dve", og[:], on[:], sr[:], ALU.mult, [on, sr], [og])
        kb.tr(peb[:, 256:384], og[:, 0:128], identb[:], [og, identb], [pE])
        kb.tr(peb[:, 384:512], og[:, 128:256], identb[:], [og, identb], [pE])
        sp_, j = t // 4, t % 4
        ogs = ogT[sp_ % 2]
        kb.cp("act", ogs[:, 0, j * 128:(j + 1) * 128], peb[:, 256:384], [pE], [ogs])
        kb.cp("act", ogs[:, 1, j * 128:(j + 1) * 128], peb[:, 384:512], [pE], [ogs])
        if j == 3 or t == NT - 1:
            w = (j + 1) * 128
            kb.store("sp", oTd[:, :, sp_ * 512:sp_ * 512 + w], ogs[:, :, 0:w], [ogs], final=True)


def _psum_banks(kb):
    return [kb.ps("psb%d" % i, [128, 512], F32) for i in range(8)]


def build_A(S, debug=False):
    nc = bass.Bass("TRN2", target_bir_lowering=False)
    dt_ = lambda n, sh, dt, kind="ExternalInput": nc.dram_tensor(n, sh, dt, kind=kind).ap()
    D = {
        "x": dt_("x", [S, 1024], F32), "posT": dt_("posT", [128, S // 128], I32),
        "g_attn": dt_("g_attn", [1, 1024], F32), "w_down": dt_("w_down", [1024, 672], F32),
        "g_q": dt_("g_q", [1, 384], F32), "w_uq": dt_("w_uq", [384, 384], F32),
        "g_kv": dt_("g_kv", [1, 256], F32), "w_ukv": dt_("w_ukv", [256, 512], F32),
        "g_qn": dt_("g_qn", [1, 96], F32), "g_kn": dt_("g_kn", [1, 96], F32),
        "oT": dt_("oT", [256, S], BF16, "ExternalOutput"),
    }
    kind = "ExternalOutput" if debug else "Internal"
    D["QT_d"] = dt_("QT_d", [4, 96, S], BF16, kind)
    D["KT_d"] = dt_("KT_d", [4, 96, S], BF16, kind)
    D["V_d"] = dt_("V_d", [4, 128, S // 128, 64], BF16, kind)
    c_d = dt_("cst", [128, 336], F32)
    kb = KB(nc)
    cst = Consts(kb, c_d)
    psb = _psum_banks(kb)
    phase_A(kb, cst, psb, S, D)
    kb.finish()
    return nc


def build_P(NTOK):
    nc = bass.Bass("TRN2", target_bir_lowering=False)
    dt_ = lambda n, sh, dt, kind="ExternalInput": nc.dram_tensor(n, sh, dt, kind=kind).ap()
    D = {
        "oT": dt_("oT", [1024, NTOK], BF16), "resid": dt_("resid", [NTOK, 1024], F32), "w_o": dt_("w_o", [1024, 1024], F32),
        "out": dt_("out", [NTOK, 1024], F32, "ExternalOutput"),
    }
    g_d = dt_("g_ffn", [1, 1024], F32); wq_d = dt_("w_query", [1024, 1024], F32); sk_d = dt_("sub_keys", [2, 128, 64], F32)
    u_d = dt_("u_tab", [16384, 1024], F32); v_d = dt_("v_tab", [16384, 1024], F32)
    c_d = dt_("cst", [128, 336], F32)
    kb = KB(nc)
    cst = Consts(kb, c_d)
    psb = _psum_banks(kb)
    peer = Peer(kb, cst)
    peer.load_weights(g_d, wq_d, sk_d, u_d, v_d, psb[7])
    phase_P(kb, cst, psb, NTOK, D, peer)
    kb.finish()
    return nc


def build_G(S):
    nc = bass.Bass("TRN2", target_bir_lowering=False)
    dt_ = lambda n, sh, dt, kind="ExternalInput": nc.dram_tensor(n, sh, dt, kind=kind).ap()
    D = {
        "x": dt_("x", [S, 1024], F32), "g_attn": dt_("g_attn", [1, 1024], F32), "w_in": dt_("w_in", [1024, 896], F32),
        "w_g2": dt_("w_g2", [128, 128], F32), "b_g": dt_("b_g", [1, 128], F32), "g_on": dt_("g_on", [1, 256], F32),
        "oT": dt_("oT", [256, S], BF16, "ExternalOutput"),
    }
    c_d = dt_("cst", [128, 336], F32)
    kb = KB(nc)
    cst = Consts(kb, c_d)
    psb = _psum_banks(kb)
    phase_G(kb, cst, psb, S, D)
    kb.finish()
    return nc


def _c(a):
    return np.ascontiguousarray(a)


def inputs_A(x_b, pos_b, P, g):
    S = x_b.shape[0]
    hs = slice(4 * g, 4 * g + 4)
    w_uq = P["mla_w_uq"][0].reshape(384, 16, 96)[:, hs, :].reshape(384, 384)
    w_ukv = P["mla_w_ukv"][0].reshape(256, 16, 128)[:, hs, :].reshape(256, 512)
    return dict(x=_c(x_b), posT=_c(pos_b.reshape(S // 128, 128).T.astype(np.int32)),
                g_attn=_c(P["attn_norm_g"][0:1]), w_down=_c(P["mla_w_down"][0]), g_q=_c(P["mla_g_q_lat"][0:1]), w_uq=_c(w_uq),
                g_kv=_c(P["mla_g_kv_lat"][0:1]), w_ukv=_c(w_ukv), g_qn=_c(P["mla_g_qn"][0:1]), g_kn=_c(P["mla_g_kn"][0:1]),
                cst=host_consts())


def inputs_P(oT_tok, resid_tok, w_o, P, layer):
    return dict(oT=_c(oT_tok), resid=_c(resid_tok), w_o=_c(w_o), g_ffn=_c(P["ffn_norm_g"][layer:layer + 1]),
                w_query=_c(P["peer_w_query"][layer]), sub_keys=_c(P["peer_sub_keys"][layer]),
                u_tab=_c(P["peer_u"][layer]), v_tab=_c(P["peer_v"][layer]), cst=host_consts())


def inputs_G(h_b, P, hh):
    w = P["gla_w_in"][0]
    w_in = np.concatenate([w[:, 128 * hh:128 * (hh + 1)], w[:, 512 + 128 * hh:512 + 128 * (hh + 1)],
                           w[:, 1024 + 256 * hh:1024 + 256 * (hh + 1)], w[:, 2048 + 256 * hh:2048 + 256 * (hh + 1)], w[:, 3072:3088],
                           np.zeros((1024, 112), np.float32)], axis=1)
    wg2 = np.zeros((128, 128), np.float32)
    wg2[0:16] = P["gla_w_g2"][0][:, 128 * hh:128 * (hh + 1)]
    return dict(x=_c(h_b), g_attn=_c(P["attn_norm_g"][1:2]), w_in=_c(w_in), w_g2=wg2,
                b_g=_c(P["gla_b_g"][0:1, 128 * hh:128 * (hh + 1)]), g_on=_c(P["gla_g_on"][0:1]), cst=host_consts())


def kernel(**inp):
    P = {k: np.asarray(v) for k, v in inp.items()}
    x = P["x"]
    B, S, _ = x.shape
    ncore = 4 * B
    ids = list(range(ncore))
    TOK = S // 4
    ncA = build_A(S)
    ims = [inputs_A(x[b], P["positions"][b], P, g) for b in range(B) for g in range(4)]
    res = run_bass_kernel_spmd(ncA, ims, core_ids=ids)
    oT = [np.concatenate([np.asarray(res.results[b * 4 + g]["oT"]) for g in range(4)], axis=0) for b in range(B)]
    ncP = build_P(TOK)
    ims = [inputs_P(oT[b][:, j * TOK:(j + 1) * TOK], x[b, j * TOK:(j + 1) * TOK], P["mla_w_o"][0], P, 0) for b in range(B) for j in range(4)]
    res = run_bass_kernel_spmd(ncP, ims, core_ids=ids)
    h1 = np.stack([np.concatenate([np.asarray(res.results[b * 4 + j]["out"]) for j in range(4)], axis=0) for b in range(B)])
    ncG = build_G(S)
    ims = [inputs_G(h1[b], P, hh) for b in range(B) for hh in range(4)]
    res = run_bass_kernel_spmd(ncG, ims, core_ids=ids)
    gT = [np.concatenate([np.asarray(res.results[b * 4 + hh]["oT"]) for hh in range(4)], axis=0) for b in range(B)]
    ncP2 = build_P(TOK)
    ims = [inputs_P(gT[b][:, j * TOK:(j + 1) * TOK], h1[b, j * TOK:(j + 1) * TOK], P["gla_w_o"][0], P, 1) for b in range(B) for j in range(4)]
    res = run_bass_kernel_spmd(ncP2, ims, core_ids=ids)
    out = np.stack([np.concatenate([np.asarray(res.results[b * 4 + j]["out"]) for j in range(4)], axis=0) for b in range(B)])
    return out.astype(np.float32)
```

```python
from contextlib import ExitStack
import numpy as np
import ml_dtypes
import concourse.bass as bass
import concourse.mybir as mybir
from concourse.bass_utils import run_bass_kernel_spmd

AF = mybir.ActivationFunctionType
ALU = mybir.AluOpType
AX = mybir.AxisListType
F32, BF16, I32, U32 = mybir.dt.float32, mybir.dt.bfloat16, mybir.dt.int32, mybir.dt.uint32

ENGS = ("pe", "act", "dve", "pool", "sp")
EPS = 1e-6


class T:
    __slots__ = ("h", "w", "r", "name", "psum")

    def __init__(self, h, name="", psum=False):
        self.h = h
        self.w = None
        self.r = {}
        self.name = name
        self.psum = psum

    def __getitem__(self, idx):
        return self.h[idx]


class KB:
    def __init__(self, nc, n_dma_sems=32):
        self.nc = nc
        self.es = ExitStack()
        self.prog = {e: [] for e in ENGS}
        self.sems = {}
        self.cnt = {}
        for e in ENGS:
            self.sems[e] = self.es.enter_context(nc.semaphore("s_" + e))
            self.cnt[e] = 0
        self.dsems = []
        self.dq = {"sp": [], "pool": [], "act": []}
        for q, n in (("sp", n_dma_sems), ("pool", 16)):
            for i in range(n):
                k = "d%s%d" % (q, i)
                self.sems[k] = self.es.enter_context(nc.semaphore("s_" + k))
                self.cnt[k] = 0
                self.dsems.append(k)
                self.dq[q].append(k)
        self.dnext = {"sp": 0, "pool": 0}
        self.seen = {e: {} for e in ENGS}
        self.final = []
        self.pending = {e: [] for e in ENGS}

    def sb(self, name, shape, dt):
        h = self.es.enter_context(self.nc.sbuf_tensor(name, list(shape), dt))
        return T(h, name)

    def ps(self, name, shape, dt):
        h = self.es.enter_context(self.nc.psum_tensor(name, list(shape), dt))
        return T(h, name, psum=True)

    def _waits(self, eng, reads, writes, relaxed=()):
        deps = {}

        def add(k, v):
            if v > deps.get(k, 0):
                deps[k] = v
        for t in reads:
            if t.w is not None:
                k, v = t.w
                if not (k == eng and eng == "pe"):
                    add(k, v)
            if t.psum:
                for k, v in t.r.items():
                    if k != eng:
                        add(k, v)
        for t in writes:
            same_ok = (eng == "pe") or any(t is r for r in relaxed)
            if t.w is not None:
                k, v = t.w
                if k != eng or not same_ok:
                    add(k, v)
            for k, v in t.r.items():
                if k != eng or not same_ok:
                    add(k, v)
        out = []
        seen = self.seen[eng]
        for k, v in deps.items():
            if seen.get(k, 0) >= v:
                continue
            seen[k] = v
            out.append((k, v))
        return out

    def _commit(self, done, reads, writes):
        k, v = done
        for t in writes:
            t.w = done
            t.r = {}
        for t in reads:
            if t.r.get(k, 0) < v:
                t.r[k] = v

    def dma_barrier(self, eng):
        for k in self.dsems:
            v = self.cnt[k]
            if v > 0 and self.seen[eng].get(k, 0) < v:
                self.seen[eng][k] = v
                self.pending[eng].append((k, v))

    def op(self, eng, fn, reads=(), writes=(), relaxed=()):
        waits = self.pending[eng] + self._waits(eng, reads, writes, relaxed)
        self.pending[eng] = []
        self.cnt[eng] += 1
        done = (eng, self.cnt[eng])
        self.prog[eng].append((waits, fn, (eng, 1)))
        self._commit(done, reads, writes)
        return done

    def dma(self, eng, fn, reads=(), writes=(), final=False):
        k = self.dq[eng][self.dnext[eng]]
        self.dnext[eng] = (self.dnext[eng] + 1) % len(self.dq[eng])
        waits = self.pending[eng] + self._waits(eng, reads, writes)
        self.pending[eng] = []
        prev = self.cnt[k]
        if prev > 0 and self.seen[eng].get(k, 0) < prev:
            self.seen[eng][k] = prev
            waits.append((k, prev))
        self.cnt[k] += 16
        done = (k, self.cnt[k])
        self.prog[eng].append((waits, fn, (k, 16)))
        self._commit(done, reads, writes)
        if final:
            self.final.append(done)
        return done

    def finish(self):
        nc = self.nc
        fw = [(k, self.cnt[k]) for k in self.dsems if self.cnt[k] > 0]
        sems = self.sems
        prog = self.prog

        def replay(name):
            def f(eng):
                for waits, fn, inc in prog[name]:
                    for k, v in waits:
                        eng.wait_ge(sems[k], v)
                    ins = fn(eng)
                    ins.then_inc(sems[inc[0]], inc[1])
                if name == "sp":
                    for k, v in fw:
                        eng.wait_ge(sems[k], v)
            return f
        with nc.Block() as block:
            block.tensor(replay("pe"))
            block.scalar(replay("act"))
            block.vector(replay("dve"))
            block.gpsimd(replay("pool"))
            block.sync(replay("sp"))
        self.es.close()

    def mm(self, out, lhsT, rhs, start, stop, reads, writes):
        return self.op("pe", lambda e: e.matmul(out, lhsT=lhsT, rhs=rhs, start=start, stop=stop), reads, writes)

    def tr(self, out, in_, ident, reads, writes):
        return self.op("pe", lambda e: e.transpose(out, in_, ident), reads, writes)

    def act(self, out, in_, func, reads, writes, bias=None, scale=None, accum=None):
        kw = {}
        if bias is not None:
            kw["bias"] = bias
        if scale is not None:
            kw["scale"] = scale
        if accum is not None:
            kw["accum_out"] = accum
        return self.op("act", lambda e: e.activation(out=out, in_=in_, func=func, **kw), reads, writes)

    def cp(self, eng, out, in_, reads, writes):
        if eng == "act":
            return self.op("act", lambda e: e.copy(out=out, in_=in_), reads, writes)
        return self.op(eng, lambda e: e.tensor_copy(out=out, in_=in_), reads, writes)

    def tt(self, eng, out, in0, in1, op, reads, writes):
        return self.op(eng, lambda e: e.tensor_tensor(out=out, in0=in0, in1=in1, op=op), reads, writes)

    def ts(self, eng, out, in0, s1, s2, op0, op1, reads, writes, accum=None):
        if op1 is None:
            return self.op(eng, lambda e: e.tensor_scalar(out=out, in0=in0, scalar1=s1, scalar2=None, op0=op0), reads, writes)
        if accum is not None:
            return self.op(eng, lambda e: e.tensor_scalar(out=out, in0=in0, scalar1=s1, scalar2=s2, op0=op0, op1=op1, accum_out=accum), reads, writes)
        return self.op(eng, lambda e: e.tensor_scalar(out=out, in0=in0, scalar1=s1, scalar2=s2, op0=op0, op1=op1), reads, writes)

    def stt(self, out, in0, scalar, in1, op0, op1, reads, writes, accum=None, relaxed=()):
        if accum is not None:
            return self.op("dve", lambda e: e.scalar_tensor_tensor(out=out, in0=in0, scalar=scalar, in1=in1, op0=op0, op1=op1, accum_out=accum), reads, writes, relaxed)
        return self.op("dve", lambda e: e.scalar_tensor_tensor(out=out, in0=in0, scalar=scalar, in1=in1, op0=op0, op1=op1), reads, writes)

    def red(self, eng, out, in_, op, reads, writes):
        return self.op(eng, lambda e: e.tensor_reduce(out=out, in_=in_, axis=AX.X, op=op), reads, writes)

    def memset(self, eng, ap, val, writes):
        return self.op(eng, lambda e: e.memset(ap, val), (), writes)

    def load(self, eng, out, in_, writes, reads=()):
        return self.dma(eng, lambda e: e.dma_start(out=out, in_=in_), reads, writes)

    def store(self, eng, out, in_, reads, final=False, writes=()):
        return self.dma(eng, lambda e: e.dma_start(out=out, in_=in_), reads, writes, final=final)


def dram_bcast(row, nparts):
    n = row.shape[-1]
    return bass.AP(row.tensor, row.offset, [[0, nparts], [1, n]])


def fap(ap, dims):
    return bass.AP(ap.tensor, ap.offset, [list(ap.ap[0])] + [list(d) for d in dims])


def rms_rstd(kb, x_ap, n, junk, ssq, rstd, reads, eng="act"):
    kb.act(junk[:, 0:n], x_ap, AF.Square, reads, [junk, ssq], accum=ssq[:, 0:1])
    kb.ts("dve", rstd[:, 0:1], ssq[:, 0:1], 1.0 / n, EPS, ALU.mult, ALU.add, [ssq], [rstd])
    kb.act(rstd[:, 0:1], rstd[:, 0:1], AF.Sqrt, [rstd], [rstd])
    kb.op("dve", lambda e: e.reciprocal(out=rstd[:, 0:1], in_=rstd[:, 0:1]), [rstd], [rstd])


class Peer:
    def __init__(self, kb, cst, NG=6):
        self.kb = kb
        self.cst = cst
        sb, ps = kb.sb, kb.ps
        self.G = sb("pe_G", [128, 1024], F32)
        self.Wq = sb("pe_Wq", [128, 8, 1024], BF16)
        self.KBD = sb("pe_KBD", [128, 256], F32)
        self.SK = sb("pe_SK", [128, 128], F32)
        self.junk = sb("pe_junk", [128, 1024], F32)
        self.junk2 = sb("pe_junk2", [128, 1024], F32)
        self.ssq = sb("pe_ssq", [128, 1], F32)
        self.rstd = sb("pe_rstd", [128, 1], F32)
        self.xn = sb("pe_xn", [128, 1024], F32)
        self.xnb = sb("pe_xnb", [128, 1024], BF16)
        self.xnT = sb("pe_xnT", [128, 8, 128], BF16)
        self.qT = sb("pe_qT", [128, 8, 128], F32)
        self.S = sb("pe_S", [128, 16, 128], F32)
        self.S2 = sb("pe_S2", [128, 16, 128], F32)
        self.V1 = sb("pe_V1", [128, 16, 16], F32)
        self.I1 = sb("pe_I1", [128, 16, 16], U32)
        self.I1f = sb("pe_I1f", [128, 16, 16], F32)
        self.CA = sb("pe_CA", [128, 8, 256], F32)
        self.CA2 = sb("pe_CA2", [128, 8, 256], F32)
        self.BV = sb("pe_BV", [128, 8, 16], F32)
        self.BP = sb("pe_BP", [128, 8, 16], U32)
        self.PA = sb("pe_PA", [128, 8, 16], U32)
        self.PB = sb("pe_PB", [128, 8, 16], U32)
        self.PAf = sb("pe_PAf", [128, 8, 16], F32)
        self.PBf = sb("pe_PBf", [128, 8, 16], F32)
        self.OH = sb("pe_OH", [128, 8, 16, 16], F32)
        self.SEL1 = sb("pe_SEL1", [128, 8, 16], F32)
        self.SEL2 = sb("pe_SEL2", [128, 8, 16], F32)
        self.IDXf = sb("pe_IDXf", [128, 128], F32)
        self.IDX = sb("pe_IDX", [128, 128], I32)
        self.GT = sb("pe_GT", [128, 8, 16], F32)
        self.Z = sb("pe_Z", [128, 8], F32)
        self.HD = sb("pe_HD", [128, 128], F32)
        self.W = sb("pe_W", [128, 128], F32)
        self.NG = NG
        self.gi = 0
        self.DG = [sb("pe_DG%d" % i, [128, 128], BF16) for i in range(4)]
        self.stg = [sb("pe_stg%d" % i, [128, 8, 1024], BF16) for i in range(2)]
        self.gb = []
        for i in range(2):
            flat = self.stg[i][:].rearrange("p r d -> p (r d)")
            for j in range(4):
                self.gb.append((T(None, "pe_gbv%d_%d" % (i, j)), flat[:, j * 2048:(j + 1) * 2048]))
        for i in range(4):
            t_ = sb("pe_gbx%d" % i, [128, 2048], BF16)
            self.gb.append((t_, t_[:]))
        self.NG = len(self.gb)

    def load_weights(self, g_ffn, w_query, sub_keys, u_tab, v_tab, ps_tr):
        kb = self.kb
        nc = kb.nc
        self.uv = nc.dram_tensor("pe_uv_bf", [16384, 2, 1024], BF16, kind="Internal").ap()
        ci = 0
        dv = self.uv.rearrange("(c p r) t d -> c p r t d", p=128, r=8)
        for ti, src in enumerate((u_tab, v_tab)):
            sv = src.rearrange("(c p r) d -> c p r d", p=128, r=8)
            for c in range(16):
                st = self.stg[ci % 2]
                ci += 1
                kb.dma("pool", (lambda st, a: lambda e: e.dma_start(out=st[:], in_=a, max_dma_last_dim=4096))(st, sv[c]), (), [st])
                kb.store("sp", dv[c][:, :, ti, :], st[:], [st])
        kb.dma_barrier("pool")
        kb.dma_barrier("pool")
        kb.load("sp", self.G[:], dram_bcast(g_ffn, 128), [self.G])
        wq = w_query.rearrange("(c p) n -> p c n", p=128)
        for c in range(8):
            kb.load("pool", self.Wq[:, c, :], wq[:, c, :], [self.Wq])
        kb.load("sp", self.SK[:].rearrange("p (c d) -> p c d", c=2), sub_keys.rearrange("c n d -> n c d"), [self.SK])
        kb.tr(ps_tr[:, 0:128], self.SK[:], self.cst.identf[:], [self.SK, self.cst.identf], [ps_tr])
        kb.memset("dve", self.KBD[:], 0.0, [self.KBD])
        kb.cp("dve", self.KBD[0:64, 0:128], ps_tr[0:64, 0:128], [ps_tr], [self.KBD])
        kb.cp("dve", self.KBD[64:128, 128:256], ps_tr[64:128, 0:128], [ps_tr], [self.KBD])

    def tile(self, h, psb):
        kb, c = self.kb, self.cst
        rms_rstd(kb, h[:], 1024, self.junk, self.ssq, self.rstd, [h])
        kb.stt(self.xn[:], h[:], self.rstd[:, 0:1], self.G[:], ALU.mult, ALU.mult, [h, self.rstd, self.G], [self.xn])
        kb.cp("pool", self.xnb[:], self.xn[:], [self.xn], [self.xnb])
        pt = psb[0]
        ptb = pt[:].bitcast(BF16)
        for ch in range(8):
            kb.tr(ptb[:, ch * 128:(ch + 1) * 128], self.xnb[:, ch * 128:(ch + 1) * 128], c.identb[:], [self.xnb, c.identb], [pt])
        kb.cp("act", self.xnT[:].rearrange("p c t -> p (c t)"), ptb, [pt], [self.xnT])
        for hh in range(8):
            bank = psb[1 + hh // 4]
            o = bank[:, (hh % 4) * 128:(hh % 4 + 1) * 128]
            for ch in range(8):
                kb.mm(o, self.Wq[:, ch, hh * 128:(hh + 1) * 128], self.xnT[:, ch, :], ch == 0, ch == 7, [self.Wq, self.xnT], [bank])
        kb.cp("act", self.qT[:, 0:4, :].rearrange("p h t -> p (h t)"), psb[1][:], [psb[1]], [self.qT])
        kb.cp("act", self.qT[:, 4:8, :].rearrange("p h t -> p (h t)"), psb[2][:], [psb[2]], [self.qT])
        for hh in range(8):
            bank = psb[3 + hh // 2]
            o = bank[:, (hh % 2) * 256:(hh % 2 + 1) * 256]
            kb.mm(o, self.qT[:, hh, :], self.KBD[:], True, True, [self.qT, self.KBD], [bank])
        for b4 in range(4):
            kb.cp("act" if b4 % 2 == 0 else "dve", self.S[:, b4 * 4:(b4 + 1) * 4, :].rearrange("p g n -> p (g n)"), psb[3 + b4][:], [psb[3 + b4]], [self.S])
        S, S2, V1, I1 = self.S, self.S2, self.V1, self.I1
        for g in range(16):
            kb.op("dve", (lambda g: lambda e: e.max(out=V1[:, g, 0:8], in_=S[:, g, :]))(g), [S], [V1], relaxed=(V1,))
        for g in range(16):
            kb.op("dve", (lambda g: lambda e: e.match_replace(out=S2[:, g, :], in_to_replace=V1[:, g, 0:8], in_values=S[:, g, :], imm_value=-1e30))(g), [S, V1], [S2], relaxed=(S2,))
        for g in range(16):
            kb.op("dve", (lambda g: lambda e: e.max(out=V1[:, g, 8:16], in_=S2[:, g, :]))(g), [S2], [V1], relaxed=(V1,))
        for g in range(16):
            kb.op("dve", (lambda g: lambda e: e.max_index(out=I1[:, g, 0:8], in_max=V1[:, g, 0:8], in_values=S[:, g, :]))(g), [S, V1], [I1], relaxed=(I1,))
            kb.op("dve", (lambda g: lambda e: e.max_index(out=I1[:, g, 8:16], in_max=V1[:, g, 8:16], in_values=S[:, g, :]))(g), [S, V1], [I1], relaxed=(I1,))
        kb.cp("dve", self.I1f[:], I1[:], [I1], [self.I1f])
        v1 = V1[:]
        in0 = fap(v1, [[32, 8], [1, 16], [0, 16]])
        in1 = fap(V1[:, 1:2, :], [[32, 8], [0, 16], [1, 16]])
        CA = self.CA
        kb.tt("dve", CA[:].rearrange("p h (a b) -> p h a b", a=16), in0, in1, ALU.add, [V1], [CA])
        CA2, BV, BP = self.CA2, self.BV, self.BP
        for hh in range(8):
            kb.op("dve", (lambda g: lambda e: e.max(out=BV[:, g, 0:8], in_=CA[:, g, :]))(hh), [CA], [BV], relaxed=(BV,))
        for hh in range(8):
            kb.op("dve", (lambda g: lambda e: e.match_replace(out=CA2[:, g, :], in_to_replace=BV[:, g, 0:8], in_values=CA[:, g, :], imm_value=-1e30))(hh), [CA, BV], [CA2], relaxed=(CA2,))
        for hh in range(8):
            kb.op("dve", (lambda g: lambda e: e.max(out=BV[:, g, 8:16], in_=CA2[:, g, :]))(hh), [CA2], [BV], relaxed=(BV,))
        for hh in range(8):
            kb.op("dve", (lambda g: lambda e: e.max_index(out=BP[:, g, 0:8], in_max=BV[:, g, 0:8], in_values=CA[:, g, :]))(hh), [CA, BV], [BP], relaxed=(BP,))
            kb.op("dve", (lambda g: lambda e: e.max_index(out=BP[:, g, 8:16], in_max=BV[:, g, 8:16], in_values=CA[:, g, :]))(hh), [CA, BV], [BP], relaxed=(BP,))
        BPf = self.SEL1
        kb.cp("dve", BPf[:], BP[:], [BP], [BPf])
        OH = self.OH
        kb.tt("dve", OH[:], fap(BPf[:], [[16, 8], [1, 16], [0, 16]]), fap(c.thr16[:], [[0, 8], [0, 16], [1, 16]]), ALU.is_ge, [BPf, c.thr16], [OH])
        kb.red("dve", self.PAf[:], OH[:], ALU.add, [OH], [self.PAf])
        kb.stt(self.PBf[:].rearrange("p h k -> p (h k)"), self.PAf[:].rearrange("p h k -> p (h k)"), -16.0, BPf[:].rearrange("p h k -> p (h k)"),
               ALU.mult, ALU.add, [self.PAf, BPf], [self.PBf])
        OH = self.OH
        io = fap(c.iota16[:], [[0, 8], [0, 16], [1, 16]])
        for (Pf, SEL, off) in ((self.PAf, self.SEL1, 0), (self.PBf, self.SEL2, 16)):
            pfb = fap(Pf[:], [[16, 8], [1, 16], [0, 16]])
            kb.tt("dve", OH[:], pfb, io, ALU.is_equal, [Pf, c.iota16], [OH])
            i1b = fap(self.I1f[:, off // 16:off // 16 + 1, :], [[32, 8], [0, 16], [1, 16]])
            kb.tt("dve", OH[:], OH[:], i1b, ALU.mult, [OH, self.I1f], [OH])
            kb.red("dve", SEL[:], OH[:], ALU.add, [OH], [SEL])
        kb.stt(self.IDXf[:], self.SEL1[:].rearrange("p h k -> p (h k)"), 128.0, self.SEL2[:].rearrange("p h k -> p (h k)"),
               ALU.mult, ALU.add, [self.SEL1, self.SEL2], [self.IDXf])
        kb.cp("dve", self.IDX[:], self.IDXf[:], [self.IDXf], [self.IDX])
        GT, Z = self.GT, self.Z
        kb.tt("dve", GT[:], BV[:], fap(BV[:], [[16, 8], [0, 16]]), ALU.subtract, [BV], [GT])
        kb.act(GT[:], GT[:], AF.Exp, [GT], [GT])
        kb.red("dve", Z[:], GT[:], ALU.add, [GT], [Z])
        kb.op("dve", lambda e: e.reciprocal(out=Z[:], in_=Z[:]), [Z], [Z])
        kb.tt("dve", GT[:], GT[:], fap(Z[:], [[1, 8], [0, 16]]), ALU.mult, [GT, Z], [GT])
        HD, W, GT = self.HD, self.W, self.GT
        uvrows = self.uv.rearrange("e t d -> e (t d)")
        pa, pb = psb[1], psb[2]
        GS = 4
        gt2 = GT[:].rearrange("p h k -> p (h k)")
        for g in range(128 // GS):
            bufs = []
            for s in range(g * GS, (g + 1) * GS):
                gt_, gap = self.gb[self.gi % self.NG]
                self.gi += 1
                bufs.append((gt_, gap))
                kb.dma("pool", (lambda gap, s: lambda e: e.indirect_dma_start(out=gap, out_offset=None, in_=uvrows,
                       in_offset=bass.IndirectOffsetOnAxis(ap=self.IDX[:, s:s + 1], axis=0)))(gap, s), [self.IDX], [gt_])
                jk = self.junk if s % 2 == 0 else self.junk2
                kb.stt(jk[:], gap[:, 0:1024], 1.0, self.xn[:], ALU.mult, ALU.mult, [gt_, self.xn], [jk, HD], accum=HD[:, s:s + 1], relaxed=(HD,))
            sl = slice(g * GS, (g + 1) * GS)
            kb.act(W[:, sl], HD[:, sl], AF.Gelu, [HD], [W])
            kb.tt("dve", W[:, sl], W[:, sl], gt2[:, sl], ALU.mult, [W, GT], [W])
            for i, s in enumerate(range(g * GS, (g + 1) * GS)):
                gt_, gap = bufs[i]
                dg = self.DG[s % 4]
                kb.act(dg[:], c.identb[:], AF.Identity, [c.identb, W], [dg], scale=W[:, s:s + 1])
                kb.mm(pa[:, :], dg[:], gap[:, 1024:1536], s == 0, s == 127, [dg, gt_], [pa])
                kb.mm(pb[:, :], dg[:], gap[:, 1536:2048], s == 0, s == 127, [dg, gt_], [pb])
        kb.tt("dve", h[:, 0:512], h[:, 0:512], pa[:, :], ALU.add, [h, pa], [h])
        kb.tt("dve", h[:, 512:1024], h[:, 512:1024], pb[:, :], ALU.add, [h, pb], [h])


class Consts:
    def __init__(self, kb, cst_dram):
        self.identf = kb.sb("c_identf", [128, 128], F32)
        self.identb = kb.sb("c_identb", [128, 128], BF16)
        self.iota16 = kb.sb("c_iota16", [128, 16], F32)
        self.triuf = kb.sb("c_triuf", [128, 128], F32)
        self.triub = kb.sb("c_triub", [128, 128], BF16)
        self.ropec = kb.sb("c_ropec", [128, 64], F32)
        self.onesf = kb.sb("c_onesf", [128, 128], F32)
        self.onesb = kb.sb("c_onesb", [128, 128], BF16)
        kb.load("sp", self.identf[:], cst_dram[:, 0:128], [self.identf])
        kb.load("sp", self.iota16[:], cst_dram[:, 128:144], [self.iota16])
        kb.load("sp", self.triuf[:], cst_dram[:, 144:272], [self.triuf])
        kb.load("sp", self.ropec[:], cst_dram[:, 272:336], [self.ropec])
        kb.cp("dve", self.identb[:], self.identf[:], [self.identf], [self.identb])
        kb.cp("dve", self.triub[:], self.triuf[:], [self.triuf], [self.triub])
        kb.memset("dve", self.onesf[:], 1.0, [self.onesf])
        kb.memset("dve", self.onesb[:], 1.0, [self.onesb])
        self.thr16 = kb.sb("c_thr16", [128, 16], F32)
        kb.ts("dve", self.thr16[:], self.iota16[:], 1.0, 16.0, ALU.add, ALU.mult, [self.iota16], [self.thr16])


def host_consts():
    c = np.zeros((128, 336), np.float32)
    c[:, 0:128] = np.eye(128, dtype=np.float32)
    c[:, 128:144] = np.arange(16, dtype=np.float32)[None, :]
    c[:, 144:272] = np.triu(np.ones((128, 128), np.float32))
    inv = (10000.0 ** (-np.arange(16, dtype=np.float32) / 16)).astype(np.float32)
    c[:, 272:288] = inv[None, :]
    c[:, 288:304] = inv[None, :]
    c[:, 304:320] = np.float32(np.pi / 2)
    c[:, 320:336] = 0.0
    return c


def build_peer_only(ntiles, dbg=False):
    nc = bass.Bass("TRN2", target_bir_lowering=False)
    h_d = nc.dram_tensor("h", [ntiles * 128, 1024], F32, kind="ExternalInput").ap()
    g_d = nc.dram_tensor("g_ffn", [1, 1024], F32, kind="ExternalInput").ap()
    wq_d = nc.dram_tensor("w_query", [1024, 1024], F32, kind="ExternalInput").ap()
    sk_d = nc.dram_tensor("sub_keys", [2, 128, 64], F32, kind="ExternalInput").ap()
    u_d = nc.dram_tensor("u_tab", [16384, 1024], F32, kind="ExternalInput").ap()
    v_d = nc.dram_tensor("v_tab", [16384, 1024], F32, kind="ExternalInput").ap()
    c_d = nc.dram_tensor("cst", [128, 336], F32, kind="ExternalInput").ap()
    o_d = nc.dram_tensor("out", [ntiles * 128, 1024], F32, kind="ExternalOutput").ap()
    kb = KB(nc)
    cst = Consts(kb, c_d)
    psb = [kb.ps("psb%d" % i, [128, 512], F32) for i in range(8)]
    peer = Peer(kb, cst)
    if dbg:
        peer.dbg = {}
        for nm, w, dt in (("IDX", 128, I32), ("GT", 128, F32), ("HD", 128, F32), ("V1", 256, F32), ("I1", 256, U32), ("BV", 128, F32),
                          ("BP", 128, U32), ("xn", 1024, F32), ("S", 2048, F32), ("PAf", 128, F32), ("PBf", 128, F32), ("qT", 1024, F32), ("W", 128, F32)):
            peer.dbg[nm] = nc.dram_tensor("dbg_" + nm, [128, w], dt, kind="ExternalOutput").ap()
    peer.load_weights(g_d[0:1, :], wq_d, sk_d, u_d, v_d, psb[7])
    hb = [kb.sb("hb%d" % i, [128, 1024], F32) for i in range(2)]
    for t in range(ntiles):
        h = hb[t % 2]
        kb.load("sp", h[:], h_d[t * 128:(t + 1) * 128, :], [h])
        peer.tile(h, psb)
        kb.store("sp", o_d[t * 128:(t + 1) * 128, :], h[:], [h], final=True)
    kb.finish()
    return nc


def phase_A(kb, cst, psb, S, D):
    sb = kb.sb
    NT, NS = S // 128, S // 512
    TWO_PI = float(2 * np.pi)
    Wd = sb("a_Wd", [128, 8, 672], BF16); Wuq = sb("a_Wuq", [128, 3, 384], BF16); Wukv = sb("a_Wukv", [128, 2, 512], BF16)
    Gat = sb("a_Gat", [128, 1024], F32); Gq = sb("a_Gq", [128, 384], F32); Gkv = sb("a_Gkv", [128, 256], F32)
    Gqn = sb("a_Gqn", [128, 96], F32); Gkn = sb("a_Gkn", [128, 96], F32)
    wd = D["w_down"].rearrange("(c p) n -> p c n", p=128)
    for c in range(8):
        kb.load("pool", Wd[:, c, :], wd[:, c, :], [Wd])
    wq = D["w_uq"].rearrange("(c p) n -> p c n", p=128)
    for c in range(3):
        kb.load("pool", Wuq[:, c, :], wq[:, c, :], [Wuq])
    wk = D["w_ukv"].rearrange("(c p) n -> p c n", p=128)
    for c in range(2):
        kb.load("pool", Wukv[:, c, :], wk[:, c, :], [Wukv])
    for (g, src) in ((Gat, "g_attn"), (Gq, "g_q"), (Gkv, "g_kv"), (Gqn, "g_qn"), (Gkn, "g_kn")):
        kb.load("sp", g[:], dram_bcast(D[src], 128), [g])
    POS = sb("a_POS", [128, NT], I32); POSF = sb("a_POSF", [128, NT], F32)
    ANG = sb("a_ANG", [128, NT, 32], F32); KI = sb("a_KI", [128, NT, 32], I32); CS = sb("a_CS", [128, NT, 32], F32)
    kb.load("sp", POS[:], D["posT"], [POS])
    kb.cp("dve", POSF[:], POS[:], [POS], [POSF])
    rc = cst.ropec
    kb.tt("dve", ANG[:], fap(POSF[:], [[1, NT], [0, 32]]), fap(rc[:, 0:32], [[0, NT], [1, 32]]), ALU.mult, [POSF, rc], [ANG])
    kb.tt("dve", ANG[:], ANG[:], fap(rc[:, 32:64], [[0, NT], [1, 32]]), ALU.add, [ANG, rc], [ANG])
    A2 = ANG[:].rearrange("p t f -> p (t f)"); C2 = CS[:].rearrange("p t f -> p (t f)"); K2 = KI[:].rearrange("p t f -> p (t f)")
    kb.ts("dve", C2, A2, 1.0 / TWO_PI, None, ALU.mult, None, [ANG], [CS])
    kb.cp("dve", K2, C2, [CS], [KI])
    kb.cp("dve", C2, K2, [KI], [CS])
    kb.stt(A2, C2, -TWO_PI, A2, ALU.mult, ALU.add, [CS, ANG], [ANG])
    kb.ts("dve", C2, A2, float(np.pi), -TWO_PI, ALU.is_gt, ALU.mult, [ANG], [CS])
    kb.tt("dve", A2, A2, C2, ALU.add, [ANG, CS], [ANG])
    kb.ts("dve", C2, A2, -float(np.pi), TWO_PI, ALU.is_lt, ALU.mult, [ANG], [CS])
    kb.tt("dve", A2, A2, C2, ALU.add, [ANG, CS], [ANG])
    kb.act(C2, A2, AF.Sin, [ANG], [CS])

    xbuf = [sb("a_x%d" % i, [128, 1024], F32) for i in range(2)]
    junk = sb("a_junk", [128, 1024], F32)
    ssq = sb("a_ssq", [128, 1], F32); rstd = sb("a_rstd", [128, 1], F32)
    ssq2 = sb("a_ssq2", [128, 1], F32); rstd2 = sb("a_rstd2", [128, 1], F32)
    ssqr = sb("a_ssqr", [128, 1], F32)
    hnb = sb("a_hnb", [128, 1024], BF16); hnT = sb("a_hnT", [128, 8, 128], BF16)
    cqb = sb("a_cqb", [128, 384], BF16); ckb = sb("a_ckb", [128, 256], BF16); cT = sb("a_cT", [128, 5, 128], BF16)
    krr = sb("a_krr", [128, 32], F32); krg = sb("a_krg", [128, 32], F32); krot = sb("a_krot", [128, 32], F32)
    sq = sb("a_sq", [128, 512], F32)
    rq = sb("a_rq", [128, 4], F32); rk = sb("a_rk", [128, 4], F32)
    qn = sb("a_qn", [128, 4, 96], F32); kn = sb("a_kn", [128, 4, 64], F32)
    t1 = sb("a_t1", [128, 4, 16], F32); t2 = sb("a_t2", [128, 4, 16], F32); t3 = sb("a_t3", [128, 4, 16], F32); t4 = sb("a_t4", [128, 4, 16], F32)
    u1 = sb("a_u1", [128, 16], F32); u2 = sb("a_u2", [128, 16], F32); u3 = sb("a_u3", [128, 16], F32); u4 = sb("a_u4", [128, 16], F32)
    qb = sb("a_qb", [128, 4, 128], BF16); kbf = sb("a_kbf", [128, 4, 128], BF16)
    vb = [sb("a_vb%d" % i, [128, 4, 64], BF16) for i in range(2)]
    QTs = [sb("a_QTs%d" % i, [128, 512], BF16) for i in range(2)]
    KTs = [sb("a_KTs%d" % i, [128, 512], BF16) for i in range(2)]
    kb.memset("pool", qb[:], 0.0, [qb]); kb.memset("pool", kbf[:], 0.0, [kbf])
    identb = cst.identb
    QTd = D["QT_d"].rearrange("h d s -> d h s"); KTd = D["KT_d"].rearrange("h d s -> d h s")
    Vd = D["V_d"].rearrange("h p t d -> p h t d")

    def sqrt_recip(t_ap, tt_):
        kb.act(t_ap, t_ap, AF.Sqrt, [tt_], [tt_])
        kb.op("dve", lambda e: e.reciprocal(out=t_ap, in_=t_ap), [tt_], [tt_])

    for t in range(NT):
        xt = xbuf[t % 2]
        kb.load("sp", xt[:], D["x"][t * 128:(t + 1) * 128, :], [xt])
        rms_rstd(kb, xt[:], 1024, junk, ssq, rstd, [xt])
        kb.stt(hnb[:], xt[:], rstd[:, 0:1], Gat[:], ALU.mult, ALU.mult, [xt, rstd, Gat], [hnb])
        pt = psb[0]
        ptb = pt[:].bitcast(BF16)
        for c in range(8):
            kb.tr(ptb[:, c * 128:(c + 1) * 128], hnb[:, c * 128:(c + 1) * 128], identb[:], [hnb, identb], [pt])
        kb.cp("act", hnT[:].rearrange("p c t -> p (c t)"), ptb, [pt], [hnT])
        for c in range(8):
            kb.mm(psb[1][:, 0:384], hnT[:, c, :], Wd[:, c, 0:384], c == 0, c == 7, [hnT, Wd], [psb[1]])
        for c in range(8):
            kb.mm(psb[2][:, 0:288], hnT[:, c, :], Wd[:, c, 384:672], c == 0, c == 7, [hnT, Wd], [psb[2]])
        rms_rstd(kb, psb[1][:, 0:384], 384, junk, ssq, rstd, [psb[1]])
        kb.stt(cqb[:], psb[1][:, 0:384], rstd[:, 0:1], Gq[:], ALU.mult, ALU.mult, [psb[1], rstd, Gq], [cqb])
        rms_rstd(kb, psb[2][:, 0:256], 256, junk, ssq2, rstd2, [psb[2]])
        kb.stt(ckb[:], psb[2][:, 0:256], rstd2[:, 0:1], Gkv[:], ALU.mult, ALU.mult, [psb[2], rstd2, Gkv], [ckb])
        kb.cp("act", krr[:], psb[2][:, 256:288], [psb[2]], [krr])
        for c in range(3):
            kb.tr(ptb[:, c * 128:(c + 1) * 128], cqb[:, c * 128:(c + 1) * 128], identb[:], [cqb, identb], [pt])
        for c in range(2):
            kb.tr(ptb[:, (3 + c) * 128:(4 + c) * 128], ckb[:, c * 128:(c + 1) * 128], identb[:], [ckb, identb], [pt])
        kb.cp("act", cT[:].rearrange("p c t -> p (c t)"), ptb[:, 0:640], [pt], [cT])
        for c in range(3):
            kb.mm(psb[3][:, 0:384], cT[:, c, :], Wuq[:, c, :], c == 0, c == 2, [cT, Wuq], [psb[3]])
        for c in range(2):
            kb.mm(psb[4][:, 0:512], cT[:, 3 + c, :], Wukv[:, c, :], c == 0, c == 1, [cT, Wukv], [psb[4]])
        cosq = fap(CS[:, t, 0:16], [[0, 4], [1, 16]]); sinq = fap(CS[:, t, 16:32], [[0, 4], [1, 16]])
        q3 = psb[3][:, 0:384].rearrange("p (h d) -> p h d", h=4)
        kb.act(sq[:, 0:384], psb[3][:, 0:384], AF.Square, [psb[3]], [sq])
        kb.red("dve", rq[:, 0:4], sq[:, 0:384].rearrange("p (h d) -> p h d", h=4), ALU.add, [sq], [rq])
        kb.ts("dve", rq[:, 0:4], rq[:, 0:4], 1.0 / 96, EPS, ALU.mult, ALU.add, [rq], [rq])
        sqrt_recip(rq[:, 0:4], rq)
        kb.tt("dve", qn[:], q3, fap(rq[:, 0:4], [[1, 4], [0, 96]]), ALU.mult, [psb[3], rq], [qn])
        kb.tt("dve", qn[:], qn[:], fap(Gqn[:], [[0, 4], [1, 96]]), ALU.mult, [qn, Gqn], [qn])
        kb.tt("dve", t1[:], qn[:, :, 64:80], cosq, ALU.mult, [qn, CS], [t1])
        kb.tt("dve", t2[:], qn[:, :, 80:96], sinq, ALU.mult, [qn, CS], [t2])
        kb.tt("dve", t3[:], qn[:, :, 80:96], cosq, ALU.mult, [qn, CS], [t3])
        kb.tt("dve", t4[:], qn[:, :, 64:80], sinq, ALU.mult, [qn, CS], [t4])
        kb.cp("act", qb[:, :, 0:64], qn[:, :, 0:64], [qn], [qb])
        kb.tt("dve", qb[:, :, 64:80], t1[:], t2[:], ALU.subtract, [t1, t2], [qb])
        kb.tt("dve", qb[:, :, 80:96], t3[:], t4[:], ALU.add, [t3, t4], [qb])
        kv3 = psb[4][:, 0:512].rearrange("p (h d) -> p h d", h=4)
        kb.act(sq[:, 0:512], psb[4][:, 0:512], AF.Square, [psb[4]], [sq])
        kb.red("dve", rk[:, 0:4], sq[:, 0:512].rearrange("p (h d) -> p h d", h=4)[:, :, 0:64], ALU.add, [sq], [rk])
        kb.act(junk[:, 0:32], krr[:], AF.Square, [krr], [junk, ssqr], accum=ssqr[:, 0:1])
        kb.ts("dve", rk[:, 0:4], rk[:, 0:4], ssqr[:, 0:1], 1.0 / 96, ALU.add, ALU.mult, [rk, ssqr], [rk])
        kb.ts("dve", rk[:, 0:4], rk[:, 0:4], EPS, None, ALU.add, None, [rk], [rk])
        sqrt_recip(rk[:, 0:4], rk)
        kb.tt("dve", krg[:], krr[:], Gkn[:, 64:96], ALU.mult, [krr, Gkn], [krg])
        cs_, sn_ = CS[:, t, 0:16], CS[:, t, 16:32]
        kb.tt("dve", u1[:], krg[:, 0:16], cs_, ALU.mult, [krg, CS], [u1])
        kb.tt("dve", u2[:], krg[:, 16:32], sn_, ALU.mult, [krg, CS], [u2])
        kb.tt("dve", u3[:], krg[:, 16:32], cs_, ALU.mult, [krg, CS], [u3])
        kb.tt("dve", u4[:], krg[:, 0:16], sn_, ALU.mult, [krg, CS], [u4])
        kb.tt("dve", krot[:, 0:16], u1[:], u2[:], ALU.subtract, [u1, u2], [krot])
        kb.tt("dve", krot[:, 16:32], u3[:], u4[:], ALU.add, [u3, u4], [krot])
        kb.tt("dve", kn[:], kv3[:, :, 0:64], fap(rk[:, 0:4], [[1, 4], [0, 64]]), ALU.mult, [psb[4], rk], [kn])
        kb.tt("dve", kbf[:, :, 0:64], kn[:], fap(Gkn[:, 0:64], [[0, 4], [1, 64]]), ALU.mult, [kn, Gkn], [kbf])
        kb.tt("dve", kbf[:, :, 64:96], fap(krot[:], [[0, 4], [1, 32]]), fap(rk[:, 0:4], [[1, 4], [0, 32]]), ALU.mult, [krot, rk], [kbf])
        vbt = vb[t % 2]
        kb.cp("act", vbt[:], kv3[:, :, 64:128], [psb[4]], [vbt])
        kb.store("sp", Vd[:, :, t, :], vbt[:], [vbt])
        pq = psb[5]
        pqb = pq[:].bitcast(BF16)
        for h in range(4):
            kb.tr(pqb[:, h * 128:(h + 1) * 128], qb[:, h, :], identb[:], [qb, identb], [pq])
        for h in range(4):
            kb.tr(pqb[:, (4 + h) * 128:(5 + h) * 128], kbf[:, h, :], identb[:], [kbf, identb], [pq])
        qs, ks = QTs[t % 2], KTs[t % 2]
        kb.cp("act", qs[:, :], pqb[:, 0:512], [pq], [qs])
        kb.cp("act", ks[:, :], pqb[:, 512:1024], [pq], [ks])
        kb.store("sp", QTd[:, :, t * 128:(t + 1) * 128], qs[0:96, :].rearrange("p (h t) -> p h t", h=4), [qs])
        kb.store("sp", KTd[:, :, t * 128:(t + 1) * 128], ks[0:96, :].rearrange("p (h t) -> p h t", h=4), [ks])

    kb.dma_barrier("sp")
    KT = sb("a_KT", [96, S], BF16)
    VA = sb("a_VA", [128, NT, 128], BF16)
    QTb = [sb("a_QTb%d" % i, [96, 512], BF16) for i in range(2)]
    PT = [sb("a_PT%d" % i, [128, 512], BF16) for i in range(3)]
    osb = sb("a_osb", [128, 512], F32); rl = sb("a_rl", [128, 512], F32)
    oTt = [sb("a_oTt%d" % i, [64, 512], BF16) for i in range(2)]
    nbias = sb("a_nbias", [128, 1], F32)
    kb.memset("dve", nbias[:], -8.0, [nbias])
    kb.memset("pool", VA[:], 1.0, [VA])
    SPS = [psb[0], psb[1]]; OACC = [psb[2], psb[3]]; bcb = psb[4]
    scale = 96 ** -0.5
    ip = 0
    for h in range(4):
        KC = min(2048, S)
        for c in range(S // KC):
            kb.load("sp", KT[0:96, c * KC:(c + 1) * KC], D["KT_d"][h, :, c * KC:(c + 1) * KC], [KT])
        TC = min(32, NT)
        for c in range(NT // TC):
            kb.load("sp", VA[:, c * TC:(c + 1) * TC, 0:64], D["V_d"][h, :, c * TC:(c + 1) * TC, :], [VA])
        for p in range(NS):
            qt = QTb[p % 2]
            kb.load("sp", qt[0:96, :], D["QT_d"][h, :, p * 512:(p + 1) * 512], [qt])
            oacc = OACC[p % 2]
            nk = 4 * (p + 1)
            slots = {}

            def emit_S(ki):
                nonlocal ip
                r = ki - 4 * p
                c0 = 128 * r if r > 0 else 0
                sps = SPS[ip % 2]; pT = PT[ip % 3]; ip += 1
                slots[ki] = (r, c0, sps, pT)
                kb.mm(sps[:, c0:512], KT[0:96, ki * 128:(ki + 1) * 128], qt[0:96, c0:512], True, True, [KT, qt], [sps])

            emit_S(0)
            for ki in range(nk):
                if ki + 1 < nk:
                    emit_S(ki + 1)
                r, c0, sps, pT = slots.pop(ki)
                kb.act(pT[:, c0:512], sps[:, c0:512], AF.Exp, [sps, nbias], [pT], scale=scale, bias=nbias[:, 0:1])
                if r >= 0:
                    kb.tt("pool", pT[:, 128 * r:128 * (r + 1)], pT[:, 128 * r:128 * (r + 1)], cst.triub[:], ALU.mult, [pT, cst.triub], [pT])
                kb.mm(oacc[:, c0:512], VA[:, ki, :], pT[:, c0:512], ki == 0, ki == nk - 1, [VA, pT], [oacc])
            kb.cp("act", osb[:, :], oacc[:, :], [oacc], [osb])
            kb.op("dve", lambda e: e.reciprocal(out=rl[64:128, :], in_=osb[64:128, :]), [osb], [rl])
            kb.mm(bcb[0:64, :], cst.onesf[64:65, 0:64], rl[64:65, :], True, True, [cst.onesf, rl], [bcb])
            ot = oTt[p % 2]
            kb.tt("dve", ot[0:64, :], osb[0:64, :], bcb[0:64, :], ALU.mult, [osb, bcb], [ot])
            kb.store("sp", D["oT"][h * 64:(h + 1) * 64, p * 512:(p + 1) * 512], ot[0:64, :], [ot], final=True)


def phase_P(kb, cst, psb, NTOK, D, peer):
    sb = kb.sb
    Wo = sb("p_Wo", [128, 8, 1024], BF16)
    wo = D["w_o"].rearrange("(c p) n -> p c n", p=128)
    for c in range(8):
        kb.load("pool", Wo[:, c, :], wo[:, c, :], [Wo])
    otb = [sb("p_ot%d" % i, [128, 8, 128], BF16) for i in range(2)]
    hb = [sb("p_h%d" % i, [128, 1024], F32) for i in range(2)]
    oT = D["oT"].rearrange("(c p) s -> p c s", p=128)
    for t in range(NTOK // 128):
        ot, h = otb[t % 2], hb[t % 2]
        kb.load("sp", ot[:], oT[:, :, t * 128:(t + 1) * 128], [ot])
        kb.load("sp", h[:], D["resid"][t * 128:(t + 1) * 128, :], [h])
        for half in range(2):
            bank = psb[7 - half]
            for c in range(8):
                kb.mm(bank[:, :], ot[:, c, :], Wo[:, c, half * 512:(half + 1) * 512], c == 0, c == 7, [ot, Wo], [bank])
        kb.tt("dve", h[:, 0:512], h[:, 0:512], psb[7][:, :], ALU.add, [h, psb[7]], [h])
        kb.tt("dve", h[:, 512:1024], h[:, 512:1024], psb[6][:, :], ALU.add, [h, psb[6]], [h])
        peer.tile(h, psb)
        kb.store("sp", D["out"][t * 128:(t + 1) * 128, :], h[:], [h], final=True)


def phase_G(kb, cst, psb, S, D):
    sb = kb.sb
    NT = S // 128
    Win = sb("g_Win", [128, 8, 896], BF16)
    win = D["w_in"].rearrange("(c p) n -> p c n", p=128)
    for c in range(8):
        kb.load("pool", Win[:, c, :], win[:, c, :], [Win])
    Gat = sb("g_Gat", [128, 1024], F32); Gon = sb("g_Gon", [128, 256], F32)
    kb.load("sp", Gat[:], dram_bcast(D["g_attn"], 128), [Gat])
    kb.load("sp", Gon[:], dram_bcast(D["g_on"], 128), [Gon])
    Wg2 = sb("g_Wg2", [128, 128], BF16); bg = sb("g_bg", [128, 128], F32)
    kb.load("pool", Wg2[:], D["w_g2"], [Wg2])
    kb.load("sp", bg[:], dram_bcast(D["b_g"], 128), [bg])
    zb = sb("g_zb", [128, 128], F32)
    xbuf = [sb("g_x%d" % i, [128, 1024], F32) for i in range(2)]
    junk = sb("g_junk", [128, 1024], F32)
    ssq = sb("g_ssq", [128, 1], F32); rstd = sb("g_rstd", [128, 1], F32)
    hnb = sb("g_hnb", [128, 1024], BF16); hnT = sb("g_hnT", [128, 8, 128], BF16)
    glT = sb("g_glT", [128, 128], BF16)
    ez = sb("g_ez", [128, 128], F32); la = sb("g_la", [128, 128], F32)
    cs = sb("g_cs", [128, 128], F32); dd = sb("g_dd", [128, 128], F32)
    epos = sb("g_epos", [128, 128], F32); eneg = sb("g_eneg", [128, 128], F32); erel = sb("g_erel", [128, 128], F32)
    dec = sb("g_dec", [128, 1], F32)
    qd = sb("g_qd", [128, 128], BF16); ki = sb("g_ki", [128, 128], BF16); kd = sb("g_kd", [128, 128], BF16)
    qdT = sb("g_qdT", [128, 128], BF16); kiT = sb("g_kiT", [128, 128], BF16)
    vb = sb("g_vb", [128, 256], BF16)
    at = sb("g_at", [128, 128], BF16)
    St = sb("g_S", [128, 256], F32); Sb = sb("g_Sb", [128, 256], BF16)
    on = sb("g_onb", [128, 256], F32); sr = sb("g_sr", [128, 256], F32); og = sb("g_og", [128, 256], BF16)
    ogT = [sb("g_ogT%d" % i, [128, 2, 512], BF16) for i in range(2)]
    kb.memset("dve", St[:], 0.0, [St]); kb.memset("dve", Sb[:], 0.0, [Sb])
    identb = cst.identb
    oTd = D["oT"].rearrange("(c p) s -> p c s", p=128)
    for t in range(NT):
        xt = xbuf[t % 2]
        kb.load("sp", xt[:], D["x"][t * 128:(t + 1) * 128, :], [xt])
        rms_rstd(kb, xt[:], 1024, junk, ssq, rstd, [xt])
        kb.stt(hnb[:], xt[:], rstd[:, 0:1], Gat[:], ALU.mult, ALU.mult, [xt, rstd, Gat], [hnb])
        pt = psb[0]
        ptb = pt[:].bitcast(BF16)
        for c in range(8):
            kb.tr(ptb[:, c * 128:(c + 1) * 128], hnb[:, c * 128:(c + 1) * 128], identb[:], [hnb, identb], [pt])
        kb.cp("act", hnT[:].rearrange("p c t -> p (c t)"), ptb, [pt], [hnT])
        pA, pB, pC = psb[1], psb[2], psb[3]
        for c in range(8):
            kb.mm(pA[:, 0:512], hnT[:, c, :], Win[:, c, 0:512], c == 0, c == 7, [hnT, Win], [pA])
        for c in range(8):
            kb.mm(pB[:, 0:256], hnT[:, c, :], Win[:, c, 512:768], c == 0, c == 7, [hnT, Win], [pB])
        for c in range(8):
            kb.mm(pC[:, 0:128], Win[:, c, 768:896], hnT[:, c, :], c == 0, c == 7, [hnT, Win], [pC])
        kb.cp("act", glT[:], pC[:, 0:128], [pC], [glT])
        kb.mm(pC[:, 128:256], glT[:], Wg2[:], True, True, [glT, Wg2], [pC])
        kb.tt("dve", zb[:], pC[:, 128:256], bg[:], ALU.add, [pC, bg], [zb])
        kb.act(ez[:], zb[:], AF.Exp, [zb], [ez], scale=-1.0)
        kb.act(la[:], ez[:], AF.Ln, [ez], [la], bias=1.0)
        pD = psb[4]
        kb.mm(pD[:, 0:128], cst.triuf[:], la[:], True, True, [cst.triuf, la], [pD])
        kb.mm(pD[:, 128:256], cst.onesf[:], la[:], True, True, [cst.onesf, la], [pD])
        kb.mm(pD[:, 256:384], la[:], cst.onesf[:], True, True, [cst.onesf, la], [pD])
        kb.cp("dve", cs[:], pD[:, 0:128], [pD], [cs])
        kb.tt("dve", dd[:], pD[:, 128:256], cs[:], ALU.subtract, [pD, cs], [dd])
        kb.act(epos[:], cs[:], AF.Exp, [cs], [epos], scale=-1.0 / 16)
        kb.act(eneg[:], cs[:], AF.Exp, [cs], [eneg], scale=1.0 / 16)
        kb.act(erel[:], dd[:], AF.Exp, [dd], [erel], scale=-1.0 / 16)
        kb.act(dec[:, 0:1], pD[:, 256:257], AF.Exp, [pD], [dec], scale=-1.0 / 16)
        kb.stt(qd[:], pA[:, 0:128], 128 ** -0.5, epos[:], ALU.mult, ALU.mult, [pA, epos], [qd])
        kb.tt("dve", ki[:], pA[:, 128:256], eneg[:], ALU.mult, [pA, eneg], [ki])
        kb.tt("dve", kd[:], pA[:, 128:256], erel[:], ALU.mult, [pA, erel], [kd])
        kb.cp("act", vb[:], pA[:, 256:512], [pA], [vb])
        pE = psb[5]
        peb = pE[:].bitcast(BF16)
        kb.tr(peb[:, 0:128], qd[:], identb[:], [qd, identb], [pE])
        kb.tr(peb[:, 128:256], ki[:], identb[:], [ki, identb], [pE])
        kb.cp("act", qdT[:], peb[:, 0:128], [pE], [qdT])
        kb.cp("act", kiT[:], peb[:, 128:256], [pE], [kiT])
        pF = psb[6]
        kb.mm(pF[:, 0:128], kiT[:], qdT[:], True, True, [kiT, qdT], [pF])
        kb.tt("dve", at[:], pF[:, 0:128], cst.triuf[:], ALU.mult, [pF, cst.triuf], [at])
        kb.mm(pF[:, 256:512], at[:], vb[:], True, False, [at, vb], [pF])
        kb.mm(pF[:, 256:512], qdT[:], Sb[:], False, True, [qdT, Sb], [pF])
        pG = psb[7]
        kb.mm(pG[:, 0:256], kd[:], vb[:], True, True, [kd, vb], [pG])
        kb.stt(St[:], St[:], dec[:, 0:1], pG[:, 0:256], ALU.mult, ALU.add, [St, dec, pG], [St])
        kb.cp("act", Sb[:], St[:], [St], [Sb])
        rms_rstd(kb, pF[:, 256:512], 256, junk, ssq, rstd, [pF])
        kb.stt(on[:], pF[:, 256:512], rstd[:, 0:1], Gon[:], ALU.mult, ALU.mult, [pF, rstd, Gon], [on])
        kb.act(sr[:], pB[:, 0:256], AF.Silu, [pB], [sr])
        kb.tt("dve", og[:], on[:], sr[:], ALU.mult, [on, sr], [og])
        kb.tr(peb[:, 256:384], og[:, 0:128], identb[:], [og, identb], [pE])
        kb.tr(peb[:, 384:512], og[:, 128:256], identb[:], [og, identb], [pE])
        sp_, j = t // 4, t % 4
        ogs = ogT[sp_ % 2]
        kb.cp("act", ogs[:, 0, j * 128:(j + 1) * 128], peb[:, 256:384], [pE], [ogs])
        kb.cp("act", ogs[:, 1, j * 128:(j + 1) * 128], peb[:, 384:512], [pE], [ogs])
        if j == 3 or t == NT - 1:
            w = (j + 1) * 128
            kb.store("sp", oTd[:, :, sp_ * 512:sp_ * 512 + w], ogs[:, :, 0:w], [ogs], final=True)


def _psum_banks(kb):
    return [kb.ps("psb%d" % i, [128, 512], F32) for i in range(8)]


def build_A(S, debug=False):
    nc = bass.Bass("TRN2", target_bir_lowering=False)
    dt_ = lambda n, sh, dt, kind="ExternalInput": nc.dram_tensor(n, sh, dt, kind=kind).ap()
    D = {
        "x": dt_("x", [S, 1024], F32), "posT": dt_("posT", [128, S // 128], I32),
        "g_attn": dt_("g_attn", [1, 1024], F32), "w_down": dt_("w_down", [1024, 672], F32),
        "g_q": dt_("g_q", [1, 384], F32), "w_uq": dt_("w_uq", [384, 384], F32),
        "g_kv": dt_("g_kv", [1, 256], F32), "w_ukv": dt_("w_ukv", [256, 512], F32),
        "g_qn": dt_("g_qn", [1, 96], F32), "g_kn": dt_("g_kn", [1, 96], F32),
        "oT": dt_("oT", [256, S], BF16, "ExternalOutput"),
    }
    kind = "ExternalOutput" if debug else "Internal"
    D["QT_d"] = dt_("QT_d", [4, 96, S], BF16, kind)
    D["KT_d"] = dt_("KT_d", [4, 96, S], BF16, kind)
    D["V_d"] = dt_("V_d", [4, 128, S // 128, 64], BF16, kind)
    c_d = dt_("cst", [128, 336], F32)
    kb = KB(nc)
    cst = Consts(kb, c_d)
    psb = _psum_banks(kb)
    phase_A(kb, cst, psb, S, D)
    kb.finish()
    return nc


def build_P(NTOK):
    nc = bass.Bass("TRN2", target_bir_lowering=False)
    dt_ = lambda n, sh, dt, kind="ExternalInput": nc.dram_tensor(n, sh, dt, kind=kind).ap()
    D = {
        "oT": dt_("oT", [1024, NTOK], BF16), "resid": dt_("resid", [NTOK, 1024], F32), "w_o": dt_("w_o", [1024, 1024], F32),
        "out": dt_("out", [NTOK, 1024], F32, "ExternalOutput"),
    }
    g_d = dt_("g_ffn", [1, 1024], F32); wq_d = dt_("w_query", [1024, 1024], F32); sk_d = dt_("sub_keys", [2, 128, 64], F32)
    u_d = dt_("u_tab", [16384, 1024], F32); v_d = dt_("v_tab", [16384, 1024], F32)
    c_d = dt_("cst", [128, 336], F32)
    kb = KB(nc)
    cst = Consts(kb, c_d)
    psb = _psum_banks(kb)
    peer = Peer(kb, cst)
    peer.load_weights(g_d, wq_d, sk_d, u_d, v_d, psb[7])
    phase_P(kb, cst, psb, NTOK, D, peer)
    kb.finish()
    return nc


def build_G(S):
    nc = bass.Bass("TRN2", target_bir_lowering=False)
    dt_ = lambda n, sh, dt, kind="ExternalInput": nc.dram_tensor(n, sh, dt, kind=kind).ap()
    D = {
        "x": dt_("x", [S, 1024], F32), "g_attn": dt_("g_attn", [1, 1024], F32), "w_in": dt_("w_in", [1024, 896], F32),
        "w_g2": dt_("w_g2", [128, 128], F32), "b_g": dt_("b_g", [1, 128], F32), "g_on": dt_("g_on", [1, 256], F32),
        "oT": dt_("oT", [256, S], BF16, "ExternalOutput"),
    }
    c_d = dt_("cst", [128, 336], F32)
    kb = KB(nc)
    cst = Consts(kb, c_d)
    psb = _psum_banks(kb)
    phase_G(kb, cst, psb, S, D)
    kb.finish()
    return nc


def _c(a):
    return np.ascontiguousarray(a)


def inputs_A(x_b, pos_b, P, g):
    S = x_b.shape[0]
    hs = slice(4 * g, 4 * g + 4)
    w_uq = P["mla_w_uq"][0].reshape(384, 16, 96)[:, hs, :].reshape(384, 384)
    w_ukv = P["mla_w_ukv"][0].reshape(256, 16, 128)[:, hs, :].reshape(256, 512)
    return dict(x=_c(x_b), posT=_c(pos_b.reshape(S // 128, 128).T.astype(np.int32)),
                g_attn=_c(P["attn_norm_g"][0:1]), w_down=_c(P["mla_w_down"][0]), g_q=_c(P["mla_g_q_lat"][0:1]), w_uq=_c(w_uq),
                g_kv=_c(P["mla_g_kv_lat"][0:1]), w_ukv=_c(w_ukv), g_qn=_c(P["mla_g_qn"][0:1]), g_kn=_c(P["mla_g_kn"][0:1]),
                cst=host_consts())


def inputs_P(oT_tok, resid_tok, w_o, P, layer):
    return dict(oT=_c(oT_tok), resid=_c(resid_tok), w_o=_c(w_o), g_ffn=_c(P["ffn_norm_g"][layer:layer + 1]),
                w_query=_c(P["peer_w_query"][layer]), sub_keys=_c(P["peer_sub_keys"][layer]),
                u_tab=_c(P["peer_u"][layer]), v_tab=_c(P["peer_v"][layer]), cst=host_consts())


def inputs_G(h_b, P, hh):
    w = P["gla_w_in"][0]
    w_in = np.concatenate([w[:, 128 * hh:128 * (hh + 1)], w[:, 512 + 128 * hh:512 + 128 * (hh + 1)],
                           w[:, 1024 + 256 * hh:1024 + 256 * (hh + 1)], w[:, 2048 + 256 * hh:2048 + 256 * (hh + 1)], w[:, 3072:3088],
                           np.zeros((1024, 112), np.float32)], axis=1)
    wg2 = np.zeros((128, 128), np.float32)
    wg2[0:16] = P["gla_w_g2"][0][:, 128 * hh:128 * (hh + 1)]
    return dict(x=_c(h_b), g_attn=_c(P["attn_norm_g"][1:2]), w_in=_c(w_in), w_g2=wg2,
                b_g=_c(P["gla_b_g"][0:1, 128 * hh:128 * (hh + 1)]), g_on=_c(P["gla_g_on"][0:1]), cst=host_consts())


def kernel(**inp):
    P = {k: np.asarray(v) for k, v in inp.items()}
    x = P["x"]
    B, S, _ = x.shape
    ncore = 4 * B
    ids = list(range(ncore))
    TOK = S // 4
    ncA = build_A(S)
    ims = [inputs_A(x[b], P["positions"][b], P, g) for b in range(B) for g in range(4)]
    res = run_bass_kernel_spmd(ncA, ims, core_ids=ids)
    oT = [np.concatenate([np.asarray(res.results[b * 4 + g]["oT"]) for g in range(4)], axis=0) for b in range(B)]
    ncP = build_P(TOK)
    ims = [inputs_P(oT[b][:, j * TOK:(j + 1) * TOK], x[b, j * TOK:(j + 1) * TOK], P["mla_w_o"][0], P, 0) for b in range(B) for j in range(4)]
    res = run_bass_kernel_spmd(ncP, ims, core_ids=ids)
    h1 = np.stack([np.concatenate([np.asarray(res.results[b * 4 + j]["out"]) for j in range(4)], axis=0) for b in range(B)])
    ncG = build_G(S)
    ims = [inputs_G(h1[b], P, hh) for b in range(B) for hh in range(4)]
    res = run_bass_kernel_spmd(ncG, ims, core_ids=ids)
    gT = [np.concatenate([np.asarray(res.results[b * 4 + hh]["oT"]) for hh in range(4)], axis=0) for b in range(B)]
    ncP2 = build_P(TOK)
    ims = [inputs_P(gT[b][:, j * TOK:(j + 1) * TOK], h1[b, j * TOK:(j + 1) * TOK], P["gla_w_o"][0], P, 1) for b in range(B) for j in range(4)]
    res = run_bass_kernel_spmd(ncP2, ims, core_ids=ids)
    out = np.stack([np.concatenate([np.asarray(res.results[b * 4 + j]["out"]) for j in range(4)], axis=0) for b in range(B)])
    return out.astype(np.float32)
```
